# Optimizing a Trainium2 kernel written in Bass

```python
import jax, jax.numpy as jnp
from jax import lax
import numpy as np

D_MODEL = 1024
BATCH = 16
SEQ = 4096
DEPTH = 2

CHUNK = 64
CONV_WIDTH = 4
A_HEADS = 4
A_DK = 64
A_DV = 64
B_HEADS = 4
B_DK = 48
B_DV = 96
GLA_RANK = 16
GLA_GATE_TEMP = 16.0
C_HEADS = 6
C_DK = 64
C_DV = 64

A_QK = A_HEADS * A_DK
A_V = A_HEADS * A_DV
B_QK = B_HEADS * B_DK
B_V = B_HEADS * B_DV
C_QK = C_HEADS * C_DK
C_V = C_HEADS * C_DV
C_QKV = 2 * C_QK + C_V
D_MIX = A_V + B_V + C_V
SPLIT_SIZES = (A_QK, A_QK, A_V, A_V, B_QK, B_QK, B_V, GLA_RANK, B_V, C_QKV, C_HEADS, C_HEADS, C_V)
D_IN = A_QK * 2 + A_V * 2 + B_QK * 2 + B_V * 2 + GLA_RANK + C_QKV + 2 * C_HEADS + C_V

DEEPNORM_ALPHA = (2 * DEPTH) ** 0.25
DEEPNORM_BETA = (8 * DEPTH) ** -0.25
LN_EPS = 1e-5
NORM_EPS = 1e-6
FORGET_FLOOR = 1e-30

kernel_name = 'hybrid_hgrn2_gla_gdn_streaming_layer'


def _layer_norm(x):
    x32 = x.astype(jnp.float32)
    mu = jnp.mean(x32, axis=-1, keepdims=True)
    var = jnp.mean(jnp.square(x32 - mu), axis=-1, keepdims=True)
    return (x32 - mu) * lax.rsqrt(var + LN_EPS)


def _head_rmsnorm(o, gain):
    return o * lax.rsqrt(jnp.mean(o * o, axis=-1, keepdims=True) + NORM_EPS) * gain.astype(jnp.float32)


def _l2norm(t):
    return t * lax.rsqrt(jnp.sum(t * t, axis=-1, keepdims=True) + NORM_EPS)


def _causal_depthwise_conv(x, w):
    s = x.shape[1]
    xp = jnp.pad(x, ((0, 0), (CONV_WIDTH - 1, 0), (0, 0)))
    return sum(xp[:, j:j + s] * w[j] for j in range(CONV_WIDTH))


def _chunk(t):
    b, s, h, d = t.shape
    return t.reshape(b, s // CHUNK, CHUNK, h, d).transpose(1, 0, 3, 2, 4)


def _unchunk(t):
    n, b, h, c, d = t.shape
    return t.transpose(1, 0, 3, 2, 4).reshape(b, n * c, h, d)


def _masked_decay(rel, mask):
    return jnp.where(mask, jnp.exp(jnp.where(mask, rel, 0.0)), 0.0)


def _gla_chunked(q, k, v, log_g):
    qc, kc, vc, gc = _chunk(q), _chunk(k), _chunk(v), _chunk(log_g)
    causal = jnp.tril(jnp.ones((CHUNK, CHUNK), dtype=bool))[:, :, None]

    def step(state, xs):
        qi, ki, vi, gi = xs
        bcum = jnp.cumsum(gi, axis=-2)
        rel = bcum[..., :, None, :] - bcum[..., None, :, :]
        decay = _masked_decay(rel, causal)
        scores = jnp.sum(qi[..., :, None, :] * ki[..., None, :, :] * decay, axis=-1)
        o = (jnp.einsum('bhts,bhsv->bhtv', scores, vi)
             + jnp.einsum('bhtk,bhkv->bhtv', qi * jnp.exp(bcum), state))
        b_last = bcum[..., -1:, :]
        state = (jnp.exp(b_last[..., 0, :])[..., None] * state
                 + jnp.einsum('bhsk,bhsv->bhkv', ki * jnp.exp(b_last - bcum), vi))
        return state, o

    bsz, _, h, dk = q.shape
    state0 = jnp.zeros((bsz, h, dk, v.shape[-1]), jnp.float32)
    _, o = lax.scan(step, state0, (qc, kc, vc, gc))
    return _unchunk(o)


def _gated_delta_chunked(q, k, v, log_a, beta):
    qc, kc, vc = _chunk(q), _chunk(k), _chunk(v)
    gc = _chunk(log_a[..., None])[..., 0]
    bc = _chunk(beta[..., None])[..., 0]
    bcum = jnp.cumsum(gc, axis=-1)
    causal = jnp.tril(jnp.ones((CHUNK, CHUNK), dtype=bool))
    strict = jnp.tril(jnp.ones((CHUNK, CHUNK), dtype=bool), k=-1)
    rel = bcum[..., :, None] - bcum[..., None, :]
    decay = _masked_decay(rel, causal)
    k_beta = kc * bc[..., None]
    v_beta = vc * bc[..., None]
    lower = jnp.where(strict, jnp.einsum('nbhtk,nbhsk->nbhts', k_beta, kc) * decay, 0.0)
    eye = jnp.eye(CHUNK, dtype=lower.dtype)
    t_inv = lax.linalg.triangular_solve(eye + lower, jnp.broadcast_to(eye, lower.shape),
                                        left_side=True, lower=True, unit_diagonal=True)
    u = jnp.einsum('nbhts,nbhsv->nbhtv', t_inv, v_beta)
    w = jnp.einsum('nbhts,nbhsk->nbhtk', t_inv, k_beta * jnp.exp(bcum)[..., None])
    attn = jnp.where(causal, jnp.einsum('nbhtk,nbhsk->nbhts', qc, kc) * decay, 0.0)

    def step(state, xs):
        qi, ki, ui, wi, bi, ai = xs
        v_new = ui - jnp.einsum('bhtk,bhkv->bhtv', wi, state)
        o = (jnp.einsum('bhtk,bhkv->bhtv', qi * jnp.exp(bi)[..., None], state)
             + jnp.einsum('bhts,bhsv->bhtv', ai, v_new))
        b_last = bi[..., -1:]
        state = (jnp.exp(b_last)[..., None] * state
                 + jnp.einsum('bhsk,bhsv->bhkv', ki * jnp.exp(b_last - bi)[..., None], v_new))
        return state, o

    bsz, _, h, dk = q.shape
    state0 = jnp.zeros((bsz, h, dk, v.shape[-1]), jnp.float32)
    _, o = lax.scan(step, state0, (qc, kc, u, w, bcum, attn))
    return _unchunk(o)


def _hybrid_layer(x, c_act, lb, w_in, w_out, ada_w, ada_b, ln_g, ln_b, gla_w_gk, gla_b_gk,
                  gdn_conv_w, gdn_a_log, gdn_dt_bias, gain_a, gain_b, gain_c):
    bsz, s, _ = x.shape
    f32 = jnp.float32
    mod = (c_act @ ada_w + ada_b).astype(f32)
    shift, scale, gate = jnp.split(mod, 3, axis=-1)
    h = _layer_norm(x) * (1 + scale[:, None, :]) + shift[:, None, :]
    proj = (h.astype(x.dtype) @ w_in).astype(f32)
    offsets = [int(o) for o in np.cumsum(SPLIT_SIZES)[:-1]]
    qa, fa, ia, za, qb, kb, vb, lrb, zb, qkvc, ac, bc, zc = jnp.split(proj, offsets, axis=-1)

    def heads(t, n):
        return t.reshape(bsz, s, n, -1)

    fa_logit = heads(fa, A_HEADS)
    lb_h = lb.astype(f32).reshape(A_HEADS, A_DK)
    f_a = lb_h + (1 - lb_h) * jax.nn.sigmoid(fa_logit)
    log_f = jnp.log(jnp.maximum(f_a, FORGET_FLOOR))
    k_a = (1 - lb_h) * jax.nn.sigmoid(-fa_logit)
    o_a = _gla_chunked(jax.nn.silu(heads(qa, A_HEADS)), k_a, heads(ia, A_HEADS), log_f)

    gk = jax.nn.log_sigmoid(lrb @ gla_w_gk.astype(f32) + gla_b_gk.astype(f32)) / GLA_GATE_TEMP
    o_b = _gla_chunked(heads(qb, B_HEADS) * (B_DK ** -0.5), heads(kb, B_HEADS),
                       heads(vb, B_HEADS), heads(gk, B_HEADS))

    qkvc = jax.nn.silu(_causal_depthwise_conv(qkvc, gdn_conv_w.astype(f32)))
    qc, kc, vc = jnp.split(qkvc, [C_QK, 2 * C_QK], axis=-1)
    qc = _l2norm(heads(qc, C_HEADS)) * (C_DK ** -0.5)
    kc = _l2norm(heads(kc, C_HEADS))
    log_a = -jnp.exp(gdn_a_log.astype(f32)) * jax.nn.softplus(ac + gdn_dt_bias.astype(f32))
    beta = jax.nn.sigmoid(bc)
    o_c = _gated_delta_chunked(qc, kc, heads(vc, C_HEADS), log_a, beta)

    y = jnp.concatenate([
        _head_rmsnorm(o_a, gain_a).reshape(bsz, s, A_V) * jax.nn.silu(za),
        _head_rmsnorm(o_b, gain_b).reshape(bsz, s, B_V) * jax.nn.silu(zb),
        _head_rmsnorm(o_c, gain_c).reshape(bsz, s, C_V) * jax.nn.silu(zc),
    ], axis=-1)
    out = (y.astype(x.dtype) @ w_out).astype(f32)
    res = DEEPNORM_ALPHA * x.astype(f32) + gate[:, None, :] * out
    return (_layer_norm(res) * ln_g.astype(f32) + ln_b.astype(f32)).astype(x.dtype)


def setup_inputs(seed: int = 0) -> dict:
    key = jax.random.key(seed)
    ks = jax.random.split(key, 18)
    d = D_MODEL
    x = jax.random.normal(ks[0], (BATCH, SEQ, d), jnp.float32)
    c = jax.random.normal(ks[1], (BATCH, d), jnp.float32)
    b_ = DEEPNORM_BETA
    seg = [(A_QK, 1.0), (A_QK, 1.0), (A_V, b_), (A_V, 1.0),
           (B_QK, 1.0), (B_QK, 1.0), (B_V, b_), (GLA_RANK, 1.0), (B_V, 1.0),
           (2 * C_QK, 1.0), (C_V, b_), (C_HEADS, 1.0), (C_HEADS, 1.0), (C_V, 1.0)]
    col_scale = jnp.concatenate([jnp.full((n,), sc, jnp.float32) for n, sc in seg])
    w_in = jax.random.normal(ks[2], (DEPTH, d, D_IN), jnp.float32) * (d ** -0.5) * col_scale
    w_out = jax.random.normal(ks[3], (DEPTH, D_MIX, d), jnp.float32) * (D_MIX ** -0.5) * DEEPNORM_BETA
    ada_w = jax.random.normal(ks[4], (DEPTH, d, 3 * d), jnp.float32) * (0.5 * d ** -0.5)
    ada_b = jax.random.normal(ks[5], (DEPTH, 3 * d), jnp.float32) * 0.02
    ln_g = 1.0 + 0.02 * jax.random.normal(ks[6], (DEPTH, d), jnp.float32)
    ln_b = 0.02 * jax.random.normal(ks[7], (DEPTH, d), jnp.float32)
    hgrn_lb_logits = 0.5 * jax.random.normal(ks[8], (DEPTH, A_QK), jnp.float32)
    gla_w_gk = jax.random.normal(ks[9], (DEPTH, GLA_RANK, B_QK), jnp.float32) * (GLA_RANK ** -0.5)
    gla_b_gk = 0.1 * jax.random.normal(ks[10], (DEPTH, B_QK), jnp.float32)
    gdn_conv_w = jax.random.normal(ks[11], (DEPTH, CONV_WIDTH, C_QKV), jnp.float32) * (CONV_WIDTH ** -0.5)
    gdn_a_log = jnp.log(jax.random.uniform(ks[12], (DEPTH, C_HEADS), jnp.float32, 1.0, 16.0))
    dt = jnp.exp(jax.random.uniform(ks[13], (DEPTH, C_HEADS), jnp.float32, float(np.log(1e-3)), float(np.log(1e-1))))
    gdn_dt_bias = dt + jnp.log(-jnp.expm1(-dt))
    gain_a = 1.0 + 0.02 * jax.random.normal(ks[14], (DEPTH, A_DV), jnp.float32)
    gain_b = 1.0 + 0.02 * jax.random.normal(ks[15], (DEPTH, B_DV), jnp.float32)
    gain_c = 1.0 + 0.02 * jax.random.normal(ks[16], (DEPTH, C_DV), jnp.float32)
    return {'x': x, 'c': c, 'w_in': w_in, 'w_out': w_out, 'ada_w': ada_w, 'ada_b': ada_b,
            'ln_g': ln_g, 'ln_b': ln_b, 'hgrn_lb_logits': hgrn_lb_logits,
            'gla_w_gk': gla_w_gk, 'gla_b_gk': gla_b_gk, 'gdn_conv_w': gdn_conv_w,
            'gdn_a_log': gdn_a_log, 'gdn_dt_bias': gdn_dt_bias,
            'gain_a': gain_a, 'gain_b': gain_b, 'gain_c': gain_c}


def reference(x, c, w_in, w_out, ada_w, ada_b, ln_g, ln_b, hgrn_lb_logits, gla_w_gk, gla_b_gk,
              gdn_conv_w, gdn_a_log, gdn_dt_bias, gain_a, gain_b, gain_c):
    c_act = jax.nn.silu(c)
    p = jax.nn.softmax(hgrn_lb_logits.astype(jnp.float32), axis=0)
    lb_table = jnp.cumsum(p, axis=0) - p[0:1]
    for l in range(DEPTH):
        x = _hybrid_layer(x, c_act, lb_table[l], w_in[l], w_out[l], ada_w[l], ada_b[l],
                          ln_g[l], ln_b[l], gla_w_gk[l], gla_b_gk[l], gdn_conv_w[l],
                          gdn_a_log[l], gdn_dt_bias[l], gain_a[l], gain_b[l], gain_c[l])
    return x
```

```python
import os
import numpy as np
import concourse.bass as bass
import concourse.mybir as mybir
from concourse.bass_utils import run_bass_kernel_spmd

F32 = mybir.dt.float32
BF16 = mybir.dt.bfloat16
AF = mybir.ActivationFunctionType
ALU = mybir.AluOpType
AX = mybir.AxisListType

D = 1024
SEQ = 4096
BATCH = 16
DEPTH = 2
NCORES = 8
D_IN = 3740
LN_EPS = 1e-5
NORM_EPS = 1e-6
ALPHA = (2 * DEPTH) ** 0.25

OFF_QA, OFF_FA = 0, 256
OFF_QB, OFF_KB = 512, 768
OFF_IA, OFF_AB = 1024, 1280
OFF_VB = 1292
OFF_Z = 1676
OFF_LR = 2700
OFF_C = 2716
NCOLS = OFF_C + 1152 + 4
TM_BANKS = [(0, 512), (512, 512), (1024, 268), (1292, 384), (1676, 512), (2188, 512)]

C_ID, C_MC, C_CST, C_BD32, C_C32, C_LBLK, C_UBLK, C_UBT, C_ONES, C_CIND, C_BONES, C_SEL, C_INVDV, C_BD64S = (
    0, 128, 256, 272, 400, 464, 592, 720, 848, 976, 980, 1108, 1364, 1380)
NCONST = 1508
R_GAIN, R_LNG, R_LNB, R_DT, R_ALOG = 0, 1024, 2048, 3072, 3078
NROW = 3 * D + 16


def make_consts():
    c = np.zeros((128, NCONST), np.float32)
    p = np.arange(128)
    ch = p // 64
    loc = p % 64
    c[:, C_ID:C_ID + 128] = np.eye(128)
    same = ch[:, None] == ch[None, :]
    le = loc[:, None] <= loc[None, :]
    ch32 = p // 32
    loc32 = p % 32
    same32 = ch32[:, None] == ch32[None, :]
    le32 = loc32[:, None] <= loc32[None, :]
    c[:, C_MC:C_MC + 128] = same32 * (le32.astype(np.float32) - (loc32[:, None] <= 15).astype(np.float32))
    for cc in range(4):
        inc = (ch32 == cc)
        c[:, C_CST + 3 * cc + 0] = inc * (loc32 <= 15)
        c[:, C_CST + 3 * cc + 1] = inc
        c[:, C_CST + 3 * cc + 2] = inc * (loc32 > 15)
    c[:, C_BD32:C_BD32 + 128] = same32 * le32
    for cc in range(4):
        c[:, C_C32 + cc] = (ch32 == cc)
    for cc in range(2):
        c[:, C_CIND + cc] = (ch == cc)
    c[:, C_BD64S:C_BD64S + 128] = same * (loc[:, None] < loc[None, :])
    c[:, C_LBLK:C_LBLK + 128] = same * le
    c[:, C_UBLK:C_UBLK + 128] = same * (loc[:, None] > loc[None, :])
    c[:, C_UBT:C_UBT + 128] = same * (loc[:, None] > loc[None, :])
    c[:, C_ONES:C_ONES + 128] = 1.0
    c[:, C_BONES:C_BONES + 128] = same
    for b in range(2):
        c[b, C_SEL + 128 * b:C_SEL + 128 * (b + 1)] = 1.0
    c[:, C_INVDV:C_INVDV + 4] = 1.0 / 64
    c[:, C_INVDV + 4:C_INVDV + 8] = 1.0 / 96
    c[:, C_INVDV + 8:C_INVDV + 14] = 1.0 / 64
    return c


class Buf:
    __slots__ = ("name", "ws", "rs")

    def __init__(self, name):
        self.name = name
        self.ws = {}
        self.rs = {}


class Sched:
    ENG = ("pe", "act", "dve", "pool", "sp")

    def __init__(self, nc):
        self.nc = nc
        self.sems = {}
        self.count = {}
        self.prog = {e: [] for e in self.ENG}
        self.seen = {e: {} for e in self.ENG}
        for e in self.ENG:
            self._sem(e)

    def _sem(self, key):
        if key not in self.sems:
            self.sems[key] = self.nc.alloc_semaphore(name="s_" + key)
            self.count[key] = 0
        return self.sems[key]

    def _deps(self, eng, reads, writes, part):
        deps = {}

        def add(k, v):
            if k == eng and eng == "pe":
                return
            if deps.get(k, 0) < v:
                deps[k] = v
        for b in reads:
            for k, v in b.ws.items():
                add(k, v)
        for b in writes:
            for k, v in b.rs.items():
                add(k, v)
            for k, v in b.ws.items():
                if part and k == eng:
                    continue
                add(k, v)
        waits = []
        sn = self.seen[eng]
        for k, v in deps.items():
            if sn.get(k, 0) < v:
                sn[k] = v
                waits.append((self.sems[k], v))
        return waits

    def _update(self, key, val, reads, writes, part):
        for b in reads:
            if b.rs.get(key, 0) < val:
                b.rs[key] = val
        for b in writes:
            if part and not b.rs:
                b.ws[key] = val
            else:
                b.ws = {key: val}
            b.rs = {}

    def op(self, eng, fn, reads=(), writes=(), part=False):
        waits = self._deps(eng, reads, writes, part)
        self.count[eng] += 1
        n = self.count[eng]
        self.prog[eng].append((waits, fn, self.sems[eng], 1))
        self._update(eng, n, reads, writes, part)

    def dma(self, queue, key, fn, reads=(), writes=()):
        self._sem(key)
        waits = self._deps(queue, reads, writes, False)
        self.count[key] += 16
        n = self.count[key]
        self.prog[queue].append((waits, fn, self.sems[key], 16))
        self._update(key, n, reads, writes, False)

    def final_wait(self, eng, keys):
        waits = [(self.sems[k], self.count[k]) for k in keys if self.count[k] > 0]
        self.prog[eng].append((waits, None, None, 0))

    def emit(self, block):
        nc = self.nc
        prog = self.prog

        def run(e, lst):
            for waits, fn, sem, inc in lst:
                for s, v in waits:
                    e.wait_ge(s, v)
                if fn is not None:
                    fn(e).then_inc(sem, inc)

        @block.tensor
        def _(e):
            run(e, prog["pe"])

        @block.scalar
        def _(e):
            run(e, prog["act"])

        @block.vector
        def _(e):
            run(e, prog["dve"])

        @block.gpsimd
        def _(e):
            run(e, prog["pool"])

        @block.sync
        def _(e):
            run(e, prog["sp"])


def build(nseq, ntile, layers, first_in_x=True, debug=False):
    nc = bass.Bass("TRN2", target_bir_lowering=False)
    NTOK = nseq * ntile * 128
    S = Sched(nc)
    T = 128

    def dram_in(name, shape):
        return nc.dram_tensor(name, list(shape), F32, kind="ExternalInput").ap()

    x_d = dram_in("x", [NTOK, D])
    cT_d = dram_in("cT", [128, 8 * nseq])
    consts_d = dram_in("consts", [128, NCONST])
    out_d = nc.dram_tensor("out", [NTOK, D], F32, kind="ExternalOutput").ap()
    L = {}
    for l in layers:
        L[l] = dict(
            win=dram_in(f"win{l}", [D, NCOLS]), wout=dram_in(f"wout{l}", [D, D]),
            adaw=dram_in(f"adaw{l}", [D, 3 * D]), adab=dram_in(f"adab{l}", [1, 3 * D]),
            rows=dram_in(f"rows{l}", [1, NROW]), logits=dram_in(f"logits{l}", [1, 256 * DEPTH]), wgk=dram_in(f"wgk{l}", [17, 256]),
            convw=dram_in(f"convw{l}", [128, 36]))
    scratch = None
    if len(layers) > 1:
        scratch = nc.dram_tensor("xmid", [NTOK, D], F32, kind="Internal").ap()

    _cnt = [0]

    def sb(shape, dt=F32, name=None):
        _cnt[0] += 1
        nm = (name or "t") + str(_cnt[0])
        return nc.alloc_sbuf_tensor(nm, list(shape), dt), Buf(nm)

    consts, b_consts = sb([128, NCONST], name="consts")
    constb, b_constb = sb([128, 128 + 128 + 64], BF16, name="constb")
    win, b_win = sb([128, 8, NCOLS], BF16, name="win")
    wout, b_wout = sb([128, 8, D], BF16, name="wout")
    rows, b_rows = sb([128, 3 * D + 16], name="rows")
    wgk, b_wgk = sb([17, 256], name="wgk")
    convw, b_convw = sb([128, 9, 4], name="convw")
    cact, b_cact = sb([128, 8 * nseq], name="cact")
    shiftT, b_shiftT = sb([128, 8, nseq], name="shiftT")
    scaleT, b_scaleT = sb([128, 8, nseq], name="scaleT")
    gateb = [sb([128, D], name="gateb") for _ in range(nseq)]
    lbt, b_lbt = sb([128, 256], name="lb")
    omlb, b_omlb = sb([128, 256], name="omlb")
    negA, b_negA = sb([128, 6], name="negA")
    lrT, b_lrT = sb([17, 128], name="lrT")
    S_ab, b_S_ab = sb([128, 320], name="S_ab")
    S_c, b_S_c = sb([128, 3, 64], name="S_c")
    cpre, b_cpre = sb([128, 9, 131], name="cpre")

    ident = consts[:, C_ID:C_ID + 128]
    identb = constb[:, 0:128]
    bonesb = constb[:, 128:256]

    PSB = []
    for i in range(8):
        PSB.append((nc.alloc_psum_tensor(f"ps{i}", [128, 512], F32), Buf(f"ps{i}")))
    _psi = [0]

    def ps():
        r = PSB[_psi[0] % 6]
        _psi[0] += 1
        return r

    epsc, b_epsc = sb([128, 4], name="epsc")

    def act(out, in_, func, reads, writes, bias=None, scale=None, part=False):
        kw = {}
        if bias is not None:
            kw["bias"] = bias
            if not isinstance(bias, (int, float)):
                reads = tuple(reads) + (b_epsc,)
        if scale is not None:
            kw["scale"] = scale
        S.op("act", lambda e: e.activation(out=out, in_=in_, func=func, **kw), reads, writes, part)

    def tt(eng, out, in0, in1, op, reads, writes, part=False):
        S.op(eng, lambda e: e.tensor_tensor(out=out, in0=in0, in1=in1, op=op), reads, writes, part)

    def ts(eng, out, in0, s1, s2, op0, op1, reads, writes, part=False):
        if s2 is None:
            S.op(eng, lambda e: e.tensor_scalar(out=out, in0=in0, scalar1=s1, scalar2=None, op0=op0), reads, writes, part)
        else:
            S.op(eng, lambda e: e.tensor_scalar(out=out, in0=in0, scalar1=s1, scalar2=s2, op0=op0, op1=op1), reads, writes, part)

    def stt(out, in0, scalar, in1, op0, op1, reads, writes, part=False):
        S.op("dve", lambda e: e.scalar_tensor_tensor(out=out, in0=in0, scalar=scalar, in1=in1, op0=op0, op1=op1),
             reads, writes, part)

    def cp(eng, out, in_, reads, writes, part=False):
        if eng == "act":
            act(out, in_, AF.Copy, reads, writes, part=part)
        else:
            S.op(eng, lambda e: e.tensor_copy(out=out, in_=in_), reads, writes, part)

    NOTP = os.environ.get("K_NOTP") == "1"

    def mm(out, lhsT, rhs, start, stop, reads, writes, tp=None, skip=False):
        kw = {}
        if tp is not None and not NOTP:
            kw["tile_position"] = tp
        if skip:
            kw["skip_group_check"] = True
        S.op("pe", lambda e: e.matmul(out, lhsT, rhs, start=start, stop=stop, **kw), reads, writes, True)

    def tr(out, in_, idn, reads, writes, tp=None):
        if tp is None or NOTP:
            S.op("pe", lambda e: e.transpose(out, in_, idn), reads, writes, True)
        else:
            S.op("pe", lambda e: e.transpose(out, in_, idn, tile_position=tp), reads, writes, True)

    def bc(ap, shape, axis):
        return ap.unsqueeze(axis).broadcast_to(list(shape))

    def rstd_from(var_ap, eps, tmp_ap, out_ap, reads, bufs):
        act(tmp_ap, var_ap, AF.Ln, reads, bufs, bias=eps_ap(eps, var_ap))
        act(out_ap, tmp_ap, AF.Exp, bufs, bufs, scale=-0.5)

    def eps_ap(eps, like):
        npart = like.shape[0]
        base = like.base_partition()
        col = {LN_EPS: 0, NORM_EPS: 1, 1.0: 2}[eps]
        return epsc[base:base + npart, col:col + 1]

    S.dma("sp", "cst", lambda e: e.dma_start(out=consts[:, :], in_=consts_d[:, :]), (), (b_consts,))
    S.dma("sp", "cT", lambda e: e.dma_start(out=cact[:, :], in_=cT_d[:, :]), (), (b_cact,))
    S.op("pool", lambda e: e.memset(epsc[:, 0:1], LN_EPS), (), (b_epsc,))
    S.op("pool", lambda e: e.memset(epsc[:, 1:2], NORM_EPS), (b_epsc,), (b_epsc,))
    S.op("pool", lambda e: e.memset(epsc[:, 2:3], 1.0), (b_epsc,), (b_epsc,))
    cp("dve", identb, ident, (b_consts,), (b_constb,))
    cp("dve", bonesb, consts[:, C_BONES:C_BONES + 128], (b_consts, b_constb), (b_constb,))
    act(cact[:, :], cact[:, :], AF.Silu, (b_cact,), (b_cact,))
    S.op("pool", lambda e: e.memset(lrT[:, :], 1.0), (), (b_lrT,))

    xb = [sb([128, D], name="x") for _ in range(2)]
    st6, b_st = sb([128, 12], name="st")
    mv, b_mv = sb([128, 8], name="mv")
    hT, b_hT = sb([128, 8, 128], BF16, name="hT")
    qAs, b_qAs = sb([128, 256], name="qAs")
    kA, b_kA = sb([128, 256], name="kA")
    qkB, b_qkB = sb([128, 512], name="qkB")
    vAB, b_vAB = sb([128, 640], BF16, name="vAB")
    abC, b_abC = sb([128, 12], name="abC")
    zsb = [sb([128, D], name="zs") for _ in range(2)]
    zs, b_zs = zsb[0]
    modrow, b_modrow = zs[0:nseq, 0:512], b_zs
    adab, b_adab = zs[0:1, 512:1024], b_zs
    gAB, b_gAB = sb([128, 512], name="gAB")
    fA, b_fA = gAB[:, 0:256], b_gAB
    epm, b_epm = sb([128, 1024], name="epm")
    ep, b_ep = epm[:, 0:512], b_epm
    em, b_em = epm[:, 512:1024], b_epm
    e1, b_e1 = epm[:, 512:768], b_epm
    utmp, b_utmp = epm[:, 0:320], b_epm
    Sb, b_Sb = epm[:, 512:672].bitcast(BF16), b_epm
    fac, b_fac = sb([128, 4, 12], name="fac")
    qt, b_qt = sb([128, 512], BF16, name="qt")
    kt, b_kt = sb([128, 512], BF16, name="kt")
    qtTm = [sb([128, 4, 128], BF16, name="qtTm") for _ in range(2)]
    ktTm = [sb([128, 4, 128], BF16, name="ktTm") for _ in range(2)]
    bigb, _b = sb([128, 3072], BF16, name="bigb")
    b_bigb = (Buf("M0h"), Buf("M1h"))
    vbd, b_vbd = sb([128, 4, 640], BF16, name="vbd")
    zerob, b_zerob = sb([128, 128], BF16, name="zerob")
    scT, b_scT = sb([128, 8, 128], BF16, name="scT")
    sqr, b_sqr = sb([128, 6, 128], BF16, name="sqr")
    b_csv = Buf("csv")
    cs, b_cs = sb([128, 9, 128], name="cs")
    qnTm = [sb([128, 3, 128], BF16, name="qnTm") for _ in range(2)]
    knTm = [sb([128, 3, 128], BF16, name="knTm") for _ in range(2)]
    gC, b_gC = sb([128, 6], name="gC")
    gtmp, b_gtmp = sb([128, 4, 6], name="gtmp")
    beta, b_beta = sb([128, 6], name="beta")
    nbeta, b_nbeta = sb([128, 6], name="nbeta")
    eb, b_eb = sb([128, 6], name="eb")
    elb, b_elb = sb([128, 6], name="elb")
    gci, b_gci = sb([128, 6, 2], name="gci")
    eblr, b_eblr = sb([128, 6, 2], name="eblr")
    DTf, _b = sb([128, 6, 128], name="DTf")
    B_DTf = (Buf("DTf0"), Buf("DTf1"))
    Mb = [(bigb[:, 768 * i:768 * (i + 1)].rearrange("p (a b) -> p a b", b=128), b_bigb) for i in range(2)]
    MTb = [(bigb[:, 768 * (2 + i):768 * (3 + i)].rearrange("p (a b) -> p a b", b=128), b_bigb) for i in range(2)]
    attnT, _b = sb([128, 6, 128], BF16, name="attnT")
    B_attnT = (Buf("attnT0"), Buf("attnT1"))
    Xbf, _b = sb([128, 6, 128], BF16, name="Xbf")
    B_Xbf = (Buf("Xbf0"), Buf("Xbf1"))
    Xlo, _b = sb([128, 6, 128], BF16, name="Xlo")
    B_Xlo = (Buf("Xlo0"), Buf("Xlo1"))
    X32, _b = sb([128, 6, 128], name="X32")
    B_X32 = (Buf("X320"), Buf("X321"))
    kupdm = [sb([128, 6, 64], BF16, name="kupdm") for _ in range(2)]
    XwTm = [sb([128, 3, 128], BF16, name="XwTm") for _ in range(2)]
    Scb, b_Scb = sb([128, 3, 64], BF16, name="Scb")
    dtmp = DTf[:, :, 0:64]
    vnew, b_vnew = sb([128, 6, 64], BF16, name="vnew")
    ss, b_ss = sb([128, 16], name="ss")
    rs, b_rs = sb([128, 16], name="rs")
    y1, b_y1 = sb([128, D], name="y1")
    yb, b_yb = sb([128, D], BF16, name="yb")
    yT, b_yT = sb([128, 8, 128], BF16, name="yT")
    st6b, b_stb = sb([128, 12], name="stb")
    mvb, b_mvb = sb([128, 8], name="mvb")
    t1, b_t1 = sb([128, D], name="t1")
    xh2b = [sb([128, D], name="xh2")] * 2
    xhat, b_xhat = sb([128, D], name="xhat")
    y2, b_y2 = y1, b_y1
    sq, b_sq = t1, b_t1
    res, b_res = t1, b_t1
    oAB, b_oAB = y1[:, 0:640], b_y1
    oC, b_oC = y1[:, 640:1024].rearrange("p (a b) -> p a b", b=64), b_y1
    gLf, b_gLf = cs[:, 0:6, :], b_cs
    rinv = DTf


    for (t_, b_) in qtTm + ktTm + qnTm + knTm + XwTm + kupdm + [(vnew, b_vnew), (zerob, b_zerob)]:
        S.op("pool", (lambda t_: lambda e: e.memset(t_[:], 0.0))(t_), (), (b_,))

    dbg = {}
    STOP = float(os.environ.get("K_STOP", "99"))

    def early_out(xt, b_xt, r0, dst_d, li, slot):
        S.dma("sp", f"st{li}_{slot}", lambda e: e.dma_start(out=dst_d[r0:r0 + 128, :], in_=xt[:, :]), (b_xt,), ())

    def D_(name, ap, buf, dt=F32):
        if not debug or name in dbg:
            return
        shp = list(ap.shape)
        d = nc.dram_tensor("dbg_" + name, shp, dt, kind="ExternalOutput").ap()
        dbg[name] = d
        S.dma("sp", "dbg_" + name, lambda e: e.dma_start(out=d, in_=ap), (buf,), ())

    pending_tail = [None]

    def run_gens(gens):
        gens = list(gens)
        while gens:
            for g_ in list(gens):
                try:
                    next(g_)
                except StopIteration:
                    gens.remove(g_)

    def layer(l, src_d, dst_d, li):
        W = L[l]
        if STOP <= 0:
            for ti in range(nseq * ntile):
                xt, b_xt = xb[ti % 2]
                r0 = ti * 128
                S.dma("sp", f"xl{li}_{ti % 2}", (lambda xt, r0: lambda e: e.dma_start(out=xt[:, :], in_=src_d[r0:r0 + 128, :]))(xt, r0), (), (b_xt,))
                early_out(xt, b_xt, r0, dst_d, li, ti % 2)
            return
        winr = W["win"].rearrange("(kc p) n -> p kc n", p=128)
        for kc in range(8):
            S.dma("pool", f"win{kc}", (lambda kc: lambda e: e.dma_start(
                out=win[:, kc, :], in_=winr[:, kc, :], max_dma_last_dim=2048))(kc), (), (b_win,))
        woutr = W["wout"].rearrange("(kc p) n -> p kc n", p=128)
        for kc in range(8):
            S.dma("pool", f"wout{kc}", (lambda kc: lambda e: e.dma_start(
                out=wout[:, kc, :], in_=woutr[:, kc, :], max_dma_last_dim=2048))(kc), (), (b_wout,))
        rows_src = bass.AP(W["rows"].tensor, 0, [[0, 128], [1, NROW]])
        S.dma("sp", "rows", lambda e: e.dma_start(out=rows[:, :], in_=rows_src), (), (b_rows,))
        S.dma("sp", "wgk", lambda e: e.dma_start(out=wgk[:, :], in_=W["wgk"][:, :]), (), (b_wgk,))
        S.dma("sp", "convw", lambda e: e.dma_start(out=convw[:, :, :].rearrange("p a b -> p (a b)"), in_=W["convw"][:, :]), (), (b_convw,))
        adawr = W["adaw"].rearrange("(kc p) n -> p kc n", p=128)
        big4 = [xb[0], xb[1], xh2b[0], (xhat, b_xhat)]
        for nchunk in range(6):
            c0 = nchunk * 512
            S.dma("sp", "adab", (lambda c0: lambda e: e.dma_start(out=adab[:, :], in_=W["adab"][:, c0:c0 + 512]))(c0), (), (b_adab,))
            for j4 in range(4):
                bt, b_bt = big4[j4]
                S.dma("sp", f"adaw{j4}", (lambda c0, j4, bt: lambda e: e.dma_start(
                    out=bt[:, :].rearrange("p (a b) -> p a b", b=512), in_=adawr[:, 2 * j4:2 * j4 + 2, c0:c0 + 512]))(c0, j4, bt),
                    (), (b_bt,))
            pt, b_pt = ps()
            for kc in range(8):
                bt, b_bt = big4[kc // 2]
                mm(pt[0:nseq, :], cact[:, kc * nseq:(kc + 1) * nseq], bt[:, (kc % 2) * 512:(kc % 2 + 1) * 512], kc == 0, False,
                   (b_cact, b_bt), (b_pt,))
            mm(pt[0:nseq, :], consts[0:1, C_ONES:C_ONES + nseq], adab[0:1, :], False, True, (b_consts, b_adab), (b_pt,))
            cp("dve", modrow[:, :], pt[0:nseq, :], (b_pt,), (b_modrow,))
            if nchunk < 4:
                p2_, b_p2_ = ps()
                for j in range(4):
                    tr(p2_[:, j * nseq:(j + 1) * nseq], modrow[0:nseq, j * 128:(j + 1) * 128], ident[0:nseq, 0:nseq],
                       (b_modrow, b_consts), (b_p2_,))
                if nchunk < 2:
                    cp("dve", shiftT[:, nchunk * 4:nchunk * 4 + 4, :].rearrange("p a b -> p (a b)"), p2_[:, 0:4 * nseq], (b_p2_,), (b_shiftT,))
                else:
                    ts("dve", scaleT[:, (nchunk - 2) * 4:(nchunk - 2) * 4 + 4, :].rearrange("p a b -> p (a b)"), p2_[:, 0:4 * nseq],
                       1.0, None, ALU.add, None, (b_p2_,), (b_scaleT,))
            else:
                half = nchunk - 4
                for b in range(nseq):
                    g_t, b_g = gateb[b]
                    p2_, b_p2_ = ps()
                    mm(p2_[:, :], consts[0:nseq, C_SEL + 128 * b:C_SEL + 128 * (b + 1)], modrow[0:nseq, :], True, True,
                       (b_consts, b_modrow), (b_p2_,))
                    cp("dve", g_t[:, half * 512:(half + 1) * 512], p2_[:, :], (b_p2_,), (b_g,))
        lbtmp = y1[:, :].rearrange("p (a b) -> p a b", b=256)
        b_lbtmp = b_y1
        lg_src = bass.AP(W["logits"].tensor, 0, [[0, 128], [1, 256 * DEPTH]])
        S.dma("sp", "logits", lambda e: e.dma_start(out=y1[:, 0:256 * DEPTH], in_=lg_src), (), (b_lbtmp,))
        act(y1[:, 0:256 * DEPTH], y1[:, 0:256 * DEPTH], AF.Exp, (b_lbtmp,), (b_lbtmp,))
        den = lbtmp[:, DEPTH, :]
        cp("dve", den, lbtmp[:, 0, :], (b_lbtmp,), (b_lbtmp,))
        for j in range(1, DEPTH):
            tt("dve", den, den, lbtmp[:, j, :], ALU.add, (b_lbtmp,), (b_lbtmp,))
        S.op("dve", lambda e: e.reciprocal(out=den, in_=den), (b_lbtmp,), (b_lbtmp,))
        acc = lbtmp[:, DEPTH + 1, :]
        S.op("dve", lambda e: e.memset(acc, 0.0), (b_lbtmp,), (b_lbtmp,))
        for j in range(1, l + 1):
            tt("dve", acc, acc, lbtmp[:, j, :], ALU.add, (b_lbtmp,), (b_lbtmp,))
        tt("dve", lbt[:, :], acc, den, ALU.mult, (b_lbtmp,), (b_lbt,))
        ts("dve", omlb[:, :], lbt[:, :], -1.0, 1.0, ALU.mult, ALU.add, (b_lbt,), (b_omlb,))
        act(negA[:, :], rows[:, R_ALOG:R_ALOG + 6], AF.Exp, (b_rows,), (b_negA,))
        ts("dve", negA[:, :], negA[:, :], -1.0, None, ALU.mult, None, (b_negA,), (b_negA,))

        gain_b = rows[:, R_GAIN:R_GAIN + D]
        lng_b = rows[:, R_LNG:R_LNG + D]
        lnb_b = rows[:, R_LNB:R_LNB + D]
        dtb = rows[:, R_DT:R_DT + 6]

        segs = [(0, 64), (64, 64), (128, 96), (224, 96)]

        def head_cols(hh):
            if hh < 4:
                return hh * 64, 64
            return 256 + (hh - 4) * 96, 96

        def state_cols(hh):
            ct = hh // 2
            off, dv = segs[ct]
            return off, dv

        def head_gen(b, xt, b_xt, zs, b_zs, r0, slot):
            S.dma("sp", f"xl{li}_{slot}", lambda e: e.dma_start(out=xt[:, :], in_=src_d[r0:r0 + 128, :]), (), (b_xt,))
            S.op("dve", lambda e, xt=xt: e.bn_stats(out=st6[:, 0:6], in_=xt[:, 0:512]), (b_xt,), (b_st,))
            S.op("dve", lambda e, xt=xt: e.bn_stats(out=st6[:, 6:12], in_=xt[:, 512:1024]), (b_xt,), (b_st,), part=True)
            S.op("dve", lambda e: e.bn_aggr(out=mv[:, 0:2], in_=st6[:, 0:12]), (b_st,), (b_mv,))
            rstd_from(mv[:, 1:2], LN_EPS, mv[:, 2:3], mv[:, 3:4], (b_mv,), (b_mv,))
            ts("dve", mv[:, 4:5], mv[:, 0:1], -1.0, mv[:, 3:4], ALU.mult, ALU.mult, (b_mv,), (b_mv,))
            act(xhat[:, :], xt[:, :], AF.Identity, (b_xt, b_mv), (b_xhat,), bias=mv[:, 4:5], scale=mv[:, 3:4])
            yield
            for half in range(2):
                if half == 1:
                    yield
                pt, b_pt = ps()
                for j in range(4):
                    kc = half * 4 + j
                    tr(pt[:, j * 128:(j + 1) * 128], xhat[:, kc * 128:(kc + 1) * 128], ident, (b_xhat, b_consts), (b_pt,))
                for j in range(4):
                    kc = half * 4 + j
                    act(hT[:, kc, :], pt[:, j * 128:(j + 1) * 128], AF.Identity, (b_pt, b_shiftT, b_scaleT), (b_hT,),
                        bias=shiftT[:, kc, b:b + 1], scale=scaleT[:, kc, b:b + 1], part=True)
            yield
            pbank = []
            for (off, n) in TM_BANKS:
                pt, b_pt = ps()
                for kc in range(8):
                    mm(pt[:, 0:n], hT[:, kc, :], win[:, kc, off:off + n], kc == 0, kc == 7, (b_hT, b_win), (b_pt,))
                pbank.append((pt, b_pt))
            p0, b_p0 = pbank[0]
            act(qAs[:, :], p0[:, 0:256], AF.Silu, (b_p0,), (b_qAs,))
            act(fA[:, :], p0[:, 256:512], AF.Sigmoid, (b_p0,), (b_fA,))
            tt("dve", fA[:, :], fA[:, :], omlb[:, :], ALU.mult, (b_fA, b_omlb), (b_fA,))
            tt("dve", fA[:, :], fA[:, :], lbt[:, :], ALU.add, (b_fA, b_lbt), (b_fA,))
            ts("dve", fA[:, :], fA[:, :], 1e-30, None, ALU.max, None, (b_fA,), (b_fA,))
            ts("dve", kA[:, :], fA[:, :], -1.0, 1.0, ALU.mult, ALU.add, (b_fA,), (b_kA,))
            p1, b_p1 = pbank[1]
            cp("act", qkB[:, :], p1[:, :], (b_p1,), (b_qkB,))
            p2, b_p2 = pbank[2]
            cp("act", vAB[:, 0:256], p2[:, 0:256], (b_p2,), (b_vAB,))
            cp("dve", abC[:, :], p2[:, 256:268], (b_p2,), (b_abC,))
            p3, b_p3 = pbank[3]
            cp("act", vAB[:, 256:640], p3[:, 0:384], (b_p3,), (b_vAB,), part=True)
            p4, b_p4 = pbank[4]
            p5, b_p5 = pbank[5]
            act(zs[:, 0:512], p4[:, :], AF.Silu, (b_p4,), (b_zs,))
            act(zs[:, 512:1024], p5[:, :], AF.Silu, (b_p5,), (b_zs,), part=True)
            act(gAB[:, 0:256], fA[:, :], AF.Ln, (b_fA,), (b_gAB,))
            tt("pool", zs[:, :], zs[:, :], gain_b, ALU.mult, (b_zs, b_rows), (b_zs,))
            yield
            pt, b_pt = ps()
            for kc in range(8):
                mm(pt[0:16, 0:128], win[:, kc, OFF_LR:OFF_LR + 16], hT[:, kc, :], kc == 0, kc == 7, (b_hT, b_win), (b_pt,))
            cp("dve", lrT[0:16, :], pt[0:16, 0:128], (b_pt,), (b_lrT,))
            for grp in range(3):
                yield
                pt, b_pt = ps()
                ncts = 4 if grp < 2 else 1
                for j in range(ncts):
                    ct = grp * 4 + j
                    for kc in range(8):
                        mm(pt[:, j * 128:(j + 1) * 128], win[:, kc, OFF_C + ct * 128:OFF_C + (ct + 1) * 128], hT[:, kc, :],
                           kc == 0, kc == 7, (b_hT, b_win), (b_pt,))
                for j in range(ncts):
                    ct = grp * 4 + j
                    cp("act", cpre[:, ct, 3:131], pt[:, j * 128:(j + 1) * 128], (b_pt,), (b_cpre,), part=True)

        tiles = [(b_, it_) for b_ in range(nseq) for it_ in range(ntile)]

        def head_args(k):
            b_, it_ = tiles[k]
            sl_ = k % 2
            return (b_, xb[sl_][0], xb[sl_][1], zsb[sl_][0], zsb[sl_][1], k * 128, sl_)

        run_gens([head_gen(*head_args(0))])
        for b in range(nseq):
            S.op("pool", lambda e: e.memset(S_ab[:, :], 0.0), (), (b_S_ab,))
            S.op("pool", lambda e: e.memset(S_c[:, :, :], 0.0), (), (b_S_c,))
            S.op("pool", lambda e: e.memset(cpre[:, :, 0:3], 0.0), (), (b_cpre,))
            g_t, b_g = gateb[b]
            for it in range(ntile):
                ti = b * ntile + it
                slot = ti % 2
                xt, b_xt = xb[slot]
                xh2, b_xh2 = xh2b[slot]
                zs, b_zs = zsb[slot]
                ot, b_ot = xh2, b_xh2
                r0 = ti * 128
                def chain_ab():
                    pg, b_pg = ps()
                    mm(pg[:, 0:256], lrT[0:17, :], wgk[0:17, :], True, True, (b_lrT, b_wgk), (b_pg,))
                    act(e1, pg[:, 0:256], AF.Exp, (b_pg,), (b_e1,), scale=-1.0)
                    act(e1, e1, AF.Ln, (b_e1,), (b_e1,), bias=eps_ap(1.0, epm[:, 0:1]))
                    ts("dve", gAB[:, 256:512], e1, -1.0 / 16.0, None, ALU.mult, None, (b_e1,), (b_gAB,), part=True)
                    yield
                    pc, b_pc = ps()
                    mm(pc[:, :], consts[:, C_MC:C_MC + 128], gAB[:, :], True, True, (b_consts, b_gAB), (b_pc,))
                    pf, b_pf = ps()
                    for ct in range(4):
                        mm(pf[:, ct * 12:(ct + 1) * 12], gAB[:, ct * 128:(ct + 1) * 128], consts[:, C_CST:C_CST + 12], True, True,
                           (b_gAB, b_consts), (b_pf,))
                    act(fac[:, :, :].rearrange("p a b -> p (a b)"), pf[:, 0:48], AF.Exp, (b_pf,), (b_fac,))
                    act(ep[:, :], pc[:, :], AF.Exp, (b_pc,), (b_ep,))
                    act(em[:, :], pc[:, :], AF.Exp, (b_pc,), (b_em,), scale=-1.0)
                    tt("dve", qt[:, 0:256], qAs[:, :], ep[:, 0:256], ALU.mult, (b_qAs, b_ep), (b_qt,))
                    stt(qt[:, 256:512], qkB[:, 0:256], 48.0 ** -0.5, ep[:, 256:512], ALU.mult, ALU.mult, (b_qkB, b_ep), (b_qt,), part=True)
                    tt("dve", kt[:, 0:256], kA[:, :], em[:, 0:256], ALU.mult, (b_kA, b_em), (b_kt,))
                    tt("dve", kt[:, 256:512], qkB[:, 256:512], em[:, 256:512], ALU.mult, (b_qkB, b_em), (b_kt,), part=True)
                    yield
                    pT, b_pT = ps()
                    pTb = pT[:, :].bitcast(BF16)
                    for ct in range(4):
                        tr(pTb[:, ct * 128:(ct + 1) * 128], qt[:, ct * 128:(ct + 1) * 128], identb, (b_qt, b_constb), (b_pT,))
                    for ct in range(4):
                        tr(pTb[:, 512 + ct * 128:512 + (ct + 1) * 128], kt[:, ct * 128:(ct + 1) * 128], identb, (b_kt, b_constb), (b_pT,))
                    for par in range(2):
                        hs = slice(64 * par, 64 * par + 64)
                        qm, b_qm = qtTm[par]
                        km, b_km = ktTm[par]
                        cp("act", qm[hs, :, :].rearrange("p a b -> p (a b)"), pTb[hs, 0:512], (b_pT,), (b_qm,))
                        cp("act", km[hs, :, :].rearrange("p a b -> p (a b)"), pTb[hs, 512:1024], (b_pT,), (b_km,))
                    for c in range(4):
                        if c % 2 == 0:
                            act(vbd[:, c, :], vAB[:, :], AF.Copy, (b_vAB, b_consts), (b_vbd,), scale=consts[:, C_C32 + c:C_C32 + c + 1], part=(c > 0))
                        else:
                            ts("dve", vbd[:, c, :], vAB[:, :], consts[:, C_C32 + c:C_C32 + c + 1], None, ALU.mult, None,
                               (b_vAB, b_consts), (b_vbd,), part=True)
                    yield
                    psS = [ps(), ps()]
                    for hh in range(8):
                        ct, par = hh // 2, hh % 2
                        pt, b_pt = psS[hh // 4]
                        mm(pt[:, (hh % 4) * 128:(hh % 4 + 1) * 128], ktTm[par][0][:, ct, :], qtTm[par][0][:, ct, :], True, True,
                           (ktTm[par][1], qtTm[par][1]), (b_pt,))
                    for g4 in range(2):
                        pt, b_pt = psS[g4]
                        tt("dve", scT[:, 4 * g4:4 * g4 + 4, :], pt[:, :].rearrange("p (a b) -> p a b", b=128),
                           bc(consts[:, C_BD32:C_BD32 + 128], [128, 4, 128], 1), ALU.mult, (b_pt, b_consts), (b_scT,), part=(g4 > 0))
                    poA, b_poA = PSB[6]
                    poB, b_poB = PSB[7]
                    yield
                    mm(poA[:, 0:256], zerob[:, :], vAB[:, 0:256], True, False, (b_zerob, b_vAB), (b_poA,), skip=True)
                    mm(poB[:, 0:384], zerob[:, :], vAB[:, 256:640], True, False, (b_zerob, b_vAB), (b_poB,), skip=True)
                    for hh in range(8):
                        ocol, dv = head_cols(hh)
                        po, b_po = (poA, b_poA) if hh < 4 else (poB, b_poB)
                        oc = ocol if hh < 4 else ocol - 256
                        mm(po[:, oc:oc + dv], scT[:, hh, :], vAB[:, ocol:ocol + dv], False, False, (b_scT, b_vAB), (b_po,), skip=True)
                    yield "P"
                    for c in range(4):
                        r32 = slice(32 * c, 32 * c + 32)
                        for (c0_, c1_, dv_) in ((0, 2, 64), (2, 4, 96)):
                            o0 = segs[c0_][0]
                            w_ = 2 * dv_
                            tt("dve", Sb[:, o0:o0 + w_].rearrange("p (a b) -> p a b", b=dv_),
                               S_ab[:, o0:o0 + w_].rearrange("p (a b) -> p a b", b=dv_),
                               bc(fac[:, c0_:c1_, 3 * c], [128, 2, dv_], 2), ALU.mult, (b_S_ab, b_fac), (b_Sb,), part=(c0_ > 0))
                        for hh in range(8):
                            ct, par = hh // 2, hh % 2
                            ocol, dv = head_cols(hh)
                            soff, _ = state_cols(hh)
                            po, b_po = (poA, b_poA) if hh < 4 else (poB, b_poB)
                            oc = ocol if hh < 4 else ocol - 256
                            mm(po[r32, oc:oc + dv], qtTm[par][0][:, ct, r32], Sb[:, soff:soff + dv], False, (c == 3 and hh in (3, 7)),
                               (qtTm[par][1], b_Sb), (b_po,), tp=(0, 32 * c), skip=True)
                        yield
                        pu, b_pu = ps()
                        for hh in range(8):
                            ct, par = hh // 2, hh % 2
                            ocol, dv = head_cols(hh)
                            soff, _ = state_cols(hh)
                            mm(pu[par * 64:par * 64 + 64, soff:soff + dv], kt[:, hh * 64:(hh + 1) * 64], vbd[:, c, ocol:ocol + dv],
                               True, True, (b_kt, b_vbd), (b_pu,), tp=(0, par * 64))
                        for (c0_, c1_, dv_) in ((0, 2, 64), (2, 4, 96)):
                            o0 = segs[c0_][0]
                            w_ = 2 * dv_
                            tt("dve", utmp[:, o0:o0 + w_].rearrange("p (a b) -> p a b", b=dv_),
                               pu[:, o0:o0 + w_].rearrange("p (a b) -> p a b", b=dv_),
                               bc(fac[:, c0_:c1_, 3 * c + 2], [128, 2, dv_], 2), ALU.mult, (b_pu, b_fac), (b_utmp,), part=(c0_ > 0))
                        for (c0_, c1_, dv_) in ((0, 2, 64), (2, 4, 96)):
                            o0 = segs[c0_][0]
                            w_ = 2 * dv_
                            tt("dve", S_ab[:, o0:o0 + w_].rearrange("p (a b) -> p a b", b=dv_),
                               S_ab[:, o0:o0 + w_].rearrange("p (a b) -> p a b", b=dv_),
                               bc(fac[:, c0_:c1_, 3 * c + 1], [128, 2, dv_], 2), ALU.mult, (b_S_ab, b_fac), (b_S_ab,))
                        tt("dve", S_ab[:, :], S_ab[:, :], utmp[:, :], ALU.add, (b_S_ab, b_utmp), (b_S_ab,))
                        yield
                    cp("act", oAB[:, 0:256], poA[:, 0:256], (b_poA,), (b_oAB,))
                    cp("act", oAB[:, 256:640], poB[:, 0:384], (b_poB,), (b_oAB,), part=True)
                def chain_c():
                    for ct in range(9):
                        bq = b_cs if ct < 6 else b_csv
                        ts("dve", cs[:, ct, :], cpre[:, ct, 0:128], convw[:, ct, 0:1], None, ALU.mult, None, (b_cpre, b_convw), (bq,),
                           part=(ct % 6 > 0))
                        for j in range(1, 4):
                            stt(cs[:, ct, :], cpre[:, ct, j:j + 128], convw[:, ct, j:j + 1], cs[:, ct, :], ALU.mult, ALU.add,
                                (b_cpre, b_convw, bq), (bq,), part=True)
                        if ct == 5:
                            act(cs[:, 0:6, :].rearrange("p a b -> p (a b)"), cs[:, 0:6, :].rearrange("p a b -> p (a b)"), AF.Silu, (b_cs,), (b_cs,))
                            act(sqr[:, :, :].rearrange("p a b -> p (a b)"), cs[:, 0:6, :].rearrange("p a b -> p (a b)"), AF.Square, (b_cs,), (b_sqr,))
                            pn = [ps(), ps()]
                            for j in range(6):
                                pt, b_pt = pn[j // 4]
                                mm(pt[:, (j % 4) * 128:(j % 4 + 1) * 128], bonesb, sqr[:, j, :], True, True, (b_constb, b_sqr), (b_pt,))
                            for (pt, b_pt), lo, n in ((pn[0], 0, 4), (pn[1], 4, 2)):
                                dst = rinv[:, lo:lo + n, :].rearrange("p a b -> p (a b)")
                                act(dst, pt[:, 0:n * 128], AF.Ln, (b_pt,), (*B_DTf,), bias=eps_ap(NORM_EPS, pt[:, 0:1]), part=True)
                                act(dst, dst, AF.Exp, (*B_DTf,), (*B_DTf,), scale=-0.5)
                        if ct == 8:
                            act(cs[:, 6:9, :].rearrange("p a b -> p (a b)"), cs[:, 6:9, :].rearrange("p a b -> p (a b)"), AF.Silu, (b_csv,), (b_csv,))
                        if ct % 3 == 2:
                            yield
                    cp("pool", cpre[:, :, 0:3], cpre[:, :, 128:131], (b_cpre,), (b_cpre,))
                    for par in range(2):
                        hs = slice(64 * par, 64 * par + 64)
                        qm, b_qm = qnTm[par]
                        stt(qm[hs, :, :], cs[hs, 0:3, :], 0.125, rinv[hs, 0:3, :], ALU.mult, ALU.mult, (b_cs, *B_DTf), (b_qm,))
                    tt("dve", rinv[:, 3:6, :], cs[:, 3:6, :], rinv[:, 3:6, :], ALU.mult, (b_cs, *B_DTf), (*B_DTf,))
                    for par in range(2):
                        hs = slice(64 * par, 64 * par + 64)
                        km, b_km = knTm[par]
                        cp("act", km[hs, :, :].rearrange("p a b -> p (a b)"), rinv[hs, 3:6, :].rearrange("p a b -> p (a b)"), (*B_DTf,), (b_km,))
                    yield
                    pk, b_pk = ps()
                    for j in range(3):
                        tr(pk[:, j * 128:(j + 1) * 128], rinv[:, 3 + j, :], ident, (*B_DTf, b_consts), (b_pk,))
                    pv, b_pv = ps()
                    for j in range(3):
                        tr(pv[:, j * 128:(j + 1) * 128], cs[:, 6 + j, :], ident, (b_csv, b_consts), (b_pv,))
                    cp("act", X32[:, :, 0:64], pv[:, 0:384].rearrange("p (a b) -> p a b", b=64), (b_pv,), (*B_X32,))
                    tt("dve", gtmp[:, 0, :], abC[:, 0:6], dtb, ALU.add, (b_abC, b_rows), (b_gtmp,))
                    act(gtmp[:, 1, :], gtmp[:, 0, :], AF.Exp, (b_gtmp,), (b_gtmp,))
                    act(gtmp[:, 2, :], gtmp[:, 1, :], AF.Ln, (b_gtmp,), (b_gtmp,), bias=eps_ap(1.0, gtmp[:, 0, 0:1]))
                    tt("dve", gC[:, :], gtmp[:, 2, :], negA[:, :], ALU.mult, (b_gtmp, b_negA), (b_gC,))
                    act(beta[:, :], abC[:, 6:12], AF.Exp, (b_abC,), (b_beta,), scale=-1.0)
                    ts("dve", beta[:, :], beta[:, :], 1.0, None, ALU.add, None, (b_beta,), (b_beta,))
                    S.op("dve", lambda e: e.reciprocal(out=beta[:, :], in_=beta[:, :]), (b_beta,), (b_beta,))
                    ts("dve", nbeta[:, :], beta[:, :], -1.0, None, ALU.mult, None, (b_beta,), (b_nbeta,))
                    pb, b_pb = ps()
                    mm(pb[:, 0:6], consts[:, C_LBLK:C_LBLK + 128], gC[:, :], True, True, (b_consts, b_gC), (b_pb,))
                    mm(pb[:, 8:14], consts[:, C_UBLK:C_UBLK + 128], gC[:, :], True, True, (b_consts, b_gC), (b_pb,))
                    tt("dve", gci[:, :, :], bc(gC[:, :], [128, 6, 2], 2), bc(consts[:, C_CIND:C_CIND + 2], [128, 6, 2], 1), ALU.mult,
                       (b_gC, b_consts), (b_gci,))
                    mm(pb[:, 16:28], consts[:, C_ONES:C_ONES + 128], gci[:, :, :].rearrange("p a b -> p (a b)"), True, True,
                       (b_consts, b_gci), (b_pb,))
                    act(eb[:, :], pb[:, 0:6], AF.Exp, (b_pb,), (b_eb,))
                    act(elb[:, :], pb[:, 8:14], AF.Exp, (b_pb,), (b_elb,))
                    act(eblr[:, :, :].rearrange("p a b -> p (a b)"), pb[:, 16:28], AF.Exp, (b_pb,), (b_eblr,))
                    pk3 = pk[:, 0:384].rearrange("p (a b) -> p a b", b=64)
                    tt("dve", X32[:, :, 64:128], pk3, bc(eb[:, :], [128, 6, 64], 2), ALU.mult, (b_pk, b_eb), (*B_X32,), part=True)
                    for c in range(2):
                        rsl = slice(64 * c, 64 * c + 64)
                        kum, b_kum = kupdm[c]
                        tt("dve", kum[rsl, :, :], pk3[rsl, :, :], bc(elb[rsl, :], [64, 6, 64], 2), ALU.mult, (b_pk, b_elb), (b_kum,))
                    cp("act", Xbf[:, :, :].rearrange("p a b -> p (a b)"), X32[:, :, :].rearrange("p a b -> p (a b)"), (*B_X32,), (*B_Xbf,))
                    tt("dve", Xlo[:, :, :], X32[:, :, :], Xbf[:, :, :], ALU.subtract, (*B_X32, *B_Xbf), (*B_Xlo,))
                    yield
                    yield "P"
                    tt("dve", gLf, bc(consts[:, C_LBLK:C_LBLK + 128], [128, 6, 128], 1), bc(gC[:, :], [128, 6, 128], 2), ALU.mult,
                       (b_consts, b_gC), (b_gLf,))
                    prs = [ps(), ps()]
                    for g3 in range(2):
                        pt, b_pt = prs[g3]
                        mm(pt[:, 0:384], consts[:, C_UBT:C_UBT + 128], gLf[:, 3 * g3:3 * g3 + 3, :].rearrange("p a b -> p (a b)"), True, True,
                           (b_consts, b_gLf), (b_pt,))
                    for g3 in range(2):
                        pt, b_pt = prs[g3]
                        act(DTf[:, 3 * g3:3 * g3 + 3, :].rearrange("p a b -> p (a b)"), pt[:, 0:384], AF.Exp, (b_pt,), (*B_DTf,), part=(g3 > 0))
                    tt("dve", DTf[:, :, :], DTf[:, :, :], bc(consts[:, C_LBLK:C_LBLK + 128], [128, 6, 128], 1), ALU.mult, (*B_DTf, b_consts), (*B_DTf,))
                    yield
                    M0 = Mb[0][0]
                    MT0 = MTb[0][0]

                    def neumann(g):
                        sl = slice(3 * g, 3 * g + 3)
                        bX32, bXbf, bXlo, bM = B_X32[g], B_Xbf[g], B_Xlo[g], b_bigb[g]
                        bDT, bAT = B_DTf[g], B_attnT[g]
                        pkk_, b_pkk = ps()
                        pkq_, b_pkq = ps()
                        for j in range(3):
                            h = 3 * g + j
                            ct, par = h // 2, h % 2
                            km, b_km = knTm[par]
                            qm, b_qm = qnTm[par]
                            mm(pkk_[:, j * 128:(j + 1) * 128], km[:, ct, :], km[:, ct, :], True, True, (b_km,), (b_pkk,))
                            mm(pkq_[:, j * 128:(j + 1) * 128], km[:, ct, :], qm[:, ct, :], True, True, (b_km, b_qm), (b_pkq,))
                        tt("dve", attnT[:, sl, :], pkq_[:, 0:384].rearrange("p (a b) -> p a b", b=128), DTf[:, sl, :], ALU.mult,
                           (b_pkq, bDT), (bAT,))
                        tt("dve", DTf[:, sl, :], DTf[:, sl, :], bc(consts[:, C_BD64S:C_BD64S + 128], [128, 3, 128], 1), ALU.mult,
                           (bDT, b_consts), (bDT,))
                        tt("dve", DTf[:, sl, :], pkk_[:, 0:384].rearrange("p (a b) -> p a b", b=128), DTf[:, sl, :], ALU.mult,
                           (b_pkk, bDT), (bDT,))
                        tt("dve", M0[:, sl, :], DTf[:, sl, :], bc(nbeta[:, sl], [128, 3, 128], 2), ALU.mult, (bDT, b_nbeta), (bM,))
                        yield
                        pm, b_pm = ps()
                        pmb = pm[:, :].bitcast(BF16)
                        for j in range(3):
                            h = 3 * g + j
                            tr(pmb[:, j * 128:(j + 1) * 128], M0[:, h, :], identb, (bM, b_constb), (b_pm,))
                        cp("act", MT0[:, sl, :].rearrange("p a b -> p (a b)"), pmb[:, 0:384], (b_pm,), (bM,))
                        yield
                        for lv in range(6):
                            Mc_ = Mb[lv % 2][0]
                            MTc = MTb[lv % 2][0]
                            pt, b_pt = ps()
                            for j in range(3):
                                h = 3 * g + j
                                mm(pt[:, j * 128:(j + 1) * 128], Mc_[:, h, :], Xbf[:, h, :], True, False, (bM, bXbf), (b_pt,))
                                mm(pt[:, j * 128:(j + 1) * 128], Mc_[:, h, :], Xlo[:, h, :], False, True, (bM, bXlo), (b_pt,))
                            tt("dve", X32[:, sl, :], pt[:, 0:384].rearrange("p (a b) -> p a b", b=128), X32[:, sl, :], ALU.add,
                               (b_pt, bX32), (bX32,))
                            cp("dve", Xbf[:, sl, :].rearrange("p a b -> p (a b)"), X32[:, sl, :].rearrange("p a b -> p (a b)"), (bX32,), (bXbf,))
                            if lv < 5:
                                tt("dve", Xlo[:, sl, :], X32[:, sl, :], Xbf[:, sl, :], ALU.subtract, (bX32, bXbf), (bXlo,))
                            yield
                            if lv < 5:
                                Mn = Mb[(lv + 1) % 2][0]
                                MTn = MTb[(lv + 1) % 2][0]
                                pt1, b_pt1 = ps()
                                pt2, b_pt2 = ps()
                                for j in range(3):
                                    h = 3 * g + j
                                    mm(pt1[:, j * 128:(j + 1) * 128], MTc[:, h, :], Mc_[:, h, :], True, True, (bM,), (b_pt1,))
                                for j in range(3):
                                    h = 3 * g + j
                                    mm(pt2[:, j * 128:(j + 1) * 128], Mc_[:, h, :], MTc[:, h, :], True, True, (bM,), (b_pt2,))
                                cp("act", Mn[:, sl, :].rearrange("p a b -> p (a b)"), pt1[:, 0:384], (b_pt1,), (bM,))
                                cp("act", MTn[:, sl, :].rearrange("p a b -> p (a b)"), pt2[:, 0:384], (b_pt2,), (bM,))
                                yield
                    yield
                    subs = [neumann(0), neumann(1)]
                    while subs:
                        for g_ in list(subs):
                            try:
                                next(g_)
                            except StopIteration:
                                subs.remove(g_)
                        yield
                    Xf = Xbf
                    yield
                    pw, b_pw = ps()
                    pwb = pw[:, :].bitcast(BF16)
                    for h in range(6):
                        ct, par = h // 2, h % 2
                        if par == 0:
                            tr(pwb[0:64, ct * 128:(ct + 1) * 128], Xf[:, h, 64:128], identb, (*B_Xbf, b_constb), (b_pw,))
                        else:
                            tr(pwb[:, 384 + ct * 128:384 + (ct + 1) * 128], Xf[:, h, :], identb, (*B_Xbf, b_constb), (b_pw,))
                    cp("act", XwTm[0][0][0:64, :, :].rearrange("p a b -> p (a b)"), pwb[0:64, 0:384], (b_pw,), (XwTm[0][1],))
                    cp("act", XwTm[1][0][64:128, :, :].rearrange("p a b -> p (a b)"), pwb[64:128, 384:768], (b_pw,), (XwTm[1][1],))
                    yield
                    for c in range(2):
                        rsl = slice(64 * c, 64 * c + 64)
                        kum, b_kum = kupdm[c]
                        cp("dve", Scb[:, :, :], S_c[:, :, :], (b_S_c,), (b_Scb,))
                        pg0, b_pg0 = ps()
                        pg1, b_pg1 = ps()
                        for h in range(6):
                            ct, par = h // 2, h % 2
                            mm(pg0[rsl, h * 64:(h + 1) * 64], XwTm[par][0][:, ct, 64 * c:64 * c + 64], Scb[:, ct, :], True, True,
                               (XwTm[par][1], b_Scb), (b_pg0,), tp=(0, 64 * c))
                            mm(pg1[rsl, h * 64:(h + 1) * 64], qnTm[par][0][:, ct, 64 * c:64 * c + 64], Scb[:, ct, :], True, True,
                               (qnTm[par][1], b_Scb), (b_pg1,), tp=(0, 64 * c))
                        tt("dve", dtmp[rsl, :, :], X32[rsl, :, 0:64], pg0[rsl, 0:384].rearrange("p (a b) -> p a b", b=64), ALU.subtract,
                           (*B_X32, b_pg0), (*B_DTf,))
                        tt("dve", vnew[rsl, :, :], dtmp[rsl, :, :], bc(beta[rsl, :], [64, 6, 64], 2), ALU.mult, (*B_DTf, b_beta), (b_vnew,))
                        tt("dve", oC[rsl, :, :], pg1[rsl, 0:384].rearrange("p (a b) -> p a b", b=64), bc(eb[rsl, :], [64, 6, 64], 2), ALU.mult,
                           (b_pg1, b_eb), (b_oC,))
                        yield
                        pg2, b_pg2 = ps()
                        pg3, b_pg3 = ps()
                        for h in range(6):
                            ct, par = h // 2, h % 2
                            mm(pg2[rsl, h * 64:(h + 1) * 64], attnT[:, h, 64 * c:64 * c + 64], vnew[:, h, :], True, True, (*B_attnT, b_vnew), (b_pg2,),
                               tp=(0, 64 * c))
                            mm(pg3[par * 64:par * 64 + 64, ct * 64:(ct + 1) * 64], kum[:, h, :], vnew[:, h, :], True, True,
                               (b_kum, b_vnew), (b_pg3,), tp=(0, par * 64))
                        tt("dve", oC[rsl, :, :], oC[rsl, :, :], pg2[rsl, 0:384].rearrange("p (a b) -> p a b", b=64), ALU.add,
                           (b_oC, b_pg2), (b_oC,))
                        for par in range(2):
                            psl = slice(par * 64, par * 64 + 64)
                            ebv = eblr[psl, :, c].rearrange("p (a b) -> p a b", b=2)[:, :, par]
                            tt("dve", S_c[psl, :, :], S_c[psl, :, :], bc(ebv, [64, 3, 64], 2), ALU.mult, (b_S_c, b_eblr), (b_S_c,))
                        tt("dve", S_c[:, :, :], S_c[:, :, :], pg3[:, 0:192].rearrange("p (a b) -> p a b", b=64), ALU.add, (b_S_c, b_pg3), (b_S_c,))
                        yield
                chains = [chain_c(), chain_ab()]
                tail_prev = pending_tail[0]
                pending_tail[0] = None
                active = list(chains) + ([tail_prev] if tail_prev is not None else [])
                nxt = head_gen(*head_args(ti + 1)) if ti + 1 < len(tiles) else None
                passed = 0
                head_added = False
                noovl = os.environ.get("K_NOOVL") == "1"
                while active:
                    for g_ in list(active):
                        try:
                            tok = next(g_)
                        except StopIteration:
                            active.remove(g_)
                            continue
                        if tok == "P":
                            passed += 1
                    tail_done = tail_prev is None or tail_prev not in active
                    chains_done = not any(c_ in active for c_ in chains)
                    if nxt is not None and not head_added and tail_done and ((passed >= 2 and not noovl) or chains_done):
                        active.append(nxt)
                        head_added = True
                if nxt is not None and not head_added:
                    run_gens([nxt])
                D_("gC", gC[:, :], b_gC); D_("beta", beta[:, :], b_beta)
                D_("oC", y1[:, 640:1024], b_oC); D_("S_c", S_c[:, :, :].rearrange("p a b -> p (a b)"), b_S_c)
                act(sq[:, 0:640], oAB[:, :], AF.Square, (b_oAB,), (b_sq,))
                act(sq[:, 640:1024], oC[:, :, :].rearrange("p a b -> p (a b)"), AF.Square, (b_oC,), (b_sq,), part=True)
                S.op("dve", lambda e: e.tensor_reduce(out=ss[:, 0:4], in_=sq[:, 0:256].rearrange("p (a b) -> p a b", b=64), axis=AX.X, op=ALU.add),
                     (b_sq,), (b_ss,))
                S.op("dve", lambda e: e.tensor_reduce(out=ss[:, 4:8], in_=sq[:, 256:640].rearrange("p (a b) -> p a b", b=96), axis=AX.X, op=ALU.add),
                     (b_sq,), (b_ss,), part=True)
                S.op("dve", lambda e: e.tensor_reduce(out=ss[:, 8:14], in_=sq[:, 640:1024].rearrange("p (a b) -> p a b", b=64), axis=AX.X, op=ALU.add),
                     (b_sq,), (b_ss,), part=True)
                tt("dve", ss[:, 0:14], ss[:, 0:14], consts[:, C_INVDV:C_INVDV + 14], ALU.mult, (b_ss, b_consts), (b_ss,))
                act(rs[:, 0:14], ss[:, 0:14], AF.Ln, (b_ss,), (b_rs,), bias=eps_ap(NORM_EPS, ss[:, 0:1]))
                act(rs[:, 0:14], rs[:, 0:14], AF.Exp, (b_rs,), (b_rs,), scale=-0.5)
                tt("dve", y1[:, 0:256].rearrange("p (a b) -> p a b", b=64), oAB[:, 0:256].rearrange("p (a b) -> p a b", b=64),
                   bc(rs[:, 0:4], [128, 4, 64], 2), ALU.mult, (b_oAB, b_rs), (b_y1,))
                tt("dve", y1[:, 256:640].rearrange("p (a b) -> p a b", b=96), oAB[:, 256:640].rearrange("p (a b) -> p a b", b=96),
                   bc(rs[:, 4:8], [128, 4, 96], 2), ALU.mult, (b_oAB, b_rs), (b_y1,), part=True)
                tt("dve", y1[:, 640:1024].rearrange("p (a b) -> p a b", b=64), oC[:, :, :],
                   bc(rs[:, 8:14], [128, 6, 64], 2), ALU.mult, (b_oC, b_rs), (b_y1,), part=True)
                tt("dve", yb[:, :], y2[:, :], zs[:, :], ALU.mult, (b_y2, b_zs), (b_yb,))
                def tail_gen(xt=xt, b_xt=b_xt, xh2=xh2, b_xh2=b_xh2, ot=ot, b_ot=b_ot, r0=r0, g_t=g_t, b_g=b_g, slot=slot):
                    py, b_py = ps()
                    pyb = py[:, :].bitcast(BF16)
                    for kc in range(8):
                        tr(pyb[:, kc * 128:(kc + 1) * 128], yb[:, kc * 128:(kc + 1) * 128], identb, (b_yb, b_constb), (b_py,))
                    cp("act", yT[:, :, :].rearrange("p a b -> p (a b)"), pyb[:, :], (b_py,), (b_yT,))
                    yield
                    pos = [ps(), ps()]
                    for nb in range(2):
                        pt, b_pt = pos[nb]
                        for kc in range(8):
                            mm(pt[:, :], yT[:, kc, :], wout[:, kc, nb * 512:(nb + 1) * 512], kc == 0, kc == 7, (b_yT, b_wout), (b_pt,))
                    for nb in range(2):
                        pt, b_pt = pos[nb]
                        tt("dve", t1[:, nb * 512:(nb + 1) * 512], pt[:, :], g_t[:, nb * 512:(nb + 1) * 512], ALU.mult, (b_pt, b_g), (b_t1,),
                           part=(nb > 0))
                    stt(res[:, :], xt[:, :], ALU_ALPHA, t1[:, :], ALU.mult, ALU.add, (b_xt, b_t1), (b_res,))
                    yield
                    S.op("dve", lambda e: e.bn_stats(out=st6b[:, 0:6], in_=res[:, 0:512]), (b_res,), (b_stb,))
                    S.op("dve", lambda e: e.bn_stats(out=st6b[:, 6:12], in_=res[:, 512:1024]), (b_res,), (b_stb,), part=True)
                    S.op("dve", lambda e: e.bn_aggr(out=mvb[:, 0:2], in_=st6b[:, 0:12]), (b_stb,), (b_mvb,))
                    rstd_from(mvb[:, 1:2], LN_EPS, mvb[:, 2:3], mvb[:, 3:4], (b_mvb,), (b_mvb,))
                    ts("dve", mvb[:, 4:5], mvb[:, 0:1], -1.0, mvb[:, 3:4], ALU.mult, ALU.mult, (b_mvb,), (b_mvb,))
                    yield
                    act(xh2[:, :], res[:, :], AF.Identity, (b_res, b_mvb), (b_xh2,), bias=mvb[:, 4:5], scale=mvb[:, 3:4])
                    tt("pool", xh2[:, :], xh2[:, :], lng_b, ALU.mult, (b_xh2, b_rows), (b_xh2,))
                    tt("pool", ot[:, :], xh2[:, :], lnb_b, ALU.add, (b_xh2, b_rows), (b_ot,))
                    D_("y1", y1[:, :], b_y1); D_("t1", t1[:, :], b_t1); D_("rs", rs[:, 0:14], b_rs)
                    S.dma("sp", f"st{li}_{slot}", (lambda ot, r0: lambda e: e.dma_start(out=dst_d[r0:r0 + 128, :], in_=ot[:, :]))(ot, r0),
                          (b_ot,), ())

                pending_tail[0] = tail_gen()

        run_gens([pending_tail[0]] if pending_tail[0] is not None else [])
        pending_tail[0] = None

    ALU_ALPHA = float(ALPHA)
    nl = len(layers)
    for li, l in enumerate(layers):
        src = x_d if li == 0 else scratch
        dst = out_d if li == nl - 1 else scratch
        if li > 0:
            S.final_wait("sp", [k for k in S.count if k.startswith(f"st{li - 1}_")])
        layer(l, src, dst, li)
    S.final_wait("sp", [k for k in S.count if k.startswith(f"st{nl - 1}_") or k.startswith("dbg_")])
    with nc.Block() as block:
        S.emit(block)
    return nc


def prep_layer_inputs(l, w_in, w_out, ada_w, ada_b, ln_g, ln_b, hgrn_lb_logits, gla_w_gk, gla_b_gk,
                      gdn_conv_w, gdn_a_log, gdn_dt_bias, gain_a, gain_b, gain_c):
    w = np.asarray(w_in[l], np.float32)
    offs = np.cumsum([0, 256, 256, 256, 256, 192, 192, 384, 16, 384, 1152, 6, 6, 384])
    qa, fa, ia, za, qb, kb, vb, lr, zb, qkvc, ac, bcc, zc = [w[:, offs[i]:offs[i + 1]] for i in range(13)]
    wp = np.zeros((D, NCOLS), np.float32)
    wp[:, OFF_QA:OFF_QA + 256] = qa
    wp[:, OFF_FA:OFF_FA + 256] = fa
    for h in range(4):
        wp[:, OFF_QB + 64 * h:OFF_QB + 64 * h + 48] = qb[:, 48 * h:48 * (h + 1)]
        wp[:, OFF_KB + 64 * h:OFF_KB + 64 * h + 48] = kb[:, 48 * h:48 * (h + 1)]
    wp[:, OFF_IA:OFF_IA + 256] = ia
    wp[:, OFF_AB:OFF_AB + 6] = ac
    wp[:, OFF_AB + 6:OFF_AB + 12] = bcc
    wp[:, OFF_VB:OFF_VB + 384] = vb
    wp[:, OFF_Z:OFF_Z + 256] = za
    wp[:, OFF_Z + 256:OFF_Z + 640] = zb
    wp[:, OFF_Z + 640:OFF_Z + 1024] = zc
    wp[:, OFF_LR:OFF_LR + 16] = lr
    wp[:, OFF_C:OFF_C + 1152] = qkvc
    rows = np.zeros((1, NROW), np.float32)
    rows[0, R_GAIN:R_GAIN + 1024] = np.concatenate([np.tile(gain_a[l], 4), np.tile(gain_b[l], 4), np.tile(gain_c[l], 6)])
    rows[0, R_LNG:R_LNG + 1024] = ln_g[l]
    rows[0, R_LNB:R_LNB + 1024] = ln_b[l]
    rows[0, R_DT:R_DT + 6] = gdn_dt_bias[l]
    rows[0, R_ALOG:R_ALOG + 6] = gdn_a_log[l]
    wgk = np.zeros((17, 256), np.float32)
    for h in range(4):
        wgk[0:16, 64 * h:64 * h + 48] = gla_w_gk[l][:, 48 * h:48 * (h + 1)]
        wgk[16, 64 * h:64 * h + 48] = gla_b_gk[l][48 * h:48 * (h + 1)]
    cw = np.asarray(gdn_conv_w[l], np.float32)
    convw = np.ascontiguousarray(cw.T.reshape(9, 128, 4).transpose(1, 0, 2)).reshape(128, 36)
    return {f"win{l}": wp, f"wout{l}": np.ascontiguousarray(w_out[l], np.float32),
            f"adaw{l}": np.ascontiguousarray(ada_w[l], np.float32),
            f"adab{l}": np.ascontiguousarray(ada_b[l], np.float32).reshape(1, -1),
            f"rows{l}": rows, f"logits{l}": np.ascontiguousarray(hgrn_lb_logits, np.float32).reshape(1, -1),
            f"wgk{l}": wgk, f"convw{l}": convw}


def core_inputs(xc, cc, layer_maps):
    nseq = xc.shape[0]
    m = {"x": np.ascontiguousarray(xc.reshape(-1, D), np.float32),
         "cT": np.ascontiguousarray(np.asarray(cc, np.float32).reshape(nseq, 8, 128).transpose(2, 1, 0)).reshape(128, 8 * nseq),
         "consts": make_consts()}
    for lm in layer_maps:
        m.update(lm)
    return m


_NC_CACHE = {}


def kernel(x, c, w_in, w_out, ada_w, ada_b, ln_g, ln_b, hgrn_lb_logits, gla_w_gk, gla_b_gk,
           gdn_conv_w, gdn_a_log, gdn_dt_bias, gain_a, gain_b, gain_c):
    x = np.asarray(x, np.float32)
    c = np.asarray(c, np.float32)
    params = [np.asarray(a, np.float32) for a in (w_in, w_out, ada_w, ada_b, ln_g, ln_b, hgrn_lb_logits, gla_w_gk, gla_b_gk,
                                                  gdn_conv_w, gdn_a_log, gdn_dt_bias, gain_a, gain_b, gain_c)]
    nseq = BATCH // NCORES
    ntile = SEQ // 128
    layer_maps = [prep_layer_inputs(l, *params) for l in range(DEPTH)]
    key = ("full",)
    if key not in _NC_CACHE:
        _NC_CACHE[key] = build(nseq, ntile, list(range(DEPTH)))
    nc = _NC_CACHE[key]
    in_maps = [core_inputs(x[i * nseq:(i + 1) * nseq], c[i * nseq:(i + 1) * nseq], layer_maps) for i in range(NCORES)]
    res = run_bass_kernel_spmd(nc, in_maps, core_ids=list(range(NCORES)))
    out = np.concatenate([r["out"].reshape(nseq, SEQ, D) for r in res.results], axis=0)
    return out.astype(np.float32)
```

```python
import os
import numpy as np
import concourse.bass as bass
import concourse.mybir as mybir
from concourse.bass_utils import run_bass_kernel_spmd

F32 = mybir.dt.float32
BF16 = mybir.dt.bfloat16
AF = mybir.ActivationFunctionType
ALU = mybir.AluOpType
AX = mybir.AxisListType

D = 1024
SEQ = 4096
BATCH = 16
DEPTH = 2
NCORES = 8
D_IN = 3740
LN_EPS = 1e-5
NORM_EPS = 1e-6
ALPHA = (2 * DEPTH) ** 0.25

OFF_QA, OFF_FA = 0, 256
OFF_QB, OFF_KB = 512, 768
OFF_IA, OFF_AB = 1024, 1280
OFF_VB = 1292
OFF_Z = 1676
OFF_LR = 2700
OFF_C = 2716
NCOLS = OFF_C + 1152 + 4
TM_BANKS = [(0, 512), (512, 512), (1024, 268), (1292, 384), (1676, 512), (2188, 512)]

C_ID, C_MC, C_CST, C_BD32, C_C32, C_LBLK, C_UBLK, C_UBT, C_ONES, C_CIND, C_BONES, C_SEL, C_INVDV, C_BD64S = (
    0, 128, 256, 272, 400, 464, 592, 720, 848, 976, 980, 1108, 1364, 1380)
NCONST = 1508
R_GAIN, R_LNG, R_LNB, R_DT, R_ALOG = 0, 1024, 2048, 3072, 3078
NROW = 3 * D + 16


def make_consts():
    c = np.zeros((128, NCONST), np.float32)
    p = np.arange(128)
    ch = p // 64
    loc = p % 64
    c[:, C_ID:C_ID + 128] = np.eye(128)
    same = ch[:, None] == ch[None, :]
    le = loc[:, None] <= loc[None, :]
    ch32 = p // 32
    loc32 = p % 32
    same32 = ch32[:, None] == ch32[None, :]
    le32 = loc32[:, None] <= loc32[None, :]
    c[:, C_MC:C_MC + 128] = same32 * (le32.astype(np.float32) - (loc32[:, None] <= 15).astype(np.float32))
    for cc in range(4):
        inc = (ch32 == cc)
        c[:, C_CST + 3 * cc + 0] = inc * (loc32 <= 15)
        c[:, C_CST + 3 * cc + 1] = inc
        c[:, C_CST + 3 * cc + 2] = inc * (loc32 > 15)
    c[:, C_BD32:C_BD32 + 128] = same32 * le32
    for cc in range(4):
        c[:, C_C32 + cc] = (ch32 == cc)
    for cc in range(2):
        c[:, C_CIND + cc] = (ch == cc)
    c[:, C_BD64S:C_BD64S + 128] = same * (loc[:, None] < loc[None, :])
    c[:, C_LBLK:C_LBLK + 128] = same * le
    c[:, C_UBLK:C_UBLK + 128] = same * (loc[:, None] > loc[None, :])
    c[:, C_UBT:C_UBT + 128] = same * (loc[:, None] > loc[None, :])
    c[:, C_ONES:C_ONES + 128] = 1.0
    c[:, C_BONES:C_BONES + 128] = same
    for b in range(2):
        c[b, C_SEL + 128 * b:C_SEL + 128 * (b + 1)] = 1.0
    c[:, C_INVDV:C_INVDV + 4] = 1.0 / 64
    c[:, C_INVDV + 4:C_INVDV + 8] = 1.0 / 96
    c[:, C_INVDV + 8:C_INVDV + 14] = 1.0 / 64
    return c


class Buf:
    __slots__ = ("name", "ws", "rs")

    def __init__(self, name):
        self.name = name
        self.ws = {}
        self.rs = {}


class Sched:
    ENG = ("pe", "act", "dve", "pool", "sp")

    def __init__(self, nc):
        self.nc = nc
        self.sems = {}
        self.count = {}
        self.prog = {e: [] for e in self.ENG}
        self.seen = {e: {} for e in self.ENG}
        for e in self.ENG:
            self._sem(e)

    def _sem(self, key):
        if key not in self.sems:
            self.sems[key] = self.nc.alloc_semaphore(name="s_" + key)
            self.count[key] = 0
        return self.sems[key]

    def _deps(self, eng, reads, writes, part):
        deps = {}

        def add(k, v):
            if k == eng and eng == "pe":
                return
            if deps.get(k, 0) < v:
                deps[k] = v
        for b in reads:
            for k, v in b.ws.items():
                add(k, v)
        for b in writes:
            for k, v in b.rs.items():
                add(k, v)
            for k, v in b.ws.items():
                if part and k == eng:
                    continue
                add(k, v)
        waits = []
        sn = self.seen[eng]
        for k, v in deps.items():
            if sn.get(k, 0) < v:
                sn[k] = v
                waits.append((self.sems[k], v))
        return waits

    def _update(self, key, val, reads, writes, part):
        for b in reads:
            if b.rs.get(key, 0) < val:
                b.rs[key] = val
        for b in writes:
            if part and not b.rs:
                b.ws[key] = val
            else:
                b.ws = {key: val}
            b.rs = {}

    def op(self, eng, fn, reads=(), writes=(), part=False):
        waits = self._deps(eng, reads, writes, part)
        self.count[eng] += 1
        n = self.count[eng]
        self.prog[eng].append((waits, fn, self.sems[eng], 1))
        self._update(eng, n, reads, writes, part)

    def dma(self, queue, key, fn, reads=(), writes=()):
        self._sem(key)
        waits = self._deps(queue, reads, writes, False)
        self.count[key] += 16
        n = self.count[key]
        self.prog[queue].append((waits, fn, self.sems[key], 16))
        self._update(key, n, reads, writes, False)

    def final_wait(self, eng, keys):
        waits = [(self.sems[k], self.count[k]) for k in keys if self.count[k] > 0]
        self.prog[eng].append((waits, None, None, 0))

    def emit(self, block):
        nc = self.nc
        prog = self.prog

        def run(e, lst):
            for waits, fn, sem, inc in lst:
                for s, v in waits:
                    e.wait_ge(s, v)
                if fn is not None:
                    fn(e).then_inc(sem, inc)

        @block.tensor
        def _(e):
            run(e, prog["pe"])

        @block.scalar
        def _(e):
            run(e, prog["act"])

        @block.vector
        def _(e):
            run(e, prog["dve"])

        @block.gpsimd
        def _(e):
            run(e, prog["pool"])

        @block.sync
        def _(e):
            run(e, prog["sp"])


def build(nseq, ntile, layers, first_in_x=True, debug=False):
    nc = bass.Bass("TRN2", target_bir_lowering=False)
    NTOK = nseq * ntile * 128
    S = Sched(nc)
    T = 128

    def dram_in(name, shape):
        return nc.dram_tensor(name, list(shape), F32, kind="ExternalInput").ap()

    x_d = dram_in("x", [NTOK, D])
    cT_d = dram_in("cT", [128, 8 * nseq])
    consts_d = dram_in("consts", [128, NCONST])
    out_d = nc.dram_tensor("out", [NTOK, D], F32, kind="ExternalOutput").ap()
    L = {}
    for l in layers:
        L[l] = dict(
            win=dram_in(f"win{l}", [D, NCOLS]), wout=dram_in(f"wout{l}", [D, D]),
            adaw=dram_in(f"adaw{l}", [D, 3 * D]), adab=dram_in(f"adab{l}", [1, 3 * D]),
            rows=dram_in(f"rows{l}", [1, NROW]), logits=dram_in(f"logits{l}", [1, 256 * DEPTH]), wgk=dram_in(f"wgk{l}", [17, 256]),
            convw=dram_in(f"convw{l}", [128, 36]))
    scratch = None
    if len(layers) > 1:
        scratch = nc.dram_tensor("xmid", [NTOK, D], F32, kind="Internal").ap()

    _cnt = [0]

    def sb(shape, dt=F32, name=None):
        _cnt[0] += 1
        nm = (name or "t") + str(_cnt[0])
        return nc.alloc_sbuf_tensor(nm, list(shape), dt), Buf(nm)

    consts, b_consts = sb([128, NCONST], name="consts")
    constb, b_constb = sb([128, 128 + 128 + 64], BF16, name="constb")
    win, b_win = sb([128, 8, NCOLS], BF16, name="win")
    wout, b_wout = sb([128, 8, D], BF16, name="wout")
    rows, b_rows = sb([128, 3 * D + 16], name="rows")
    wgk, b_wgk = sb([17, 256], name="wgk")
    convw, b_convw = sb([128, 9, 4], name="convw")
    cact, b_cact = sb([128, 8 * nseq], name="cact")
    shiftT, b_shiftT = sb([128, 8, nseq], name="shiftT")
    scaleT, b_scaleT = sb([128, 8, nseq], name="scaleT")
    gateb = [sb([128, D], name="gateb") for _ in range(nseq)]
    lbt, b_lbt = sb([128, 256], name="lb")
    omlb, b_omlb = sb([128, 256], name="omlb")
    negA, b_negA = sb([128, 6], name="negA")
    lrT, b_lrT = sb([17, 128], name="lrT")
    S_ab, b_S_ab = sb([128, 320], name="S_ab")
    S_c, b_S_c = sb([128, 3, 64], name="S_c")
    cpre, b_cpre = sb([128, 9, 131], name="cpre")

    ident = consts[:, C_ID:C_ID + 128]
    identb = constb[:, 0:128]
    bonesb = constb[:, 128:256]

    PSB = []
    for i in range(8):
        PSB.append((nc.alloc_psum_tensor(f"ps{i}", [128, 512], F32), Buf(f"ps{i}")))
    _psi = [0]

    def ps():
        r = PSB[_psi[0] % 6]
        _psi[0] += 1
        return r

    epsc, b_epsc = sb([128, 4], name="epsc")

    def act(out, in_, func, reads, writes, bias=None, scale=None, part=False):
        kw = {}
        if bias is not None:
            kw["bias"] = bias
            if not isinstance(bias, (int, float)):
                reads = tuple(reads) + (b_epsc,)
        if scale is not None:
            kw["scale"] = scale
        S.op("act", lambda e: e.activation(out=out, in_=in_, func=func, **kw), reads, writes, part)

    def tt(eng, out, in0, in1, op, reads, writes, part=False):
        S.op(eng, lambda e: e.tensor_tensor(out=out, in0=in0, in1=in1, op=op), reads, writes, part)

    def ts(eng, out, in0, s1, s2, op0, op1, reads, writes, part=False):
        if s2 is None:
            S.op(eng, lambda e: e.tensor_scalar(out=out, in0=in0, scalar1=s1, scalar2=None, op0=op0), reads, writes, part)
        else:
            S.op(eng, lambda e: e.tensor_scalar(out=out, in0=in0, scalar1=s1, scalar2=s2, op0=op0, op1=op1), reads, writes, part)

    def stt(out, in0, scalar, in1, op0, op1, reads, writes, part=False):
        S.op("dve", lambda e: e.scalar_tensor_tensor(out=out, in0=in0, scalar=scalar, in1=in1, op0=op0, op1=op1),
             reads, writes, part)

    def cp(eng, out, in_, reads, writes, part=False):
        if eng == "act":
            act(out, in_, AF.Copy, reads, writes, part=part)
        else:
            S.op(eng, lambda e: e.tensor_copy(out=out, in_=in_), reads, writes, part)

    NOTP = os.environ.get("K_NOTP") == "1"

    def mm(out, lhsT, rhs, start, stop, reads, writes, tp=None, skip=False):
        kw = {}
        if tp is not None and not NOTP:
            kw["tile_position"] = tp
        if skip:
            kw["skip_group_check"] = True
        S.op("pe", lambda e: e.matmul(out, lhsT, rhs, start=start, stop=stop, **kw), reads, writes, True)

    def tr(out, in_, idn, reads, writes, tp=None):
        if tp is None or NOTP:
            S.op("pe", lambda e: e.transpose(out, in_, idn), reads, writes, True)
        else:
            S.op("pe", lambda e: e.transpose(out, in_, idn, tile_position=tp), reads, writes, True)

    def bc(ap, shape, axis):
        return ap.unsqueeze(axis).broadcast_to(list(shape))

    def rstd_from(var_ap, eps, tmp_ap, out_ap, reads, bufs):
        act(tmp_ap, var_ap, AF.Ln, reads, bufs, bias=eps_ap(eps, var_ap))
        act(out_ap, tmp_ap, AF.Exp, bufs, bufs, scale=-0.5)

    def eps_ap(eps, like):
        npart = like.shape[0]
        base = like.base_partition()
        col = {LN_EPS: 0, NORM_EPS: 1, 1.0: 2}[eps]
        return epsc[base:base + npart, col:col + 1]

    S.dma("sp", "cst", lambda e: e.dma_start(out=consts[:, :], in_=consts_d[:, :]), (), (b_consts,))
    S.dma("sp", "cT", lambda e: e.dma_start(out=cact[:, :], in_=cT_d[:, :]), (), (b_cact,))
    S.op("pool", lambda e: e.memset(epsc[:, 0:1], LN_EPS), (), (b_epsc,))
    S.op("pool", lambda e: e.memset(epsc[:, 1:2], NORM_EPS), (b_epsc,), (b_epsc,))
    S.op("pool", lambda e: e.memset(epsc[:, 2:3], 1.0), (b_epsc,), (b_epsc,))
    cp("dve", identb, ident, (b_consts,), (b_constb,))
    cp("dve", bonesb, consts[:, C_BONES:C_BONES + 128], (b_consts, b_constb), (b_constb,))
    act(cact[:, :], cact[:, :], AF.Silu, (b_cact,), (b_cact,))
    S.op("pool", lambda e: e.memset(lrT[:, :], 1.0), (), (b_lrT,))

    xb = [sb([128, D], name="x") for _ in range(2)]
    st6, b_st = sb([128, 12], name="st")
    mv, b_mv = sb([128, 8], name="mv")
    hT, b_hT = sb([128, 8, 128], BF16, name="hT")
    qAs, b_qAs = sb([128, 256], name="qAs")
    kA, b_kA = sb([128, 256], name="kA")
    qkB, b_qkB = sb([128, 512], name="qkB")
    vAB, b_vAB = sb([128, 640], BF16, name="vAB")
    abC, b_abC = sb([128, 12], name="abC")
    zsb = [sb([128, D], name="zs") for _ in range(2)]
    zs, b_zs = zsb[0]
    modrow, b_modrow = zs[0:nseq, 0:512], b_zs
    adab, b_adab = zs[0:1, 512:1024], b_zs
    gAB, b_gAB = sb([128, 512], name="gAB")
    fA, b_fA = gAB[:, 0:256], b_gAB
    epm, b_epm = sb([128, 1024], name="epm")
    ep, b_ep = epm[:, 0:512], b_epm
    em, b_em = epm[:, 512:1024], b_epm
    e1, b_e1 = epm[:, 512:768], b_epm
    utmp, b_utmp = epm[:, 0:320], b_epm
    Sb, b_Sb = epm[:, 512:672].bitcast(BF16), b_epm
    fac, b_fac = sb([128, 4, 12], name="fac")
    qt, b_qt = sb([128, 512], BF16, name="qt")
    kt, b_kt = sb([128, 512], BF16, name="kt")
    qtTm = [sb([128, 4, 128], BF16, name="qtTm") for _ in range(2)]
    ktTm = [sb([128, 4, 128], BF16, name="ktTm") for _ in range(2)]
    bigb, _b = sb([128, 3072], BF16, name="bigb")
    b_bigb = (Buf("M0h"), Buf("M1h"))
    vbd, b_vbd = sb([128, 4, 640], BF16, name="vbd")
    zerob, b_zerob = sb([128, 128], BF16, name="zerob")
    scT, b_scT = sb([128, 8, 128], BF16, name="scT")
    sqr, b_sqr = sb([128, 6, 128], BF16, name="sqr")
    b_csv = Buf("csv")
    cs, b_cs = sb([128, 9, 128], name="cs")
    qnTm = [sb([128, 3, 128], BF16, name="qnTm") for _ in range(2)]
    knTm = [sb([128, 3, 128], BF16, name="knTm") for _ in range(2)]
    gC, b_gC = sb([128, 6], name="gC")
    gtmp, b_gtmp = sb([128, 4, 6], name="gtmp")
    beta, b_beta = sb([128, 6], name="beta")
    nbeta, b_nbeta = sb([128, 6], name="nbeta")
    eb, b_eb = sb([128, 6], name="eb")
    elb, b_elb = sb([128, 6], name="elb")
    gci, b_gci = sb([128, 6, 2], name="gci")
    eblr, b_eblr = sb([128, 6, 2], name="eblr")
    DTf, _b = sb([128, 6, 128], name="DTf")
    B_DTf = (Buf("DTf0"), Buf("DTf1"))
    Mb = [(bigb[:, 768 * i:768 * (i + 1)].rearrange("p (a b) -> p a b", b=128), b_bigb) for i in range(2)]
    MTb = [(bigb[:, 768 * (2 + i):768 * (3 + i)].rearrange("p (a b) -> p a b", b=128), b_bigb) for i in range(2)]
    attnT, _b = sb([128, 6, 128], BF16, name="attnT")
    B_attnT = (Buf("attnT0"), Buf("attnT1"))
    Xbf, _b = sb([128, 6, 128], BF16, name="Xbf")
    B_Xbf = (Buf("Xbf0"), Buf("Xbf1"))
    Xlo, _b = sb([128, 6, 128], BF16, name="Xlo")
    B_Xlo = (Buf("Xlo0"), Buf("Xlo1"))
    X32, _b = sb([128, 6, 128], name="X32")
    B_X32 = (Buf("X320"), Buf("X321"))
    kupdm = [sb([128, 6, 64], BF16, name="kupdm") for _ in range(2)]
    XwTm = [sb([128, 3, 128], BF16, name="XwTm") for _ in range(2)]
    Scb, b_Scb = sb([128, 3, 64], BF16, name="Scb")
    dtmp = DTf[:, :, 0:64]
    vnew, b_vnew = sb([128, 6, 64], BF16, name="vnew")
    ss, b_ss = sb([128, 16], name="ss")
    rs, b_rs = sb([128, 16], name="rs")
    y1, b_y1 = sb([128, D], name="y1")
    yb, b_yb = sb([128, D], BF16, name="yb")
    yT, b_yT = sb([128, 8, 128], BF16, name="yT")
    st6b, b_stb = sb([128, 12], name="stb")
    mvb, b_mvb = sb([128, 8], name="mvb")
    t1, b_t1 = sb([128, D], name="t1")
    xh2b = [sb([128, D], name="xh2")] * 2
    xhat, b_xhat = sb([128, D], name="xhat")
    y2, b_y2 = y1, b_y1
    sq, b_sq = t1, b_t1
    res, b_res = t1, b_t1
    oAB, b_oAB = y1[:, 0:640], b_y1
    oC, b_oC = y1[:, 640:1024].rearrange("p (a b) -> p a b", b=64), b_y1
    gLf, b_gLf = cs[:, 0:6, :], b_cs
    rinv = DTf


    for (t_, b_) in qtTm + ktTm + qnTm + knTm + XwTm + kupdm + [(vnew, b_vnew), (zerob, b_zerob)]:
        S.op("pool", (lambda t_: lambda e: e.memset(t_[:], 0.0))(t_), (), (b_,))

    dbg = {}
    STOP = float(os.environ.get("K_STOP", "99"))

    def early_out(xt, b_xt, r0, dst_d, li, slot):
        S.dma("sp", f"st{li}_{slot}", lambda e: e.dma_start(out=dst_d[r0:r0 + 128, :], in_=xt[:, :]), (b_xt,), ())

    def D_(name, ap, buf, dt=F32):
        if not debug or name in dbg:
            return
        shp = list(ap.shape)
        d = nc.dram_tensor("dbg_" + name, shp, dt, kind="ExternalOutput").ap()
        dbg[name] = d
        S.dma("sp", "dbg_" + name, lambda e: e.dma_start(out=d, in_=ap), (buf,), ())

    pending_tail = [None]

    def run_gens(gens):
        gens = list(gens)
        while gens:
            for g_ in list(gens):
                try:
                    next(g_)
                except StopIteration:
                    gens.remove(g_)

    def layer(l, src_d, dst_d, li):
        W = L[l]
        if STOP <= 0:
            for ti in range(nseq * ntile):
                xt, b_xt = xb[ti % 2]
                r0 = ti * 128
                S.dma("sp", f"xl{li}_{ti % 2}", (lambda xt, r0: lambda e: e.dma_start(out=xt[:, :], in_=src_d[r0:r0 + 128, :]))(xt, r0), (), (b_xt,))
                early_out(xt, b_xt, r0, dst_d, li, ti % 2)
            return
        winr = W["win"].rearrange("(kc p) n -> p kc n", p=128)
        for kc in range(8):
            S.dma("pool", f"win{kc}", (lambda kc: lambda e: e.dma_start(
                out=win[:, kc, :], in_=winr[:, kc, :], max_dma_last_dim=2048))(kc), (), (b_win,))
        woutr = W["wout"].rearrange("(kc p) n -> p kc n", p=128)
        for kc in range(8):
            S.dma("pool", f"wout{kc}", (lambda kc: lambda e: e.dma_start(
                out=wout[:, kc, :], in_=woutr[:, kc, :], max_dma_last_dim=2048))(kc), (), (b_wout,))
        rows_src = bass.AP(W["rows"].tensor, 0, [[0, 128], [1, NROW]])
        S.dma("sp", "rows", lambda e: e.dma_start(out=rows[:, :], in_=rows_src), (), (b_rows,))
        S.dma("sp", "wgk", lambda e: e.dma_start(out=wgk[:, :], in_=W["wgk"][:, :]), (), (b_wgk,))
        S.dma("sp", "convw", lambda e: e.dma_start(out=convw[:, :, :].rearrange("p a b -> p (a b)"), in_=W["convw"][:, :]), (), (b_convw,))
        adawr = W["adaw"].rearrange("(kc p) n -> p kc n", p=128)
        big4 = [xb[0], xb[1], xh2b[0], (xhat, b_xhat)]
        for nchunk in range(6):
            c0 = nchunk * 512
            S.dma("sp", "adab", (lambda c0: lambda e: e.dma_start(out=adab[:, :], in_=W["adab"][:, c0:c0 + 512]))(c0), (), (b_adab,))
            for j4 in range(4):
                bt, b_bt = big4[j4]
                S.dma("sp", f"adaw{j4}", (lambda c0, j4, bt: lambda e: e.dma_start(
                    out=bt[:, :].rearrange("p (a b) -> p a b", b=512), in_=adawr[:, 2 * j4:2 * j4 + 2, c0:c0 + 512]))(c0, j4, bt),
                    (), (b_bt,))
            pt, b_pt = ps()
            for kc in range(8):
                bt, b_bt = big4[kc // 2]
                mm(pt[0:nseq, :], cact[:, kc * nseq:(kc + 1) * nseq], bt[:, (kc % 2) * 512:(kc % 2 + 1) * 512], kc == 0, False,
                   (b_cact, b_bt), (b_pt,))
            mm(pt[0:nseq, :], consts[0:1, C_ONES:C_ONES + nseq], adab[0:1, :], False, True, (b_consts, b_adab), (b_pt,))
            cp("dve", modrow[:, :], pt[0:nseq, :], (b_pt,), (b_modrow,))
            if nchunk < 4:
                p2_, b_p2_ = ps()
                for j in range(4):
                    tr(p2_[:, j * nseq:(j + 1) * nseq], modrow[0:nseq, j * 128:(j + 1) * 128], ident[0:nseq, 0:nseq],
                       (b_modrow, b_consts), (b_p2_,))
                if nchunk < 2:
                    cp("dve", shiftT[:, nchunk * 4:nchunk * 4 + 4, :].rearrange("p a b -> p (a b)"), p2_[:, 0:4 * nseq], (b_p2_,), (b_shiftT,))
                else:
                    ts("dve", scaleT[:, (nchunk - 2) * 4:(nchunk - 2) * 4 + 4, :].rearrange("p a b -> p (a b)"), p2_[:, 0:4 * nseq],
                       1.0, None, ALU.add, None, (b_p2_,), (b_scaleT,))
            else:
                half = nchunk - 4
                for b in range(nseq):
                    g_t, b_g = gateb[b]
                    p2_, b_p2_ = ps()
                    mm(p2_[:, :], consts[0:nseq, C_SEL + 128 * b:C_SEL + 128 * (b + 1)], modrow[0:nseq, :], True, True,
                       (b_consts, b_modrow), (b_p2_,))
                    cp("dve", g_t[:, half * 512:(half + 1) * 512], p2_[:, :], (b_p2_,), (b_g,))
        lbtmp = y1[:, :].rearrange("p (a b) -> p a b", b=256)
        b_lbtmp = b_y1
        lg_src = bass.AP(W["logits"].tensor, 0, [[0, 128], [1, 256 * DEPTH]])
        S.dma("sp", "logits", lambda e: e.dma_start(out=y1[:, 0:256 * DEPTH], in_=lg_src), (), (b_lbtmp,))
        act(y1[:, 0:256 * DEPTH], y1[:, 0:256 * DEPTH], AF.Exp, (b_lbtmp,), (b_lbtmp,))
        den = lbtmp[:, DEPTH, :]
        cp("dve", den, lbtmp[:, 0, :], (b_lbtmp,), (b_lbtmp,))
        for j in range(1, DEPTH):
            tt("dve", den, den, lbtmp[:, j, :], ALU.add, (b_lbtmp,), (b_lbtmp,))
        S.op("dve", lambda e: e.reciprocal(out=den, in_=den), (b_lbtmp,), (b_lbtmp,))
        acc = lbtmp[:, DEPTH + 1, :]
        S.op("dve", lambda e: e.memset(acc, 0.0), (b_lbtmp,), (b_lbtmp,))
        for j in range(1, l + 1):
            tt("dve", acc, acc, lbtmp[:, j, :], ALU.add, (b_lbtmp,), (b_lbtmp,))
        tt("dve", lbt[:, :], acc, den, ALU.mult, (b_lbtmp,), (b_lbt,))
        ts("dve", omlb[:, :], lbt[:, :], -1.0, 1.0, ALU.mult, ALU.add, (b_lbt,), (b_omlb,))
        act(negA[:, :], rows[:, R_ALOG:R_ALOG + 6], AF.Exp, (b_rows,), (b_negA,))
        ts("dve", negA[:, :], negA[:, :], -1.0, None, ALU.mult, None, (b_negA,), (b_negA,))

        gain_b = rows[:, R_GAIN:R_GAIN + D]
        lng_b = rows[:, R_LNG:R_LNG + D]
        lnb_b = rows[:, R_LNB:R_LNB + D]
        dtb = rows[:, R_DT:R_DT + 6]

        segs = [(0, 64), (64, 64), (128, 96), (224, 96)]

        def head_cols(hh):
            if hh < 4:
                return hh * 64, 64
            return 256 + (hh - 4) * 96, 96

        def state_cols(hh):
            ct = hh // 2
            off, dv = segs[ct]
            return off, dv

        def head_gen(b, xt, b_xt, zs, b_zs, r0, slot):
            S.dma("sp", f"xl{li}_{slot}", lambda e: e.dma_start(out=xt[:, :], in_=src_d[r0:r0 + 128, :]), (), (b_xt,))
            S.op("dve", lambda e, xt=xt: e.bn_stats(out=st6[:, 0:6], in_=xt[:, 0:512]), (b_xt,), (b_st,))
            S.op("dve", lambda e, xt=xt: e.bn_stats(out=st6[:, 6:12], in_=xt[:, 512:1024]), (b_xt,), (b_st,), part=True)
            S.op("dve", lambda e: e.bn_aggr(out=mv[:, 0:2], in_=st6[:, 0:12]), (b_st,), (b_mv,))
            rstd_from(mv[:, 1:2], LN_EPS, mv[:, 2:3], mv[:, 3:4], (b_mv,), (b_mv,))
            ts("dve", mv[:, 4:5], mv[:, 0:1], -1.0, mv[:, 3:4], ALU.mult, ALU.mult, (b_mv,), (b_mv,))
            act(xhat[:, :], xt[:, :], AF.Identity, (b_xt, b_mv), (b_xhat,), bias=mv[:, 4:5], scale=mv[:, 3:4])
            yield
            for half in range(2):
                if half == 1:
                    yield
                pt, b_pt = ps()
                for j in range(4):
                    kc = half * 4 + j
                    tr(pt[:, j * 128:(j + 1) * 128], xhat[:, kc * 128:(kc + 1) * 128], ident, (b_xhat, b_consts), (b_pt,))
                for j in range(4):
                    kc = half * 4 + j
                    act(hT[:, kc, :], pt[:, j * 128:(j + 1) * 128], AF.Identity, (b_pt, b_shiftT, b_scaleT), (b_hT,),
                        bias=shiftT[:, kc, b:b + 1], scale=scaleT[:, kc, b:b + 1], part=True)
            yield
            pbank = []
            for (off, n) in TM_BANKS:
                pt, b_pt = ps()
                for kc in range(8):
                    mm(pt[:, 0:n], hT[:, kc, :], win[:, kc, off:off + n], kc == 0, kc == 7, (b_hT, b_win), (b_pt,))
                pbank.append((pt, b_pt))
            p0, b_p0 = pbank[0]
            act(qAs[:, :], p0[:, 0:256], AF.Silu, (b_p0,), (b_qAs,))
            act(fA[:, :], p0[:, 256:512], AF.Sigmoid, (b_p0,), (b_fA,))
            tt("dve", fA[:, :], fA[:, :], omlb[:, :], ALU.mult, (b_fA, b_omlb), (b_fA,))
            tt("dve", fA[:, :], fA[:, :], lbt[:, :], ALU.add, (b_fA, b_lbt), (b_fA,))
            ts("dve", fA[:, :], fA[:, :], 1e-30, None, ALU.max, None, (b_fA,), (b_fA,))
            ts("dve", kA[:, :], fA[:, :], -1.0, 1.0, ALU.mult, ALU.add, (b_fA,), (b_kA,))
            p1, b_p1 = pbank[1]
            cp("act", qkB[:, :], p1[:, :], (b_p1,), (b_qkB,))
            p2, b_p2 = pbank[2]
            cp("act", vAB[:, 0:256], p2[:, 0:256], (b_p2,), (b_vAB,))
            cp("dve", abC[:, :], p2[:, 256:268], (b_p2,), (b_abC,))
            p3, b_p3 = pbank[3]
            cp("act", vAB[:, 256:640], p3[:, 0:384], (b_p3,), (b_vAB,), part=True)
            p4, b_p4 = pbank[4]
            p5, b_p5 = pbank[5]
            act(zs[:, 0:512], p4[:, :], AF.Silu, (b_p4,), (b_zs,))
            act(zs[:, 512:1024], p5[:, :], AF.Silu, (b_p5,), (b_zs,), part=True)
            act(gAB[:, 0:256], fA[:, :], AF.Ln, (b_fA,), (b_gAB,))
            tt("pool", zs[:, :], zs[:, :], gain_b, ALU.mult, (b_zs, b_rows), (b_zs,))
            yield
            pt, b_pt = ps()
            for kc in range(8):
                mm(pt[0:16, 0:128], win[:, kc, OFF_LR:OFF_LR + 16], hT[:, kc, :], kc == 0, kc == 7, (b_hT, b_win), (b_pt,))
            cp("dve", lrT[0:16, :], pt[0:16, 0:128], (b_pt,), (b_lrT,))
            for grp in range(3):
                yield
                pt, b_pt = ps()
                ncts = 4 if grp < 2 else 1
                for j in range(ncts):
                    ct = grp * 4 + j
                    for kc in range(8):
                        mm(pt[:, j * 128:(j + 1) * 128], win[:, kc, OFF_C + ct * 128:OFF_C + (ct + 1) * 128], hT[:, kc, :],
                           kc == 0, kc == 7, (b_hT, b_win), (b_pt,))
                for j in range(ncts):
                    ct = grp * 4 + j
                    cp("act", cpre[:, ct, 3:131], pt[:, j * 128:(j + 1) * 128], (b_pt,), (b_cpre,), part=True)

        tiles = [(b_, it_) for b_ in range(nseq) for it_ in range(ntile)]

        def head_args(k):
            b_, it_ = tiles[k]
            sl_ = k % 2
            return (b_, xb[sl_][0], xb[sl_][1], zsb[sl_][0], zsb[sl_][1], k * 128, sl_)

        run_gens([head_gen(*head_args(0))])
        for b in range(nseq):
            S.op("pool", lambda e: e.memset(S_ab[:, :], 0.0), (), (b_S_ab,))
            S.op("pool", lambda e: e.memset(S_c[:, :, :], 0.0), (), (b_S_c,))
            S.op("pool", lambda e: e.memset(cpre[:, :, 0:3], 0.0), (), (b_cpre,))
            g_t, b_g = gateb[b]
            for it in range(ntile):
                ti = b * ntile + it
                slot = ti % 2
                xt, b_xt = xb[slot]
                xh2, b_xh2 = xh2b[slot]
                zs, b_zs = zsb[slot]
                ot, b_ot = xh2, b_xh2
                r0 = ti * 128
                def chain_ab():
                    pg, b_pg = ps()
                    mm(pg[:, 0:256], lrT[0:17, :], wgk[0:17, :], True, True, (b_lrT, b_wgk), (b_pg,))
                    act(e1, pg[:, 0:256], AF.Exp, (b_pg,), (b_e1,), scale=-1.0)
                    act(e1, e1, AF.Ln, (b_e1,), (b_e1,), bias=eps_ap(1.0, epm[:, 0:1]))
                    ts("dve", gAB[:, 256:512], e1, -1.0 / 16.0, None, ALU.mult, None, (b_e1,), (b_gAB,), part=True)
                    yield
                    pc, b_pc = ps()
                    mm(pc[:, :], consts[:, C_MC:C_MC + 128], gAB[:, :], True, True, (b_consts, b_gAB), (b_pc,))
                    pf, b_pf = ps()
                    for ct in range(4):
                        mm(pf[:, ct * 12:(ct + 1) * 12], gAB[:, ct * 128:(ct + 1) * 128], consts[:, C_CST:C_CST + 12], True, True,
                           (b_gAB, b_consts), (b_pf,))
                    act(fac[:, :, :].rearrange("p a b -> p (a b)"), pf[:, 0:48], AF.Exp, (b_pf,), (b_fac,))
                    act(ep[:, :], pc[:, :], AF.Exp, (b_pc,), (b_ep,))
                    act(em[:, :], pc[:, :], AF.Exp, (b_pc,), (b_em,), scale=-1.0)
                    tt("dve", qt[:, 0:256], qAs[:, :], ep[:, 0:256], ALU.mult, (b_qAs, b_ep), (b_qt,))
                    stt(qt[:, 256:512], qkB[:, 0:256], 48.0 ** -0.5, ep[:, 256:512], ALU.mult, ALU.mult, (b_qkB, b_ep), (b_qt,), part=True)
                    tt("dve", kt[:, 0:256], kA[:, :], em[:, 0:256], ALU.mult, (b_kA, b_em), (b_kt,))
                    tt("dve", kt[:, 256:512], qkB[:, 256:512], em[:, 256:512], ALU.mult, (b_qkB, b_em), (b_kt,), part=True)
                    yield
                    pT, b_pT = ps()
                    pTb = pT[:, :].bitcast(BF16)
                    for ct in range(4):
                        tr(pTb[:, ct * 128:(ct + 1) * 128], qt[:, ct * 128:(ct + 1) * 128], identb, (b_qt, b_constb), (b_pT,))
                    for ct in range(4):
                        tr(pTb[:, 512 + ct * 128:512 + (ct + 1) * 128], kt[:, ct * 128:(ct + 1) * 128], identb, (b_kt, b_constb), (b_pT,))
                    for par in range(2):
                        hs = slice(64 * par, 64 * par + 64)
                        qm, b_qm = qtTm[par]
                        km, b_km = ktTm[par]
                        cp("act", qm[hs, :, :].rearrange("p a b -> p (a b)"), pTb[hs, 0:512], (b_pT,), (b_qm,))
                        cp("act", km[hs, :, :].rearrange("p a b -> p (a b)"), pTb[hs, 512:1024], (b_pT,), (b_km,))
                    for c in range(4):
                        if c % 2 == 0:
                            act(vbd[:, c, :], vAB[:, :], AF.Copy, (b_vAB, b_consts), (b_vbd,), scale=consts[:, C_C32 + c:C_C32 + c + 1], part=(c > 0))
                        else:
                            ts("dve", vbd[:, c, :], vAB[:, :], consts[:, C_C32 + c:C_C32 + c + 1], None, ALU.mult, None,
                               (b_vAB, b_consts), (b_vbd,), part=True)
                    yield
                    psS = [ps(), ps()]
                    for hh in range(8):
                        ct, par = hh // 2, hh % 2
                        pt, b_pt = psS[hh // 4]
                        mm(pt[:, (hh % 4) * 128:(hh % 4 + 1) * 128], ktTm[par][0][:, ct, :], qtTm[par][0][:, ct, :], True, True,
                           (ktTm[par][1], qtTm[par][1]), (b_pt,))
                    for g4 in range(2):
                        pt, b_pt = psS[g4]
                        tt("dve", scT[:, 4 * g4:4 * g4 + 4, :], pt[:, :].rearrange("p (a b) -> p a b", b=128),
                           bc(consts[:, C_BD32:C_BD32 + 128], [128, 4, 128], 1), ALU.mult, (b_pt, b_consts), (b_scT,), part=(g4 > 0))
                    poA, b_poA = PSB[6]
                    poB, b_poB = PSB[7]
                    yield
                    mm(poA[:, 0:256], zerob[:, :], vAB[:, 0:256], True, False, (b_zerob, b_vAB), (b_poA,), skip=True)
                    mm(poB[:, 0:384], zerob[:, :], vAB[:, 256:640], True, False, (b_zerob, b_vAB), (b_poB,), skip=True)
                    for hh in range(8):
                        ocol, dv = head_cols(hh)
                        po, b_po = (poA, b_poA) if hh < 4 else (poB, b_poB)
                        oc = ocol if hh < 4 else ocol - 256
                        mm(po[:, oc:oc + dv], scT[:, hh, :], vAB[:, ocol:ocol + dv], False, False, (b_scT, b_vAB), (b_po,), skip=True)
                    yield "P"
                    for c in range(4):
                        r32 = slice(32 * c, 32 * c + 32)
                        for (c0_, c1_, dv_) in ((0, 2, 64), (2, 4, 96)):
                            o0 = segs[c0_][0]
                            w_ = 2 * dv_
                            tt("dve", Sb[:, o0:o0 + w_].rearrange("p (a b) -> p a b", b=dv_),
                               S_ab[:, o0:o0 + w_].rearrange("p (a b) -> p a b", b=dv_),
                               bc(fac[:, c0_:c1_, 3 * c], [128, 2, dv_], 2), ALU.mult, (b_S_ab, b_fac), (b_Sb,), part=(c0_ > 0))
                        for hh in range(8):
                            ct, par = hh // 2, hh % 2
                            ocol, dv = head_cols(hh)
                            soff, _ = state_cols(hh)
                            po, b_po = (poA, b_poA) if hh < 4 else (poB, b_poB)
                            oc = ocol if hh < 4 else ocol - 256
                            mm(po[r32, oc:oc + dv], qtTm[par][0][:, ct, r32], Sb[:, soff:soff + dv], False, (c == 3 and hh in (3, 7)),
                               (qtTm[par][1], b_Sb), (b_po,), tp=(0, 32 * c), skip=True)
                        yield
                        pu, b_pu = ps()
                        for hh in range(8):
                            ct, par = hh // 2, hh % 2
                            ocol, dv = head_cols(hh)
                            soff, _ = state_cols(hh)
                            mm(pu[par * 64:par * 64 + 64, soff:soff + dv], kt[:, hh * 64:(hh + 1) * 64], vbd[:, c, ocol:ocol + dv],
                               True, True, (b_kt, b_vbd), (b_pu,), tp=(0, par * 64))
                        for (c0_, c1_, dv_) in ((0, 2, 64), (2, 4, 96)):
                            o0 = segs[c0_][0]
                            w_ = 2 * dv_
                            tt("dve", utmp[:, o0:o0 + w_].rearrange("p (a b) -> p a b", b=dv_),
                               pu[:, o0:o0 + w_].rearrange("p (a b) -> p a b", b=dv_),
                               bc(fac[:, c0_:c1_, 3 * c + 2], [128, 2, dv_], 2), ALU.mult, (b_pu, b_fac), (b_utmp,), part=(c0_ > 0))
                        for (c0_, c1_, dv_) in ((0, 2, 64), (2, 4, 96)):
                            o0 = segs[c0_][0]
                            w_ = 2 * dv_
                            tt("dve", S_ab[:, o0:o0 + w_].rearrange("p (a b) -> p a b", b=dv_),
                               S_ab[:, o0:o0 + w_].rearrange("p (a b) -> p a b", b=dv_),
                               bc(fac[:, c0_:c1_, 3 * c + 1], [128, 2, dv_], 2), ALU.mult, (b_S_ab, b_fac), (b_S_ab,))
                        tt("dve", S_ab[:, :], S_ab[:, :], utmp[:, :], ALU.add, (b_S_ab, b_utmp), (b_S_ab,))
                        yield
                    cp("act", oAB[:, 0:256], poA[:, 0:256], (b_poA,), (b_oAB,))
                    cp("act", oAB[:, 256:640], poB[:, 0:384], (b_poB,), (b_oAB,), part=True)
                def chain_c():
                    for ct in range(9):
                        bq = b_cs if ct < 6 else b_csv
                        ts("dve", cs[:, ct, :], cpre[:, ct, 0:128], convw[:, ct, 0:1], None, ALU.mult, None, (b_cpre, b_convw), (bq,),
                           part=(ct % 6 > 0))
                        for j in range(1, 4):
                            stt(cs[:, ct, :], cpre[:, ct, j:j + 128], convw[:, ct, j:j + 1], cs[:, ct, :], ALU.mult, ALU.add,
                                (b_cpre, b_convw, bq), (bq,), part=True)
                        if ct == 5:
                            act(cs[:, 0:6, :].rearrange("p a b -> p (a b)"), cs[:, 0:6, :].rearrange("p a b -> p (a b)"), AF.Silu, (b_cs,), (b_cs,))
                        if ct == 8:
                            act(cs[:, 6:9, :].rearrange("p a b -> p (a b)"), cs[:, 6:9, :].rearrange("p a b -> p (a b)"), AF.Silu, (b_csv,), (b_csv,))
                        if ct % 3 == 2:
                            yield
                    cp("pool", cpre[:, :, 0:3], cpre[:, :, 128:131], (b_cpre,), (b_cpre,))
                    act(sqr[:, :, :].rearrange("p a b -> p (a b)"), cs[:, 0:6, :].rearrange("p a b -> p (a b)"), AF.Square, (b_cs,), (b_sqr,))
                    pn = [ps(), ps()]
                    for j in range(6):
                        pt, b_pt = pn[j // 4]
                        mm(pt[:, (j % 4) * 128:(j % 4 + 1) * 128], bonesb, sqr[:, j, :], True, True, (b_constb, b_sqr), (b_pt,))
                    for (pt, b_pt), lo, n in ((pn[0], 0, 4), (pn[1], 4, 2)):
                        dst = rinv[:, lo:lo + n, :].rearrange("p a b -> p (a b)")
                        act(dst, pt[:, 0:n * 128], AF.Ln, (b_pt,), (*B_DTf,), bias=eps_ap(NORM_EPS, pt[:, 0:1]), part=True)
                        act(dst, dst, AF.Exp, (*B_DTf,), (*B_DTf,), scale=-0.5)
                    yield
                    for par in range(2):
                        hs = slice(64 * par, 64 * par + 64)
                        qm, b_qm = qnTm[par]
                        stt(qm[hs, :, :], cs[hs, 0:3, :], 0.125, rinv[hs, 0:3, :], ALU.mult, ALU.mult, (b_cs, *B_DTf), (b_qm,))
                    tt("dve", rinv[:, 3:6, :], cs[:, 3:6, :], rinv[:, 3:6, :], ALU.mult, (b_cs, *B_DTf), (*B_DTf,))
                    for par in range(2):
                        hs = slice(64 * par, 64 * par + 64)
                        km, b_km = knTm[par]
                        cp("act", km[hs, :, :].rearrange("p a b -> p (a b)"), rinv[hs, 3:6, :].rearrange("p a b -> p (a b)"), (*B_DTf,), (b_km,))
                    yield
                    pk, b_pk = ps()
                    for j in range(3):
                        tr(pk[:, j * 128:(j + 1) * 128], rinv[:, 3 + j, :], ident, (*B_DTf, b_consts), (b_pk,))
                    pv, b_pv = ps()
                    for j in range(3):
                        tr(pv[:, j * 128:(j + 1) * 128], cs[:, 6 + j, :], ident, (b_csv, b_consts), (b_pv,))
                    cp("act", X32[:, :, 0:64], pv[:, 0:384].rearrange("p (a b) -> p a b", b=64), (b_pv,), (*B_X32,))
                    tt("dve", gtmp[:, 0, :], abC[:, 0:6], dtb, ALU.add, (b_abC, b_rows), (b_gtmp,))
                    act(gtmp[:, 1, :], gtmp[:, 0, :], AF.Exp, (b_gtmp,), (b_gtmp,))
                    act(gtmp[:, 2, :], gtmp[:, 1, :], AF.Ln, (b_gtmp,), (b_gtmp,), bias=eps_ap(1.0, gtmp[:, 0, 0:1]))
                    tt("dve", gC[:, :], gtmp[:, 2, :], negA[:, :], ALU.mult, (b_gtmp, b_negA), (b_gC,))
                    act(beta[:, :], abC[:, 6:12], AF.Exp, (b_abC,), (b_beta,), scale=-1.0)
                    ts("dve", beta[:, :], beta[:, :], 1.0, None, ALU.add, None, (b_beta,), (b_beta,))
                    S.op("dve", lambda e: e.reciprocal(out=beta[:, :], in_=beta[:, :]), (b_beta,), (b_beta,))
                    ts("dve", nbeta[:, :], beta[:, :], -1.0, None, ALU.mult, None, (b_beta,), (b_nbeta,))
                    pb, b_pb = ps()
                    mm(pb[:, 0:6], consts[:, C_LBLK:C_LBLK + 128], gC[:, :], True, True, (b_consts, b_gC), (b_pb,))
                    mm(pb[:, 8:14], consts[:, C_UBLK:C_UBLK + 128], gC[:, :], True, True, (b_consts, b_gC), (b_pb,))
                    tt("dve", gci[:, :, :], bc(gC[:, :], [128, 6, 2], 2), bc(consts[:, C_CIND:C_CIND + 2], [128, 6, 2], 1), ALU.mult,
                       (b_gC, b_consts), (b_gci,))
                    mm(pb[:, 16:28], consts[:, C_ONES:C_ONES + 128], gci[:, :, :].rearrange("p a b -> p (a b)"), True, True,
                       (b_consts, b_gci), (b_pb,))
                    act(eb[:, :], pb[:, 0:6], AF.Exp, (b_pb,), (b_eb,))
                    act(elb[:, :], pb[:, 8:14], AF.Exp, (b_pb,), (b_elb,))
                    act(eblr[:, :, :].rearrange("p a b -> p (a b)"), pb[:, 16:28], AF.Exp, (b_pb,), (b_eblr,))
                    pk3 = pk[:, 0:384].rearrange("p (a b) -> p a b", b=64)
                    tt("dve", X32[:, :, 64:128], pk3, bc(eb[:, :], [128, 6, 64], 2), ALU.mult, (b_pk, b_eb), (*B_X32,), part=True)
                    for c in range(2):
                        rsl = slice(64 * c, 64 * c + 64)
                        kum, b_kum = kupdm[c]
                        tt("dve", kum[rsl, :, :], pk3[rsl, :, :], bc(elb[rsl, :], [64, 6, 64], 2), ALU.mult, (b_pk, b_elb), (b_kum,))
                    cp("act", Xbf[:, :, :].rearrange("p a b -> p (a b)"), X32[:, :, :].rearrange("p a b -> p (a b)"), (*B_X32,), (*B_Xbf,))
                    tt("dve", Xlo[:, :, :], X32[:, :, :], Xbf[:, :, :], ALU.subtract, (*B_X32, *B_Xbf), (*B_Xlo,))
                    yield
                    yield "P"
                    tt("dve", gLf, bc(consts[:, C_LBLK:C_LBLK + 128], [128, 6, 128], 1), bc(gC[:, :], [128, 6, 128], 2), ALU.mult,
                       (b_consts, b_gC), (b_gLf,))
                    prs = [ps(), ps()]
                    for g3 in range(2):
                        pt, b_pt = prs[g3]
                        mm(pt[:, 0:384], consts[:, C_UBT:C_UBT + 128], gLf[:, 3 * g3:3 * g3 + 3, :].rearrange("p a b -> p (a b)"), True, True,
                           (b_consts, b_gLf), (b_pt,))
                    for g3 in range(2):
                        pt, b_pt = prs[g3]
                        act(DTf[:, 3 * g3:3 * g3 + 3, :].rearrange("p a b -> p (a b)"), pt[:, 0:384], AF.Exp, (b_pt,), (*B_DTf,), part=(g3 > 0))
                    tt("dve", DTf[:, :, :], DTf[:, :, :], bc(consts[:, C_LBLK:C_LBLK + 128], [128, 6, 128], 1), ALU.mult, (*B_DTf, b_consts), (*B_DTf,))
                    yield
                    M0 = Mb[0][0]
                    MT0 = MTb[0][0]

                    def neumann(g):
                        sl = slice(3 * g, 3 * g + 3)
                        bX32, bXbf, bXlo, bM = B_X32[g], B_Xbf[g], B_Xlo[g], b_bigb[g]
                        bDT, bAT = B_DTf[g], B_attnT[g]
                        pkk_, b_pkk = ps()
                        pkq_, b_pkq = ps()
                        for j in range(3):
                            h = 3 * g + j
                            ct, par = h // 2, h % 2
                            km, b_km = knTm[par]
                            qm, b_qm = qnTm[par]
                            mm(pkk_[:, j * 128:(j + 1) * 128], km[:, ct, :], km[:, ct, :], True, True, (b_km,), (b_pkk,))
                            mm(pkq_[:, j * 128:(j + 1) * 128], km[:, ct, :], qm[:, ct, :], True, True, (b_km, b_qm), (b_pkq,))
                        tt("dve", attnT[:, sl, :], pkq_[:, 0:384].rearrange("p (a b) -> p a b", b=128), DTf[:, sl, :], ALU.mult,
                           (b_pkq, bDT), (bAT,))
                        tt("dve", DTf[:, sl, :], DTf[:, sl, :], bc(consts[:, C_BD64S:C_BD64S + 128], [128, 3, 128], 1), ALU.mult,
                           (bDT, b_consts), (bDT,))
                        tt("dve", DTf[:, sl, :], pkk_[:, 0:384].rearrange("p (a b) -> p a b", b=128), DTf[:, sl, :], ALU.mult,
                           (b_pkk, bDT), (bDT,))
                        tt("dve", M0[:, sl, :], DTf[:, sl, :], bc(nbeta[:, sl], [128, 3, 128], 2), ALU.mult, (bDT, b_nbeta), (bM,))
                        yield
                        pm, b_pm = ps()
                        pmb = pm[:, :].bitcast(BF16)
                        for j in range(3):
                            h = 3 * g + j
                            tr(pmb[:, j * 128:(j + 1) * 128], M0[:, h, :], identb, (bM, b_constb), (b_pm,))
                        cp("act", MT0[:, sl, :].rearrange("p a b -> p (a b)"), pmb[:, 0:384], (b_pm,), (bM,))
                        yield
                        for lv in range(6):
                            Mc_ = Mb[lv % 2][0]
                            MTc = MTb[lv % 2][0]
                            pt, b_pt = ps()
                            for j in range(3):
                                h = 3 * g + j
                                mm(pt[:, j * 128:(j + 1) * 128], Mc_[:, h, :], Xbf[:, h, :], True, False, (bM, bXbf), (b_pt,))
                                mm(pt[:, j * 128:(j + 1) * 128], Mc_[:, h, :], Xlo[:, h, :], False, True, (bM, bXlo), (b_pt,))
                            tt("dve", X32[:, sl, :], pt[:, 0:384].rearrange("p (a b) -> p a b", b=128), X32[:, sl, :], ALU.add,
                               (b_pt, bX32), (bX32,))
                            cp("dve", Xbf[:, sl, :].rearrange("p a b -> p (a b)"), X32[:, sl, :].rearrange("p a b -> p (a b)"), (bX32,), (bXbf,))
                            if lv < 5:
                                tt("dve", Xlo[:, sl, :], X32[:, sl, :], Xbf[:, sl, :], ALU.subtract, (bX32, bXbf), (bXlo,))
                            yield
                            if lv < 5:
                                Mn = Mb[(lv + 1) % 2][0]
                                MTn = MTb[(lv + 1) % 2][0]
                                pt1, b_pt1 = ps()
                                pt2, b_pt2 = ps()
                                for j in range(3):
                                    h = 3 * g + j
                                    mm(pt1[:, j * 128:(j + 1) * 128], MTc[:, h, :], Mc_[:, h, :], True, True, (bM,), (b_pt1,))
                                for j in range(3):
                                    h = 3 * g + j
                                    mm(pt2[:, j * 128:(j + 1) * 128], Mc_[:, h, :], MTc[:, h, :], True, True, (bM,), (b_pt2,))
                                cp("act", Mn[:, sl, :].rearrange("p a b -> p (a b)"), pt1[:, 0:384], (b_pt1,), (bM,))
                                cp("act", MTn[:, sl, :].rearrange("p a b -> p (a b)"), pt2[:, 0:384], (b_pt2,), (bM,))
                                yield
                    yield
                    subs = [neumann(0), neumann(1)]
                    while subs:
                        for g_ in list(subs):
                            try:
                                next(g_)
                            except StopIteration:
                                subs.remove(g_)
                        yield
                    Xf = Xbf
                    yield
                    pw, b_pw = ps()
                    pwb = pw[:, :].bitcast(BF16)
                    for h in range(6):
                        ct, par = h // 2, h % 2
                        if par == 0:
                            tr(pwb[0:64, ct * 128:(ct + 1) * 128], Xf[:, h, 64:128], identb, (*B_Xbf, b_constb), (b_pw,))
                        else:
                            tr(pwb[:, 384 + ct * 128:384 + (ct + 1) * 128], Xf[:, h, :], identb, (*B_Xbf, b_constb), (b_pw,))
                    cp("act", XwTm[0][0][0:64, :, :].rearrange("p a b -> p (a b)"), pwb[0:64, 0:384], (b_pw,), (XwTm[0][1],))
                    cp("act", XwTm[1][0][64:128, :, :].rearrange("p a b -> p (a b)"), pwb[64:128, 384:768], (b_pw,), (XwTm[1][1],))
                    yield
                    for c in range(2):
                        rsl = slice(64 * c, 64 * c + 64)
                        kum, b_kum = kupdm[c]
                        cp("dve", Scb[:, :, :], S_c[:, :, :], (b_S_c,), (b_Scb,))
                        pg0, b_pg0 = ps()
                        pg1, b_pg1 = ps()
                        for h in range(6):
                            ct, par = h // 2, h % 2
                            mm(pg0[rsl, h * 64:(h + 1) * 64], XwTm[par][0][:, ct, 64 * c:64 * c + 64], Scb[:, ct, :], True, True,
                               (XwTm[par][1], b_Scb), (b_pg0,), tp=(0, 64 * c))
                            mm(pg1[rsl, h * 64:(h + 1) * 64], qnTm[par][0][:, ct, 64 * c:64 * c + 64], Scb[:, ct, :], True, True,
                               (qnTm[par][1], b_Scb), (b_pg1,), tp=(0, 64 * c))
                        tt("dve", dtmp[rsl, :, :], X32[rsl, :, 0:64], pg0[rsl, 0:384].rearrange("p (a b) -> p a b", b=64), ALU.subtract,
                           (*B_X32, b_pg0), (*B_DTf,))
                        tt("dve", vnew[rsl, :, :], dtmp[rsl, :, :], bc(beta[rsl, :], [64, 6, 64], 2), ALU.mult, (*B_DTf, b_beta), (b_vnew,))
                        tt("dve", oC[rsl, :, :], pg1[rsl, 0:384].rearrange("p (a b) -> p a b", b=64), bc(eb[rsl, :], [64, 6, 64], 2), ALU.mult,
                           (b_pg1, b_eb), (b_oC,))
                        yield
                        pg2, b_pg2 = ps()
                        pg3, b_pg3 = ps()
                        for h in range(6):
                            ct, par = h // 2, h % 2
                            mm(pg2[rsl, h * 64:(h + 1) * 64], attnT[:, h, 64 * c:64 * c + 64], vnew[:, h, :], True, True, (*B_attnT, b_vnew), (b_pg2,),
                               tp=(0, 64 * c))
                            mm(pg3[par * 64:par * 64 + 64, ct * 64:(ct + 1) * 64], kum[:, h, :], vnew[:, h, :], True, True,
                               (b_kum, b_vnew), (b_pg3,), tp=(0, par * 64))
                        tt("dve", oC[rsl, :, :], oC[rsl, :, :], pg2[rsl, 0:384].rearrange("p (a b) -> p a b", b=64), ALU.add,
                           (b_oC, b_pg2), (b_oC,))
                        for par in range(2):
                            psl = slice(par * 64, par * 64 + 64)
                            ebv = eblr[psl, :, c].rearrange("p (a b) -> p a b", b=2)[:, :, par]
                            tt("dve", S_c[psl, :, :], S_c[psl, :, :], bc(ebv, [64, 3, 64], 2), ALU.mult, (b_S_c, b_eblr), (b_S_c,))
                        tt("dve", S_c[:, :, :], S_c[:, :, :], pg3[:, 0:192].rearrange("p (a b) -> p a b", b=64), ALU.add, (b_S_c, b_pg3), (b_S_c,))
                        yield
                chains = [chain_c(), chain_ab()]
                tail_prev = pending_tail[0]
                pending_tail[0] = None
                active = list(chains) + ([tail_prev] if tail_prev is not None else [])
                nxt = head_gen(*head_args(ti + 1)) if ti + 1 < len(tiles) else None
                passed = 0
                head_added = False
                noovl = os.environ.get("K_NOOVL") == "1"
                while active:
                    for g_ in list(active):
                        try:
                            tok = next(g_)
                        except StopIteration:
                            active.remove(g_)
                            continue
                        if tok == "P":
                            passed += 1
                    tail_done = tail_prev is None or tail_prev not in active
                    chains_done = not any(c_ in active for c_ in chains)
                    if nxt is not None and not head_added and tail_done and ((passed >= 2 and not noovl) or chains_done):
                        active.append(nxt)
                        head_added = True
                if nxt is not None and not head_added:
                    run_gens([nxt])
                D_("gC", gC[:, :], b_gC); D_("beta", beta[:, :], b_beta)
                D_("oC", y1[:, 640:1024], b_oC); D_("S_c", S_c[:, :, :].rearrange("p a b -> p (a b)"), b_S_c)
                act(sq[:, 0:640], oAB[:, :], AF.Square, (b_oAB,), (b_sq,))
                act(sq[:, 640:1024], oC[:, :, :].rearrange("p a b -> p (a b)"), AF.Square, (b_oC,), (b_sq,), part=True)
                S.op("dve", lambda e: e.tensor_reduce(out=ss[:, 0:4], in_=sq[:, 0:256].rearrange("p (a b) -> p a b", b=64), axis=AX.X, op=ALU.add),
                     (b_sq,), (b_ss,))
                S.op("dve", lambda e: e.tensor_reduce(out=ss[:, 4:8], in_=sq[:, 256:640].rearrange("p (a b) -> p a b", b=96), axis=AX.X, op=ALU.add),
                     (b_sq,), (b_ss,), part=True)
                S.op("dve", lambda e: e.tensor_reduce(out=ss[:, 8:14], in_=sq[:, 640:1024].rearrange("p (a b) -> p a b", b=64), axis=AX.X, op=ALU.add),
                     (b_sq,), (b_ss,), part=True)
                tt("dve", ss[:, 0:14], ss[:, 0:14], consts[:, C_INVDV:C_INVDV + 14], ALU.mult, (b_ss, b_consts), (b_ss,))
                act(rs[:, 0:14], ss[:, 0:14], AF.Ln, (b_ss,), (b_rs,), bias=eps_ap(NORM_EPS, ss[:, 0:1]))
                act(rs[:, 0:14], rs[:, 0:14], AF.Exp, (b_rs,), (b_rs,), scale=-0.5)
                tt("dve", y1[:, 0:256].rearrange("p (a b) -> p a b", b=64), oAB[:, 0:256].rearrange("p (a b) -> p a b", b=64),
                   bc(rs[:, 0:4], [128, 4, 64], 2), ALU.mult, (b_oAB, b_rs), (b_y1,))
                tt("dve", y1[:, 256:640].rearrange("p (a b) -> p a b", b=96), oAB[:, 256:640].rearrange("p (a b) -> p a b", b=96),
                   bc(rs[:, 4:8], [128, 4, 96], 2), ALU.mult, (b_oAB, b_rs), (b_y1,), part=True)
                tt("dve", y1[:, 640:1024].rearrange("p (a b) -> p a b", b=64), oC[:, :, :],
                   bc(rs[:, 8:14], [128, 6, 64], 2), ALU.mult, (b_oC, b_rs), (b_y1,), part=True)
                tt("dve", yb[:, :], y2[:, :], zs[:, :], ALU.mult, (b_y2, b_zs), (b_yb,))
                def tail_gen(xt=xt, b_xt=b_xt, xh2=xh2, b_xh2=b_xh2, ot=ot, b_ot=b_ot, r0=r0, g_t=g_t, b_g=b_g, slot=slot):
                    py, b_py = ps()
                    pyb = py[:, :].bitcast(BF16)
                    for kc in range(8):
                        tr(pyb[:, kc * 128:(kc + 1) * 128], yb[:, kc * 128:(kc + 1) * 128], identb, (b_yb, b_constb), (b_py,))
                    cp("act", yT[:, :, :].rearrange("p a b -> p (a b)"), pyb[:, :], (b_py,), (b_yT,))
                    yield
                    pos = [ps(), ps()]
                    for nb in range(2):
                        pt, b_pt = pos[nb]
                        for kc in range(8):
                            mm(pt[:, :], yT[:, kc, :], wout[:, kc, nb * 512:(nb + 1) * 512], kc == 0, kc == 7, (b_yT, b_wout), (b_pt,))
                    for nb in range(2):
                        pt, b_pt = pos[nb]
                        tt("dve", t1[:, nb * 512:(nb + 1) * 512], pt[:, :], g_t[:, nb * 512:(nb + 1) * 512], ALU.mult, (b_pt, b_g), (b_t1,),
                           part=(nb > 0))
                    stt(res[:, :], xt[:, :], ALU_ALPHA, t1[:, :], ALU.mult, ALU.add, (b_xt, b_t1), (b_res,))
                    yield
                    S.op("dve", lambda e: e.bn_stats(out=st6b[:, 0:6], in_=res[:, 0:512]), (b_res,), (b_stb,))
                    S.op("dve", lambda e: e.bn_stats(out=st6b[:, 6:12], in_=res[:, 512:1024]), (b_res,), (b_stb,), part=True)
                    S.op("dve", lambda e: e.bn_aggr(out=mvb[:, 0:2], in_=st6b[:, 0:12]), (b_stb,), (b_mvb,))
                    rstd_from(mvb[:, 1:2], LN_EPS, mvb[:, 2:3], mvb[:, 3:4], (b_mvb,), (b_mvb,))
                    ts("dve", mvb[:, 4:5], mvb[:, 0:1], -1.0, mvb[:, 3:4], ALU.mult, ALU.mult, (b_mvb,), (b_mvb,))
                    yield
                    act(xh2[:, :], res[:, :], AF.Identity, (b_res, b_mvb), (b_xh2,), bias=mvb[:, 4:5], scale=mvb[:, 3:4])
                    tt("pool", xh2[:, :], xh2[:, :], lng_b, ALU.mult, (b_xh2, b_rows), (b_xh2,))
                    tt("pool", ot[:, :], xh2[:, :], lnb_b, ALU.add, (b_xh2, b_rows), (b_ot,))
                    D_("y1", y1[:, :], b_y1); D_("t1", t1[:, :], b_t1); D_("rs", rs[:, 0:14], b_rs)
                    S.dma("sp", f"st{li}_{slot}", (lambda ot, r0: lambda e: e.dma_start(out=dst_d[r0:r0 + 128, :], in_=ot[:, :]))(ot, r0),
                          (b_ot,), ())

                pending_tail[0] = tail_gen()

        run_gens([pending_tail[0]] if pending_tail[0] is not None else [])
        pending_tail[0] = None

    ALU_ALPHA = float(ALPHA)
    nl = len(layers)
    for li, l in enumerate(layers):
        src = x_d if li == 0 else scratch
        dst = out_d if li == nl - 1 else scratch
        if li > 0:
            S.final_wait("sp", [k for k in S.count if k.startswith(f"st{li - 1}_")])
        layer(l, src, dst, li)
    S.final_wait("sp", [k for k in S.count if k.startswith(f"st{nl - 1}_") or k.startswith("dbg_")])
    with nc.Block() as block:
        S.emit(block)
    return nc


def prep_layer_inputs(l, w_in, w_out, ada_w, ada_b, ln_g, ln_b, hgrn_lb_logits, gla_w_gk, gla_b_gk,
                      gdn_conv_w, gdn_a_log, gdn_dt_bias, gain_a, gain_b, gain_c):
    w = np.asarray(w_in[l], np.float32)
    offs = np.cumsum([0, 256, 256, 256, 256, 192, 192, 384, 16, 384, 1152, 6, 6, 384])
    qa, fa, ia, za, qb, kb, vb, lr, zb, qkvc, ac, bcc, zc = [w[:, offs[i]:offs[i + 1]] for i in range(13)]
    wp = np.zeros((D, NCOLS), np.float32)
    wp[:, OFF_QA:OFF_QA + 256] = qa
    wp[:, OFF_FA:OFF_FA + 256] = fa
    for h in range(4):
        wp[:, OFF_QB + 64 * h:OFF_QB + 64 * h + 48] = qb[:, 48 * h:48 * (h + 1)]
        wp[:, OFF_KB + 64 * h:OFF_KB + 64 * h + 48] = kb[:, 48 * h:48 * (h + 1)]
    wp[:, OFF_IA:OFF_IA + 256] = ia
    wp[:, OFF_AB:OFF_AB + 6] = ac
    wp[:, OFF_AB + 6:OFF_AB + 12] = bcc
    wp[:, OFF_VB:OFF_VB + 384] = vb
    wp[:, OFF_Z:OFF_Z + 256] = za
    wp[:, OFF_Z + 256:OFF_Z + 640] = zb
    wp[:, OFF_Z + 640:OFF_Z + 1024] = zc
    wp[:, OFF_LR:OFF_LR + 16] = lr
    wp[:, OFF_C:OFF_C + 1152] = qkvc
    rows = np.zeros((1, NROW), np.float32)
    rows[0, R_GAIN:R_GAIN + 1024] = np.concatenate([np.tile(gain_a[l], 4), np.tile(gain_b[l], 4), np.tile(gain_c[l], 6)])
    rows[0, R_LNG:R_LNG + 1024] = ln_g[l]
    rows[0, R_LNB:R_LNB + 1024] = ln_b[l]
    rows[0, R_DT:R_DT + 6] = gdn_dt_bias[l]
    rows[0, R_ALOG:R_ALOG + 6] = gdn_a_log[l]
    wgk = np.zeros((17, 256), np.float32)
    for h in range(4):
        wgk[0:16, 64 * h:64 * h + 48] = gla_w_gk[l][:, 48 * h:48 * (h + 1)]
        wgk[16, 64 * h:64 * h + 48] = gla_b_gk[l][48 * h:48 * (h + 1)]
    cw = np.asarray(gdn_conv_w[l], np.float32)
    convw = np.ascontiguousarray(cw.T.reshape(9, 128, 4).transpose(1, 0, 2)).reshape(128, 36)
    return {f"win{l}": wp, f"wout{l}": np.ascontiguousarray(w_out[l], np.float32),
            f"adaw{l}": np.ascontiguousarray(ada_w[l], np.float32),
            f"adab{l}": np.ascontiguousarray(ada_b[l], np.float32).reshape(1, -1),
            f"rows{l}": rows, f"logits{l}": np.ascontiguousarray(hgrn_lb_logits, np.float32).reshape(1, -1),
            f"wgk{l}": wgk, f"convw{l}": convw}


def core_inputs(xc, cc, layer_maps):
    nseq = xc.shape[0]
    m = {"x": np.ascontiguousarray(xc.reshape(-1, D), np.float32),
         "cT": np.ascontiguousarray(np.asarray(cc, np.float32).reshape(nseq, 8, 128).transpose(2, 1, 0)).reshape(128, 8 * nseq),
         "consts": make_consts()}
    for lm in layer_maps:
        m.update(lm)
    return m


_NC_CACHE = {}


def kernel(x, c, w_in, w_out, ada_w, ada_b, ln_g, ln_b, hgrn_lb_logits, gla_w_gk, gla_b_gk,
           gdn_conv_w, gdn_a_log, gdn_dt_bias, gain_a, gain_b, gain_c):
    x = np.asarray(x, np.float32)
    c = np.asarray(c, np.float32)
    params = [np.asarray(a, np.float32) for a in (w_in, w_out, ada_w, ada_b, ln_g, ln_b, hgrn_lb_logits, gla_w_gk, gla_b_gk,
                                                  gdn_conv_w, gdn_a_log, gdn_dt_bias, gain_a, gain_b, gain_c)]
    nseq = BATCH // NCORES
    ntile = SEQ // 128
    layer_maps = [prep_layer_inputs(l, *params) for l in range(DEPTH)]
    key = ("full",)
    if key not in _NC_CACHE:
        _NC_CACHE[key] = build(nseq, ntile, list(range(DEPTH)))
    nc = _NC_CACHE[key]
    in_maps = [core_inputs(x[i * nseq:(i + 1) * nseq], c[i * nseq:(i + 1) * nseq], layer_maps) for i in range(NCORES)]
    res = run_bass_kernel_spmd(nc, in_maps, core_ids=list(range(NCORES)))
    out = np.concatenate([r["out"].reshape(nseq, SEQ, D) for r in res.results], axis=0)
    return out.astype(np.float32)
```

```python
import os
import numpy as np
import concourse.bass as bass
import concourse.mybir as mybir
from concourse.bass_utils import run_bass_kernel_spmd

F32 = mybir.dt.float32
BF16 = mybir.dt.bfloat16
AF = mybir.ActivationFunctionType
ALU = mybir.AluOpType
AX = mybir.AxisListType

D = 1024
SEQ = 4096
BATCH = 16
DEPTH = 2
NCORES = 8
D_IN = 3740
LN_EPS = 1e-5
NORM_EPS = 1e-6
ALPHA = (2 * DEPTH) ** 0.25

OFF_QA, OFF_FA = 0, 256
OFF_QB, OFF_KB = 512, 768
OFF_IA, OFF_AB = 1024, 1280
OFF_VB = 1292
OFF_Z = 1676
OFF_LR = 2700
OFF_C = 2716
NCOLS = OFF_C + 1152 + 4
TM_BANKS = [(0, 512), (512, 512), (1024, 268), (1292, 384), (1676, 512), (2188, 512)]

C_ID, C_MC, C_CST, C_BD32, C_C32, C_LBLK, C_UBLK, C_UBT, C_ONES, C_CIND, C_BONES, C_SEL, C_INVDV, C_BD64S = (
    0, 128, 256, 272, 400, 464, 592, 720, 848, 976, 980, 1108, 1364, 1380)
NCONST = 1508
R_GAIN, R_LNG, R_LNB, R_DT, R_ALOG = 0, 1024, 2048, 3072, 3078
NROW = 3 * D + 16


def make_consts():
    c = np.zeros((128, NCONST), np.float32)
    p = np.arange(128)
    ch = p // 64
    loc = p % 64
    c[:, C_ID:C_ID + 128] = np.eye(128)
    same = ch[:, None] == ch[None, :]
    le = loc[:, None] <= loc[None, :]
    ch32 = p // 32
    loc32 = p % 32
    same32 = ch32[:, None] == ch32[None, :]
    le32 = loc32[:, None] <= loc32[None, :]
    c[:, C_MC:C_MC + 128] = same32 * (le32.astype(np.float32) - (loc32[:, None] <= 15).astype(np.float32))
    for cc in range(4):
        inc = (ch32 == cc)
        c[:, C_CST + 3 * cc + 0] = inc * (loc32 <= 15)
        c[:, C_CST + 3 * cc + 1] = inc
        c[:, C_CST + 3 * cc + 2] = inc * (loc32 > 15)
    c[:, C_BD32:C_BD32 + 128] = same32 * le32
    for cc in range(4):
        c[:, C_C32 + cc] = (ch32 == cc)
    for cc in range(2):
        c[:, C_CIND + cc] = (ch == cc)
    c[:, C_BD64S:C_BD64S + 128] = same * (loc[:, None] < loc[None, :])
    c[:, C_LBLK:C_LBLK + 128] = same * le
    c[:, C_UBLK:C_UBLK + 128] = same * (loc[:, None] > loc[None, :])
    c[:, C_UBT:C_UBT + 128] = same * (loc[:, None] > loc[None, :])
    c[:, C_ONES:C_ONES + 128] = 1.0
    c[:, C_BONES:C_BONES + 128] = same
    for b in range(2):
        c[b, C_SEL + 128 * b:C_SEL + 128 * (b + 1)] = 1.0
    c[:, C_INVDV:C_INVDV + 4] = 1.0 / 64
    c[:, C_INVDV + 4:C_INVDV + 8] = 1.0 / 96
    c[:, C_INVDV + 8:C_INVDV + 14] = 1.0 / 64
    return c


class Buf:
    __slots__ = ("name", "ws", "rs")

    def __init__(self, name):
        self.name = name
        self.ws = {}
        self.rs = {}


class Sched:
    ENG = ("pe", "act", "dve", "pool", "sp")

    def __init__(self, nc):
        self.nc = nc
        self.sems = {}
        self.count = {}
        self.prog = {e: [] for e in self.ENG}
        self.seen = {e: {} for e in self.ENG}
        for e in self.ENG:
            self._sem(e)

    def _sem(self, key):
        if key not in self.sems:
            self.sems[key] = self.nc.alloc_semaphore(name="s_" + key)
            self.count[key] = 0
        return self.sems[key]

    def _deps(self, eng, reads, writes, part):
        deps = {}

        def add(k, v):
            if k == eng and eng == "pe":
                return
            if deps.get(k, 0) < v:
                deps[k] = v
        for b in reads:
            for k, v in b.ws.items():
                add(k, v)
        for b in writes:
            for k, v in b.rs.items():
                add(k, v)
            for k, v in b.ws.items():
                if part and k == eng:
                    continue
                add(k, v)
        waits = []
        sn = self.seen[eng]
        for k, v in deps.items():
            if sn.get(k, 0) < v:
                sn[k] = v
                waits.append((self.sems[k], v))
        return waits

    def _update(self, key, val, reads, writes, part):
        for b in reads:
            if b.rs.get(key, 0) < val:
                b.rs[key] = val
        for b in writes:
            if part and not b.rs:
                b.ws[key] = val
            else:
                b.ws = {key: val}
            b.rs = {}

    def op(self, eng, fn, reads=(), writes=(), part=False):
        waits = self._deps(eng, reads, writes, part)
        self.count[eng] += 1
        n = self.count[eng]
        self.prog[eng].append((waits, fn, self.sems[eng], 1))
        self._update(eng, n, reads, writes, part)

    def dma(self, queue, key, fn, reads=(), writes=()):
        self._sem(key)
        waits = self._deps(queue, reads, writes, False)
        self.count[key] += 16
        n = self.count[key]
        self.prog[queue].append((waits, fn, self.sems[key], 16))
        self._update(key, n, reads, writes, False)

    def final_wait(self, eng, keys):
        waits = [(self.sems[k], self.count[k]) for k in keys if self.count[k] > 0]
        self.prog[eng].append((waits, None, None, 0))

    def emit(self, block):
        nc = self.nc
        prog = self.prog

        def run(e, lst):
            for waits, fn, sem, inc in lst:
                for s, v in waits:
                    e.wait_ge(s, v)
                if fn is not None:
                    fn(e).then_inc(sem, inc)

        @block.tensor
        def _(e):
            run(e, prog["pe"])

        @block.scalar
        def _(e):
            run(e, prog["act"])

        @block.vector
        def _(e):
            run(e, prog["dve"])

        @block.gpsimd
        def _(e):
            run(e, prog["pool"])

        @block.sync
        def _(e):
            run(e, prog["sp"])


def build(nseq, ntile, layers, first_in_x=True, debug=False):
    nc = bass.Bass("TRN2", target_bir_lowering=False)
    NTOK = nseq * ntile * 128
    S = Sched(nc)
    T = 128

    def dram_in(name, shape):
        return nc.dram_tensor(name, list(shape), F32, kind="ExternalInput").ap()

    x_d = dram_in("x", [NTOK, D])
    cT_d = dram_in("cT", [128, 8 * nseq])
    consts_d = dram_in("consts", [128, NCONST])
    out_d = nc.dram_tensor("out", [NTOK, D], F32, kind="ExternalOutput").ap()
    L = {}
    for l in layers:
        L[l] = dict(
            win=dram_in(f"win{l}", [D, NCOLS]), wout=dram_in(f"wout{l}", [D, D]),
            adaw=dram_in(f"adaw{l}", [D, 3 * D]), adab=dram_in(f"adab{l}", [1, 3 * D]),
            rows=dram_in(f"rows{l}", [1, NROW]), logits=dram_in(f"logits{l}", [1, 256 * DEPTH]), wgk=dram_in(f"wgk{l}", [17, 256]),
            convw=dram_in(f"convw{l}", [128, 36]))
    scratch = None
    if len(layers) > 1:
        scratch = nc.dram_tensor("xmid", [NTOK, D], F32, kind="Internal").ap()

    _cnt = [0]

    def sb(shape, dt=F32, name=None):
        _cnt[0] += 1
        nm = (name or "t") + str(_cnt[0])
        return nc.alloc_sbuf_tensor(nm, list(shape), dt), Buf(nm)

    consts, b_consts = sb([128, NCONST], name="consts")
    constb, b_constb = sb([128, 128 + 128 + 64], BF16, name="constb")
    win, b_win = sb([128, 8, NCOLS], BF16, name="win")
    wout, b_wout = sb([128, 8, D], BF16, name="wout")
    rows, b_rows = sb([128, 3 * D + 16], name="rows")
    wgk, b_wgk = sb([17, 256], name="wgk")
    convw, b_convw = sb([128, 9, 4], name="convw")
    cact, b_cact = sb([128, 8 * nseq], name="cact")
    shiftT, b_shiftT = sb([128, 8, nseq], name="shiftT")
    scaleT, b_scaleT = sb([128, 8, nseq], name="scaleT")
    gateb = [sb([128, D], name="gateb") for _ in range(nseq)]
    lbt, b_lbt = sb([128, 256], name="lb")
    omlb, b_omlb = sb([128, 256], name="omlb")
    negA, b_negA = sb([128, 6], name="negA")
    lrT, b_lrT = sb([17, 128], name="lrT")
    S_ab, b_S_ab = sb([128, 320], name="S_ab")
    S_c, b_S_c = sb([128, 3, 64], name="S_c")
    cpre, b_cpre = sb([128, 9, 131], name="cpre")

    ident = consts[:, C_ID:C_ID + 128]
    identb = constb[:, 0:128]
    bonesb = constb[:, 128:256]

    PSB = []
    for i in range(8):
        PSB.append((nc.alloc_psum_tensor(f"ps{i}", [128, 512], F32), Buf(f"ps{i}")))
    _psi = [0]

    def ps():
        r = PSB[_psi[0] % 6]
        _psi[0] += 1
        return r

    epsc, b_epsc = sb([128, 4], name="epsc")

    def act(out, in_, func, reads, writes, bias=None, scale=None, part=False):
        kw = {}
        if bias is not None:
            kw["bias"] = bias
            if not isinstance(bias, (int, float)):
                reads = tuple(reads) + (b_epsc,)
        if scale is not None:
            kw["scale"] = scale
        S.op("act", lambda e: e.activation(out=out, in_=in_, func=func, **kw), reads, writes, part)

    def tt(eng, out, in0, in1, op, reads, writes, part=False):
        S.op(eng, lambda e: e.tensor_tensor(out=out, in0=in0, in1=in1, op=op), reads, writes, part)

    def ts(eng, out, in0, s1, s2, op0, op1, reads, writes, part=False):
        if s2 is None:
            S.op(eng, lambda e: e.tensor_scalar(out=out, in0=in0, scalar1=s1, scalar2=None, op0=op0), reads, writes, part)
        else:
            S.op(eng, lambda e: e.tensor_scalar(out=out, in0=in0, scalar1=s1, scalar2=s2, op0=op0, op1=op1), reads, writes, part)

    def stt(out, in0, scalar, in1, op0, op1, reads, writes, part=False):
        S.op("dve", lambda e: e.scalar_tensor_tensor(out=out, in0=in0, scalar=scalar, in1=in1, op0=op0, op1=op1),
             reads, writes, part)

    def cp(eng, out, in_, reads, writes, part=False):
        if eng == "act":
            act(out, in_, AF.Copy, reads, writes, part=part)
        else:
            S.op(eng, lambda e: e.tensor_copy(out=out, in_=in_), reads, writes, part)

    NOTP = os.environ.get("K_NOTP") == "1"

    def mm(out, lhsT, rhs, start, stop, reads, writes, tp=None, skip=False):
        kw = {}
        if tp is not None and not NOTP:
            kw["tile_position"] = tp
        if skip:
            kw["skip_group_check"] = True
        S.op("pe", lambda e: e.matmul(out, lhsT, rhs, start=start, stop=stop, **kw), reads, writes, True)

    def tr(out, in_, idn, reads, writes, tp=None):
        if tp is None or NOTP:
            S.op("pe", lambda e: e.transpose(out, in_, idn), reads, writes, True)
        else:
            S.op("pe", lambda e: e.transpose(out, in_, idn, tile_position=tp), reads, writes, True)

    def bc(ap, shape, axis):
        return ap.unsqueeze(axis).broadcast_to(list(shape))

    def rstd_from(var_ap, eps, tmp_ap, out_ap, reads, bufs):
        act(tmp_ap, var_ap, AF.Ln, reads, bufs, bias=eps_ap(eps, var_ap))
        act(out_ap, tmp_ap, AF.Exp, bufs, bufs, scale=-0.5)

    def eps_ap(eps, like):
        npart = like.shape[0]
        base = like.base_partition()
        col = {LN_EPS: 0, NORM_EPS: 1, 1.0: 2}[eps]
        return epsc[base:base + npart, col:col + 1]

    S.dma("sp", "cst", lambda e: e.dma_start(out=consts[:, :], in_=consts_d[:, :]), (), (b_consts,))
    S.dma("sp", "cT", lambda e: e.dma_start(out=cact[:, :], in_=cT_d[:, :]), (), (b_cact,))
    S.op("pool", lambda e: e.memset(epsc[:, 0:1], LN_EPS), (), (b_epsc,))
    S.op("pool", lambda e: e.memset(epsc[:, 1:2], NORM_EPS), (b_epsc,), (b_epsc,))
    S.op("pool", lambda e: e.memset(epsc[:, 2:3], 1.0), (b_epsc,), (b_epsc,))
    cp("dve", identb, ident, (b_consts,), (b_constb,))
    cp("dve", bonesb, consts[:, C_BONES:C_BONES + 128], (b_consts, b_constb), (b_constb,))
    act(cact[:, :], cact[:, :], AF.Silu, (b_cact,), (b_cact,))
    S.op("pool", lambda e: e.memset(lrT[:, :], 1.0), (), (b_lrT,))

    xb = [sb([128, D], name="x") for _ in range(2)]
    st6, b_st = sb([128, 12], name="st")
    mv, b_mv = sb([128, 8], name="mv")
    hT, b_hT = sb([128, 8, 128], BF16, name="hT")
    qAs, b_qAs = sb([128, 256], name="qAs")
    kA, b_kA = sb([128, 256], name="kA")
    qkB, b_qkB = sb([128, 512], name="qkB")
    vAB, b_vAB = sb([128, 640], BF16, name="vAB")
    abC, b_abC = sb([128, 12], name="abC")
    zsb = [sb([128, D], name="zs") for _ in range(2)]
    zs, b_zs = zsb[0]
    modrow, b_modrow = zs[0:nseq, 0:512], b_zs
    adab, b_adab = zs[0:1, 512:1024], b_zs
    gAB, b_gAB = sb([128, 512], name="gAB")
    fA, b_fA = gAB[:, 0:256], b_gAB
    epm, b_epm = sb([128, 1024], name="epm")
    ep, b_ep = epm[:, 0:512], b_epm
    em, b_em = epm[:, 512:1024], b_epm
    e1, b_e1 = epm[:, 512:768], b_epm
    utmp, b_utmp = epm[:, 0:320], b_epm
    Sb, b_Sb = epm[:, 512:672].bitcast(BF16), b_epm
    fac, b_fac = sb([128, 4, 12], name="fac")
    qt, b_qt = sb([128, 512], BF16, name="qt")
    kt, b_kt = sb([128, 512], BF16, name="kt")
    qtTm = [sb([128, 4, 128], BF16, name="qtTm") for _ in range(2)]
    ktTm = [sb([128, 4, 128], BF16, name="ktTm") for _ in range(2)]
    bigb, _b = sb([128, 3072], BF16, name="bigb")
    b_bigb = (Buf("M0h"), Buf("M1h"))
    vbd, b_vbd = sb([128, 4, 640], BF16, name="vbd")
    zerob, b_zerob = sb([128, 128], BF16, name="zerob")
    scT, b_scT = sb([128, 8, 128], BF16, name="scT")
    sqr, b_sqr = sb([128, 6, 128], BF16, name="sqr")
    b_csv = Buf("csv")
    cs, b_cs = sb([128, 9, 128], name="cs")
    qnTm = [sb([128, 3, 128], BF16, name="qnTm") for _ in range(2)]
    knTm = [sb([128, 3, 128], BF16, name="knTm") for _ in range(2)]
    gC, b_gC = sb([128, 6], name="gC")
    gtmp, b_gtmp = sb([128, 4, 6], name="gtmp")
    beta, b_beta = sb([128, 6], name="beta")
    nbeta, b_nbeta = sb([128, 6], name="nbeta")
    eb, b_eb = sb([128, 6], name="eb")
    elb, b_elb = sb([128, 6], name="elb")
    gci, b_gci = sb([128, 6, 2], name="gci")
    eblr, b_eblr = sb([128, 6, 2], name="eblr")
    DTf, b_DTf = sb([128, 6, 128], name="DTf")
    Mb = [(bigb[:, 768 * i:768 * (i + 1)].rearrange("p (a b) -> p a b", b=128), b_bigb) for i in range(2)]
    MTb = [(bigb[:, 768 * (2 + i):768 * (3 + i)].rearrange("p (a b) -> p a b", b=128), b_bigb) for i in range(2)]
    attnT, b_attnT = sb([128, 6, 128], BF16, name="attnT")
    Xbf, _b = sb([128, 6, 128], BF16, name="Xbf")
    B_Xbf = (Buf("Xbf0"), Buf("Xbf1"))
    Xlo, _b = sb([128, 6, 128], BF16, name="Xlo")
    B_Xlo = (Buf("Xlo0"), Buf("Xlo1"))
    X32, _b = sb([128, 6, 128], name="X32")
    B_X32 = (Buf("X320"), Buf("X321"))
    kupdm = [sb([128, 6, 64], BF16, name="kupdm") for _ in range(2)]
    XwTm = [sb([128, 3, 128], BF16, name="XwTm") for _ in range(2)]
    Scb, b_Scb = sb([128, 3, 64], BF16, name="Scb")
    dtmp, b_dtmp = DTf[:, :, 0:64], b_DTf
    vnew, b_vnew = sb([128, 6, 64], BF16, name="vnew")
    ss, b_ss = sb([128, 16], name="ss")
    rs, b_rs = sb([128, 16], name="rs")
    y1, b_y1 = sb([128, D], name="y1")
    yb, b_yb = sb([128, D], BF16, name="yb")
    yT, b_yT = sb([128, 8, 128], BF16, name="yT")
    st6b, b_stb = sb([128, 12], name="stb")
    mvb, b_mvb = sb([128, 8], name="mvb")
    t1, b_t1 = sb([128, D], name="t1")
    xh2b = [sb([128, D], name="xh2")] * 2
    xhat, b_xhat = sb([128, D], name="xhat")
    y2, b_y2 = y1, b_y1
    sq, b_sq = t1, b_t1
    res, b_res = t1, b_t1
    oAB, b_oAB = y1[:, 0:640], b_y1
    oC, b_oC = y1[:, 640:1024].rearrange("p (a b) -> p a b", b=64), b_y1
    gLf, b_gLf = cs[:, 0:6, :], b_cs
    rinv, b_rinv = DTf, b_DTf


    for (t_, b_) in qtTm + ktTm + qnTm + knTm + XwTm + kupdm + [(vnew, b_vnew), (zerob, b_zerob)]:
        S.op("pool", (lambda t_: lambda e: e.memset(t_[:], 0.0))(t_), (), (b_,))

    dbg = {}
    STOP = float(os.environ.get("K_STOP", "99"))

    def early_out(xt, b_xt, r0, dst_d, li, slot):
        S.dma("sp", f"st{li}_{slot}", lambda e: e.dma_start(out=dst_d[r0:r0 + 128, :], in_=xt[:, :]), (b_xt,), ())

    def D_(name, ap, buf, dt=F32):
        if not debug or name in dbg:
            return
        shp = list(ap.shape)
        d = nc.dram_tensor("dbg_" + name, shp, dt, kind="ExternalOutput").ap()
        dbg[name] = d
        S.dma("sp", "dbg_" + name, lambda e: e.dma_start(out=d, in_=ap), (buf,), ())

    pending_tail = [None]

    def run_gens(gens):
        gens = list(gens)
        while gens:
            for g_ in list(gens):
                try:
                    next(g_)
                except StopIteration:
                    gens.remove(g_)

    def layer(l, src_d, dst_d, li):
        W = L[l]
        if STOP <= 0:
            for ti in range(nseq * ntile):
                xt, b_xt = xb[ti % 2]
                r0 = ti * 128
                S.dma("sp", f"xl{li}_{ti % 2}", (lambda xt, r0: lambda e: e.dma_start(out=xt[:, :], in_=src_d[r0:r0 + 128, :]))(xt, r0), (), (b_xt,))
                early_out(xt, b_xt, r0, dst_d, li, ti % 2)
            return
        winr = W["win"].rearrange("(kc p) n -> p kc n", p=128)
        for kc in range(8):
            S.dma("pool", f"win{kc}", (lambda kc: lambda e: e.dma_start(
                out=win[:, kc, :], in_=winr[:, kc, :], max_dma_last_dim=2048))(kc), (), (b_win,))
        woutr = W["wout"].rearrange("(kc p) n -> p kc n", p=128)
        for kc in range(8):
            S.dma("pool", f"wout{kc}", (lambda kc: lambda e: e.dma_start(
                out=wout[:, kc, :], in_=woutr[:, kc, :], max_dma_last_dim=2048))(kc), (), (b_wout,))
        rows_src = bass.AP(W["rows"].tensor, 0, [[0, 128], [1, NROW]])
        S.dma("sp", "rows", lambda e: e.dma_start(out=rows[:, :], in_=rows_src), (), (b_rows,))
        S.dma("sp", "wgk", lambda e: e.dma_start(out=wgk[:, :], in_=W["wgk"][:, :]), (), (b_wgk,))
        S.dma("sp", "convw", lambda e: e.dma_start(out=convw[:, :, :].rearrange("p a b -> p (a b)"), in_=W["convw"][:, :]), (), (b_convw,))
        adawr = W["adaw"].rearrange("(kc p) n -> p kc n", p=128)
        big4 = [xb[0], xb[1], xh2b[0], (xhat, b_xhat)]
        for nchunk in range(6):
            c0 = nchunk * 512
            S.dma("sp", "adab", (lambda c0: lambda e: e.dma_start(out=adab[:, :], in_=W["adab"][:, c0:c0 + 512]))(c0), (), (b_adab,))
            for j4 in range(4):
                bt, b_bt = big4[j4]
                S.dma("sp", f"adaw{j4}", (lambda c0, j4, bt: lambda e: e.dma_start(
                    out=bt[:, :].rearrange("p (a b) -> p a b", b=512), in_=adawr[:, 2 * j4:2 * j4 + 2, c0:c0 + 512]))(c0, j4, bt),
                    (), (b_bt,))
            pt, b_pt = ps()
            for kc in range(8):
                bt, b_bt = big4[kc // 2]
                mm(pt[0:nseq, :], cact[:, kc * nseq:(kc + 1) * nseq], bt[:, (kc % 2) * 512:(kc % 2 + 1) * 512], kc == 0, False,
                   (b_cact, b_bt), (b_pt,))
            mm(pt[0:nseq, :], consts[0:1, C_ONES:C_ONES + nseq], adab[0:1, :], False, True, (b_consts, b_adab), (b_pt,))
            cp("dve", modrow[:, :], pt[0:nseq, :], (b_pt,), (b_modrow,))
            if nchunk < 4:
                p2_, b_p2_ = ps()
                for j in range(4):
                    tr(p2_[:, j * nseq:(j + 1) * nseq], modrow[0:nseq, j * 128:(j + 1) * 128], ident[0:nseq, 0:nseq],
                       (b_modrow, b_consts), (b_p2_,))
                if nchunk < 2:
                    cp("dve", shiftT[:, nchunk * 4:nchunk * 4 + 4, :].rearrange("p a b -> p (a b)"), p2_[:, 0:4 * nseq], (b_p2_,), (b_shiftT,))
                else:
                    ts("dve", scaleT[:, (nchunk - 2) * 4:(nchunk - 2) * 4 + 4, :].rearrange("p a b -> p (a b)"), p2_[:, 0:4 * nseq],
                       1.0, None, ALU.add, None, (b_p2_,), (b_scaleT,))
            else:
                half = nchunk - 4
                for b in range(nseq):
                    g_t, b_g = gateb[b]
                    p2_, b_p2_ = ps()
                    mm(p2_[:, :], consts[0:nseq, C_SEL + 128 * b:C_SEL + 128 * (b + 1)], modrow[0:nseq, :], True, True,
                       (b_consts, b_modrow), (b_p2_,))
                    cp("dve", g_t[:, half * 512:(half + 1) * 512], p2_[:, :], (b_p2_,), (b_g,))
        lbtmp = y1[:, :].rearrange("p (a b) -> p a b", b=256)
        b_lbtmp = b_y1
        lg_src = bass.AP(W["logits"].tensor, 0, [[0, 128], [1, 256 * DEPTH]])
        S.dma("sp", "logits", lambda e: e.dma_start(out=y1[:, 0:256 * DEPTH], in_=lg_src), (), (b_lbtmp,))
        act(y1[:, 0:256 * DEPTH], y1[:, 0:256 * DEPTH], AF.Exp, (b_lbtmp,), (b_lbtmp,))
        den = lbtmp[:, DEPTH, :]
        cp("dve", den, lbtmp[:, 0, :], (b_lbtmp,), (b_lbtmp,))
        for j in range(1, DEPTH):
            tt("dve", den, den, lbtmp[:, j, :], ALU.add, (b_lbtmp,), (b_lbtmp,))
        S.op("dve", lambda e: e.reciprocal(out=den, in_=den), (b_lbtmp,), (b_lbtmp,))
        acc = lbtmp[:, DEPTH + 1, :]
        S.op("dve", lambda e: e.memset(acc, 0.0), (b_lbtmp,), (b_lbtmp,))
        for j in range(1, l + 1):
            tt("dve", acc, acc, lbtmp[:, j, :], ALU.add, (b_lbtmp,), (b_lbtmp,))
        tt("dve", lbt[:, :], acc, den, ALU.mult, (b_lbtmp,), (b_lbt,))
        ts("dve", omlb[:, :], lbt[:, :], -1.0, 1.0, ALU.mult, ALU.add, (b_lbt,), (b_omlb,))
        act(negA[:, :], rows[:, R_ALOG:R_ALOG + 6], AF.Exp, (b_rows,), (b_negA,))
        ts("dve", negA[:, :], negA[:, :], -1.0, None, ALU.mult, None, (b_negA,), (b_negA,))

        gain_b = rows[:, R_GAIN:R_GAIN + D]
        lng_b = rows[:, R_LNG:R_LNG + D]
        lnb_b = rows[:, R_LNB:R_LNB + D]
        dtb = rows[:, R_DT:R_DT + 6]

        segs = [(0, 64), (64, 64), (128, 96), (224, 96)]

        def head_cols(hh):
            if hh < 4:
                return hh * 64, 64
            return 256 + (hh - 4) * 96, 96

        def state_cols(hh):
            ct = hh // 2
            off, dv = segs[ct]
            return off, dv

        def head_gen(b, xt, b_xt, zs, b_zs, r0, slot):
            S.dma("sp", f"xl{li}_{slot}", lambda e: e.dma_start(out=xt[:, :], in_=src_d[r0:r0 + 128, :]), (), (b_xt,))
            S.op("dve", lambda e, xt=xt: e.bn_stats(out=st6[:, 0:6], in_=xt[:, 0:512]), (b_xt,), (b_st,))
            S.op("dve", lambda e, xt=xt: e.bn_stats(out=st6[:, 6:12], in_=xt[:, 512:1024]), (b_xt,), (b_st,), part=True)
            S.op("dve", lambda e: e.bn_aggr(out=mv[:, 0:2], in_=st6[:, 0:12]), (b_st,), (b_mv,))
            rstd_from(mv[:, 1:2], LN_EPS, mv[:, 2:3], mv[:, 3:4], (b_mv,), (b_mv,))
            ts("dve", mv[:, 4:5], mv[:, 0:1], -1.0, mv[:, 3:4], ALU.mult, ALU.mult, (b_mv,), (b_mv,))
            act(xhat[:, :], xt[:, :], AF.Identity, (b_xt, b_mv), (b_xhat,), bias=mv[:, 4:5], scale=mv[:, 3:4])
            yield
            for half in range(2):
                if half == 1:
                    yield
                pt, b_pt = ps()
                for j in range(4):
                    kc = half * 4 + j
                    tr(pt[:, j * 128:(j + 1) * 128], xhat[:, kc * 128:(kc + 1) * 128], ident, (b_xhat, b_consts), (b_pt,))
                for j in range(4):
                    kc = half * 4 + j
                    act(hT[:, kc, :], pt[:, j * 128:(j + 1) * 128], AF.Identity, (b_pt, b_shiftT, b_scaleT), (b_hT,),
                        bias=shiftT[:, kc, b:b + 1], scale=scaleT[:, kc, b:b + 1], part=True)
            yield
            pbank = []
            for (off, n) in TM_BANKS:
                pt, b_pt = ps()
                for kc in range(8):
                    mm(pt[:, 0:n], hT[:, kc, :], win[:, kc, off:off + n], kc == 0, kc == 7, (b_hT, b_win), (b_pt,))
                pbank.append((pt, b_pt))
            p0, b_p0 = pbank[0]
            act(qAs[:, :], p0[:, 0:256], AF.Silu, (b_p0,), (b_qAs,))
            act(fA[:, :], p0[:, 256:512], AF.Sigmoid, (b_p0,), (b_fA,))
            tt("dve", fA[:, :], fA[:, :], omlb[:, :], ALU.mult, (b_fA, b_omlb), (b_fA,))
            tt("dve", fA[:, :], fA[:, :], lbt[:, :], ALU.add, (b_fA, b_lbt), (b_fA,))
            ts("dve", fA[:, :], fA[:, :], 1e-30, None, ALU.max, None, (b_fA,), (b_fA,))
            ts("dve", kA[:, :], fA[:, :], -1.0, 1.0, ALU.mult, ALU.add, (b_fA,), (b_kA,))
            p1, b_p1 = pbank[1]
            cp("act", qkB[:, :], p1[:, :], (b_p1,), (b_qkB,))
            p2, b_p2 = pbank[2]
            cp("act", vAB[:, 0:256], p2[:, 0:256], (b_p2,), (b_vAB,))
            cp("dve", abC[:, :], p2[:, 256:268], (b_p2,), (b_abC,))
            p3, b_p3 = pbank[3]
            cp("act", vAB[:, 256:640], p3[:, 0:384], (b_p3,), (b_vAB,), part=True)
            p4, b_p4 = pbank[4]
            p5, b_p5 = pbank[5]
            act(zs[:, 0:512], p4[:, :], AF.Silu, (b_p4,), (b_zs,))
            act(zs[:, 512:1024], p5[:, :], AF.Silu, (b_p5,), (b_zs,), part=True)
            act(gAB[:, 0:256], fA[:, :], AF.Ln, (b_fA,), (b_gAB,))
            tt("pool", zs[:, :], zs[:, :], gain_b, ALU.mult, (b_zs, b_rows), (b_zs,))
            yield
            pt, b_pt = ps()
            for kc in range(8):
                mm(pt[0:16, 0:128], win[:, kc, OFF_LR:OFF_LR + 16], hT[:, kc, :], kc == 0, kc == 7, (b_hT, b_win), (b_pt,))
            cp("dve", lrT[0:16, :], pt[0:16, 0:128], (b_pt,), (b_lrT,))
            for grp in range(3):
                yield
                pt, b_pt = ps()
                ncts = 4 if grp < 2 else 1
                for j in range(ncts):
                    ct = grp * 4 + j
                    for kc in range(8):
                        mm(pt[:, j * 128:(j + 1) * 128], win[:, kc, OFF_C + ct * 128:OFF_C + (ct + 1) * 128], hT[:, kc, :],
                           kc == 0, kc == 7, (b_hT, b_win), (b_pt,))
                for j in range(ncts):
                    ct = grp * 4 + j
                    cp("act", cpre[:, ct, 3:131], pt[:, j * 128:(j + 1) * 128], (b_pt,), (b_cpre,), part=True)

        tiles = [(b_, it_) for b_ in range(nseq) for it_ in range(ntile)]

        def head_args(k):
            b_, it_ = tiles[k]
            sl_ = k % 2
            return (b_, xb[sl_][0], xb[sl_][1], zsb[sl_][0], zsb[sl_][1], k * 128, sl_)

        run_gens([head_gen(*head_args(0))])
        for b in range(nseq):
            S.op("pool", lambda e: e.memset(S_ab[:, :], 0.0), (), (b_S_ab,))
            S.op("pool", lambda e: e.memset(S_c[:, :, :], 0.0), (), (b_S_c,))
            S.op("pool", lambda e: e.memset(cpre[:, :, 0:3], 0.0), (), (b_cpre,))
            g_t, b_g = gateb[b]
            for it in range(ntile):
                ti = b * ntile + it
                slot = ti % 2
                xt, b_xt = xb[slot]
                xh2, b_xh2 = xh2b[slot]
                zs, b_zs = zsb[slot]
                ot, b_ot = xh2, b_xh2
                r0 = ti * 128
                def chain_ab():
                    pg, b_pg = ps()
                    mm(pg[:, 0:256], lrT[0:17, :], wgk[0:17, :], True, True, (b_lrT, b_wgk), (b_pg,))
                    act(e1, pg[:, 0:256], AF.Exp, (b_pg,), (b_e1,), scale=-1.0)
                    act(e1, e1, AF.Ln, (b_e1,), (b_e1,), bias=eps_ap(1.0, epm[:, 0:1]))
                    ts("dve", gAB[:, 256:512], e1, -1.0 / 16.0, None, ALU.mult, None, (b_e1,), (b_gAB,), part=True)
                    yield
                    pc, b_pc = ps()
                    mm(pc[:, :], consts[:, C_MC:C_MC + 128], gAB[:, :], True, True, (b_consts, b_gAB), (b_pc,))
                    pf, b_pf = ps()
                    for ct in range(4):
                        mm(pf[:, ct * 12:(ct + 1) * 12], gAB[:, ct * 128:(ct + 1) * 128], consts[:, C_CST:C_CST + 12], True, True,
                           (b_gAB, b_consts), (b_pf,))
                    act(fac[:, :, :].rearrange("p a b -> p (a b)"), pf[:, 0:48], AF.Exp, (b_pf,), (b_fac,))
                    act(ep[:, :], pc[:, :], AF.Exp, (b_pc,), (b_ep,))
                    act(em[:, :], pc[:, :], AF.Exp, (b_pc,), (b_em,), scale=-1.0)
                    tt("dve", qt[:, 0:256], qAs[:, :], ep[:, 0:256], ALU.mult, (b_qAs, b_ep), (b_qt,))
                    stt(qt[:, 256:512], qkB[:, 0:256], 48.0 ** -0.5, ep[:, 256:512], ALU.mult, ALU.mult, (b_qkB, b_ep), (b_qt,), part=True)
                    tt("dve", kt[:, 0:256], kA[:, :], em[:, 0:256], ALU.mult, (b_kA, b_em), (b_kt,))
                    tt("dve", kt[:, 256:512], qkB[:, 256:512], em[:, 256:512], ALU.mult, (b_qkB, b_em), (b_kt,), part=True)
                    yield
                    pT, b_pT = ps()
                    pTb = pT[:, :].bitcast(BF16)
                    for ct in range(4):
                        tr(pTb[:, ct * 128:(ct + 1) * 128], qt[:, ct * 128:(ct + 1) * 128], identb, (b_qt, b_constb), (b_pT,))
                    for ct in range(4):
                        tr(pTb[:, 512 + ct * 128:512 + (ct + 1) * 128], kt[:, ct * 128:(ct + 1) * 128], identb, (b_kt, b_constb), (b_pT,))
                    for par in range(2):
                        hs = slice(64 * par, 64 * par + 64)
                        qm, b_qm = qtTm[par]
                        km, b_km = ktTm[par]
                        cp("act", qm[hs, :, :].rearrange("p a b -> p (a b)"), pTb[hs, 0:512], (b_pT,), (b_qm,))
                        cp("act", km[hs, :, :].rearrange("p a b -> p (a b)"), pTb[hs, 512:1024], (b_pT,), (b_km,))
                    for c in range(4):
                        if c % 2 == 0:
                            act(vbd[:, c, :], vAB[:, :], AF.Copy, (b_vAB, b_consts), (b_vbd,), scale=consts[:, C_C32 + c:C_C32 + c + 1], part=(c > 0))
                        else:
                            ts("dve", vbd[:, c, :], vAB[:, :], consts[:, C_C32 + c:C_C32 + c + 1], None, ALU.mult, None,
                               (b_vAB, b_consts), (b_vbd,), part=True)
                    yield
                    psS = [ps(), ps()]
                    for hh in range(8):
                        ct, par = hh // 2, hh % 2
                        pt, b_pt = psS[hh // 4]
                        mm(pt[:, (hh % 4) * 128:(hh % 4 + 1) * 128], ktTm[par][0][:, ct, :], qtTm[par][0][:, ct, :], True, True,
                           (ktTm[par][1], qtTm[par][1]), (b_pt,))
                    for g4 in range(2):
                        pt, b_pt = psS[g4]
                        tt("dve", scT[:, 4 * g4:4 * g4 + 4, :], pt[:, :].rearrange("p (a b) -> p a b", b=128),
                           bc(consts[:, C_BD32:C_BD32 + 128], [128, 4, 128], 1), ALU.mult, (b_pt, b_consts), (b_scT,), part=(g4 > 0))
                    poA, b_poA = PSB[6]
                    poB, b_poB = PSB[7]
                    yield
                    mm(poA[:, 0:256], zerob[:, :], vAB[:, 0:256], True, False, (b_zerob, b_vAB), (b_poA,), skip=True)
                    mm(poB[:, 0:384], zerob[:, :], vAB[:, 256:640], True, False, (b_zerob, b_vAB), (b_poB,), skip=True)
                    for hh in range(8):
                        ocol, dv = head_cols(hh)
                        po, b_po = (poA, b_poA) if hh < 4 else (poB, b_poB)
                        oc = ocol if hh < 4 else ocol - 256
                        mm(po[:, oc:oc + dv], scT[:, hh, :], vAB[:, ocol:ocol + dv], False, False, (b_scT, b_vAB), (b_po,), skip=True)
                    yield "P"
                    for c in range(4):
                        r32 = slice(32 * c, 32 * c + 32)
                        for (c0_, c1_, dv_) in ((0, 2, 64), (2, 4, 96)):
                            o0 = segs[c0_][0]
                            w_ = 2 * dv_
                            tt("dve", Sb[:, o0:o0 + w_].rearrange("p (a b) -> p a b", b=dv_),
                               S_ab[:, o0:o0 + w_].rearrange("p (a b) -> p a b", b=dv_),
                               bc(fac[:, c0_:c1_, 3 * c], [128, 2, dv_], 2), ALU.mult, (b_S_ab, b_fac), (b_Sb,), part=(c0_ > 0))
                        for hh in range(8):
                            ct, par = hh // 2, hh % 2
                            ocol, dv = head_cols(hh)
                            soff, _ = state_cols(hh)
                            po, b_po = (poA, b_poA) if hh < 4 else (poB, b_poB)
                            oc = ocol if hh < 4 else ocol - 256
                            mm(po[r32, oc:oc + dv], qtTm[par][0][:, ct, r32], Sb[:, soff:soff + dv], False, (c == 3 and hh in (3, 7)),
                               (qtTm[par][1], b_Sb), (b_po,), tp=(0, 32 * c), skip=True)
                        yield
                        pu, b_pu = ps()
                        for hh in range(8):
                            ct, par = hh // 2, hh % 2
                            ocol, dv = head_cols(hh)
                            soff, _ = state_cols(hh)
                            mm(pu[par * 64:par * 64 + 64, soff:soff + dv], kt[:, hh * 64:(hh + 1) * 64], vbd[:, c, ocol:ocol + dv],
                               True, True, (b_kt, b_vbd), (b_pu,), tp=(0, par * 64))
                        for (c0_, c1_, dv_) in ((0, 2, 64), (2, 4, 96)):
                            o0 = segs[c0_][0]
                            w_ = 2 * dv_
                            tt("dve", utmp[:, o0:o0 + w_].rearrange("p (a b) -> p a b", b=dv_),
                               pu[:, o0:o0 + w_].rearrange("p (a b) -> p a b", b=dv_),
                               bc(fac[:, c0_:c1_, 3 * c + 2], [128, 2, dv_], 2), ALU.mult, (b_pu, b_fac), (b_utmp,), part=(c0_ > 0))
                        for (c0_, c1_, dv_) in ((0, 2, 64), (2, 4, 96)):
                            o0 = segs[c0_][0]
                            w_ = 2 * dv_
                            tt("dve", S_ab[:, o0:o0 + w_].rearrange("p (a b) -> p a b", b=dv_),
                               S_ab[:, o0:o0 + w_].rearrange("p (a b) -> p a b", b=dv_),
                               bc(fac[:, c0_:c1_, 3 * c + 1], [128, 2, dv_], 2), ALU.mult, (b_S_ab, b_fac), (b_S_ab,))
                        tt("dve", S_ab[:, :], S_ab[:, :], utmp[:, :], ALU.add, (b_S_ab, b_utmp), (b_S_ab,))
                        yield
                    cp("act", oAB[:, 0:256], poA[:, 0:256], (b_poA,), (b_oAB,))
                    cp("act", oAB[:, 256:640], poB[:, 0:384], (b_poB,), (b_oAB,), part=True)
                def chain_c():
                    for ct in range(9):
                        bq = b_cs if ct < 6 else b_csv
                        ts("dve", cs[:, ct, :], cpre[:, ct, 0:128], convw[:, ct, 0:1], None, ALU.mult, None, (b_cpre, b_convw), (bq,),
                           part=(ct % 6 > 0))
                        for j in range(1, 4):
                            stt(cs[:, ct, :], cpre[:, ct, j:j + 128], convw[:, ct, j:j + 1], cs[:, ct, :], ALU.mult, ALU.add,
                                (b_cpre, b_convw, bq), (bq,), part=True)
                        if ct == 5:
                            act(cs[:, 0:6, :].rearrange("p a b -> p (a b)"), cs[:, 0:6, :].rearrange("p a b -> p (a b)"), AF.Silu, (b_cs,), (b_cs,))
                        if ct == 8:
                            act(cs[:, 6:9, :].rearrange("p a b -> p (a b)"), cs[:, 6:9, :].rearrange("p a b -> p (a b)"), AF.Silu, (b_csv,), (b_csv,))
                        if ct % 3 == 2:
                            yield
                    cp("pool", cpre[:, :, 0:3], cpre[:, :, 128:131], (b_cpre,), (b_cpre,))
                    act(sqr[:, :, :].rearrange("p a b -> p (a b)"), cs[:, 0:6, :].rearrange("p a b -> p (a b)"), AF.Square, (b_cs,), (b_sqr,))
                    pn = [ps(), ps()]
                    for j in range(6):
                        pt, b_pt = pn[j // 4]
                        mm(pt[:, (j % 4) * 128:(j % 4 + 1) * 128], bonesb, sqr[:, j, :], True, True, (b_constb, b_sqr), (b_pt,))
                    for (pt, b_pt), lo, n in ((pn[0], 0, 4), (pn[1], 4, 2)):
                        dst = rinv[:, lo:lo + n, :].rearrange("p a b -> p (a b)")
                        act(dst, pt[:, 0:n * 128], AF.Ln, (b_pt,), (b_rinv,), bias=eps_ap(NORM_EPS, pt[:, 0:1]), part=True)
                        act(dst, dst, AF.Exp, (b_rinv,), (b_rinv,), scale=-0.5)
                    yield
                    for par in range(2):
                        hs = slice(64 * par, 64 * par + 64)
                        qm, b_qm = qnTm[par]
                        stt(qm[hs, :, :], cs[hs, 0:3, :], 0.125, rinv[hs, 0:3, :], ALU.mult, ALU.mult, (b_cs, b_rinv), (b_qm,))
                    tt("dve", rinv[:, 3:6, :], cs[:, 3:6, :], rinv[:, 3:6, :], ALU.mult, (b_cs, b_rinv), (b_rinv,))
                    for par in range(2):
                        hs = slice(64 * par, 64 * par + 64)
                        km, b_km = knTm[par]
                        cp("act", km[hs, :, :].rearrange("p a b -> p (a b)"), rinv[hs, 3:6, :].rearrange("p a b -> p (a b)"), (b_rinv,), (b_km,))
                    yield
                    pk, b_pk = ps()
                    for j in range(3):
                        tr(pk[:, j * 128:(j + 1) * 128], rinv[:, 3 + j, :], ident, (b_rinv, b_consts), (b_pk,))
                    pv, b_pv = ps()
                    for j in range(3):
                        tr(pv[:, j * 128:(j + 1) * 128], cs[:, 6 + j, :], ident, (b_csv, b_consts), (b_pv,))
                    cp("act", X32[:, :, 0:64], pv[:, 0:384].rearrange("p (a b) -> p a b", b=64), (b_pv,), (*B_X32,))
                    tt("dve", gtmp[:, 0, :], abC[:, 0:6], dtb, ALU.add, (b_abC, b_rows), (b_gtmp,))
                    act(gtmp[:, 1, :], gtmp[:, 0, :], AF.Exp, (b_gtmp,), (b_gtmp,))
                    act(gtmp[:, 2, :], gtmp[:, 1, :], AF.Ln, (b_gtmp,), (b_gtmp,), bias=eps_ap(1.0, gtmp[:, 0, 0:1]))
                    tt("dve", gC[:, :], gtmp[:, 2, :], negA[:, :], ALU.mult, (b_gtmp, b_negA), (b_gC,))
                    act(beta[:, :], abC[:, 6:12], AF.Exp, (b_abC,), (b_beta,), scale=-1.0)
                    ts("dve", beta[:, :], beta[:, :], 1.0, None, ALU.add, None, (b_beta,), (b_beta,))
                    S.op("dve", lambda e: e.reciprocal(out=beta[:, :], in_=beta[:, :]), (b_beta,), (b_beta,))
                    ts("dve", nbeta[:, :], beta[:, :], -1.0, None, ALU.mult, None, (b_beta,), (b_nbeta,))
                    pb, b_pb = ps()
                    mm(pb[:, 0:6], consts[:, C_LBLK:C_LBLK + 128], gC[:, :], True, True, (b_consts, b_gC), (b_pb,))
                    mm(pb[:, 8:14], consts[:, C_UBLK:C_UBLK + 128], gC[:, :], True, True, (b_consts, b_gC), (b_pb,))
                    tt("dve", gci[:, :, :], bc(gC[:, :], [128, 6, 2], 2), bc(consts[:, C_CIND:C_CIND + 2], [128, 6, 2], 1), ALU.mult,
                       (b_gC, b_consts), (b_gci,))
                    mm(pb[:, 16:28], consts[:, C_ONES:C_ONES + 128], gci[:, :, :].rearrange("p a b -> p (a b)"), True, True,
                       (b_consts, b_gci), (b_pb,))
                    act(eb[:, :], pb[:, 0:6], AF.Exp, (b_pb,), (b_eb,))
                    act(elb[:, :], pb[:, 8:14], AF.Exp, (b_pb,), (b_elb,))
                    act(eblr[:, :, :].rearrange("p a b -> p (a b)"), pb[:, 16:28], AF.Exp, (b_pb,), (b_eblr,))
                    pk3 = pk[:, 0:384].rearrange("p (a b) -> p a b", b=64)
                    tt("dve", X32[:, :, 64:128], pk3, bc(eb[:, :], [128, 6, 64], 2), ALU.mult, (b_pk, b_eb), (*B_X32,), part=True)
                    for c in range(2):
                        rsl = slice(64 * c, 64 * c + 64)
                        kum, b_kum = kupdm[c]
                        tt("dve", kum[rsl, :, :], pk3[rsl, :, :], bc(elb[rsl, :], [64, 6, 64], 2), ALU.mult, (b_pk, b_elb), (b_kum,))
                    cp("act", Xbf[:, :, :].rearrange("p a b -> p (a b)"), X32[:, :, :].rearrange("p a b -> p (a b)"), (*B_X32,), (*B_Xbf,))
                    tt("dve", Xlo[:, :, :], X32[:, :, :], Xbf[:, :, :], ALU.subtract, (*B_X32, *B_Xbf), (*B_Xlo,))
                    yield
                    yield "P"
                    tt("dve", gLf, bc(consts[:, C_LBLK:C_LBLK + 128], [128, 6, 128], 1), bc(gC[:, :], [128, 6, 128], 2), ALU.mult,
                       (b_consts, b_gC), (b_gLf,))
                    prs = [ps(), ps()]
                    for g3 in range(2):
                        pt, b_pt = prs[g3]
                        mm(pt[:, 0:384], consts[:, C_UBT:C_UBT + 128], gLf[:, 3 * g3:3 * g3 + 3, :].rearrange("p a b -> p (a b)"), True, True,
                           (b_consts, b_gLf), (b_pt,))
                    for g3 in range(2):
                        pt, b_pt = prs[g3]
                        act(DTf[:, 3 * g3:3 * g3 + 3, :].rearrange("p a b -> p (a b)"), pt[:, 0:384], AF.Exp, (b_pt,), (b_DTf,), part=(g3 > 0))
                    tt("dve", DTf[:, :, :], DTf[:, :, :], bc(consts[:, C_LBLK:C_LBLK + 128], [128, 6, 128], 1), ALU.mult, (b_DTf, b_consts), (b_DTf,))
                    yield
                    pkk = [ps(), ps()]
                    pkq = [ps(), ps()]
                    for h in range(6):
                        ct, par = h // 2, h % 2
                        km, b_km = knTm[par]
                        qm, b_qm = qnTm[par]
                        pt, b_pt = pkk[h // 3]
                        mm(pt[:, (h % 3) * 128:(h % 3 + 1) * 128], km[:, ct, :], km[:, ct, :], True, True, (b_km,), (b_pt,))
                        pt, b_pt = pkq[h // 3]
                        mm(pt[:, (h % 3) * 128:(h % 3 + 1) * 128], km[:, ct, :], qm[:, ct, :], True, True, (b_km, b_qm), (b_pt,))
                    M0, b_M0 = Mb[0]
                    for g3 in range(2):
                        pt, b_pt = pkq[g3]
                        tt("dve", attnT[:, 3 * g3:3 * g3 + 3, :], pt[:, 0:384].rearrange("p (a b) -> p a b", b=128), DTf[:, 3 * g3:3 * g3 + 3, :],
                           ALU.mult, (b_pt, b_DTf), (b_attnT,), part=(g3 > 0))
                    tt("dve", DTf[:, :, :], DTf[:, :, :], bc(consts[:, C_BD64S:C_BD64S + 128], [128, 6, 128], 1), ALU.mult, (b_DTf, b_consts), (b_DTf,))
                    for g3 in range(2):
                        pt, b_pt = pkk[g3]
                        tt("dve", DTf[:, 3 * g3:3 * g3 + 3, :], pt[:, 0:384].rearrange("p (a b) -> p a b", b=128), DTf[:, 3 * g3:3 * g3 + 3, :],
                           ALU.mult, (b_pt, b_DTf), (b_DTf,))
                    tt("dve", M0[:, :, :], DTf[:, :, :], bc(nbeta[:, :], [128, 6, 128], 2), ALU.mult, (b_DTf, b_nbeta), (*b_M0,))
                    yield
                    MT0, b_MT0 = MTb[0]
                    pm, b_pm = ps()
                    pmb = pm[:, :].bitcast(BF16)
                    for h in range(6):
                        tr(pmb[:, h * 128:(h + 1) * 128], M0[:, h, :], identb, (*b_M0, b_constb), (b_pm,))
                    cp("act", MT0[:, :, :].rearrange("p a b -> p (a b)"), pmb[:, 0:768], (b_pm,), (*b_MT0,))
                    def neumann(g):
                        sl = slice(3 * g, 3 * g + 3)
                        bX32, bXbf, bXlo, bM = B_X32[g], B_Xbf[g], B_Xlo[g], b_bigb[g]
                        for lv in range(6):
                            Mc_ = Mb[lv % 2][0]
                            MTc = MTb[lv % 2][0]
                            pt, b_pt = ps()
                            for j in range(3):
                                h = 3 * g + j
                                mm(pt[:, j * 128:(j + 1) * 128], Mc_[:, h, :], Xbf[:, h, :], True, False, (bM, bXbf), (b_pt,))
                                mm(pt[:, j * 128:(j + 1) * 128], Mc_[:, h, :], Xlo[:, h, :], False, True, (bM, bXlo), (b_pt,))
                            tt("dve", X32[:, sl, :], pt[:, 0:384].rearrange("p (a b) -> p a b", b=128), X32[:, sl, :], ALU.add,
                               (b_pt, bX32), (bX32,))
                            cp("dve", Xbf[:, sl, :].rearrange("p a b -> p (a b)"), X32[:, sl, :].rearrange("p a b -> p (a b)"), (bX32,), (bXbf,))
                            if lv < 5:
                                tt("dve", Xlo[:, sl, :], X32[:, sl, :], Xbf[:, sl, :], ALU.subtract, (bX32, bXbf), (bXlo,))
                            yield
                            if lv < 5:
                                Mn = Mb[(lv + 1) % 2][0]
                                MTn = MTb[(lv + 1) % 2][0]
                                pt1, b_pt1 = ps()
                                pt2, b_pt2 = ps()
                                for j in range(3):
                                    h = 3 * g + j
                                    mm(pt1[:, j * 128:(j + 1) * 128], MTc[:, h, :], Mc_[:, h, :], True, True, (bM,), (b_pt1,))
                                for j in range(3):
                                    h = 3 * g + j
                                    mm(pt2[:, j * 128:(j + 1) * 128], Mc_[:, h, :], MTc[:, h, :], True, True, (bM,), (b_pt2,))
                                cp("act", Mn[:, sl, :].rearrange("p a b -> p (a b)"), pt1[:, 0:384], (b_pt1,), (bM,))
                                cp("act", MTn[:, sl, :].rearrange("p a b -> p (a b)"), pt2[:, 0:384], (b_pt2,), (bM,))
                                yield
                    yield
                    subs = [neumann(0), neumann(1)]
                    while subs:
                        for g_ in list(subs):
                            try:
                                next(g_)
                            except StopIteration:
                                subs.remove(g_)
                        yield
                    Xf = Xbf
                    yield
                    pw, b_pw = ps()
                    pwb = pw[:, :].bitcast(BF16)
                    for h in range(6):
                        ct, par = h // 2, h % 2
                        if par == 0:
                            tr(pwb[0:64, ct * 128:(ct + 1) * 128], Xf[:, h, 64:128], identb, (*B_Xbf, b_constb), (b_pw,))
                        else:
                            tr(pwb[:, 384 + ct * 128:384 + (ct + 1) * 128], Xf[:, h, :], identb, (*B_Xbf, b_constb), (b_pw,))
                    cp("act", XwTm[0][0][0:64, :, :].rearrange("p a b -> p (a b)"), pwb[0:64, 0:384], (b_pw,), (XwTm[0][1],))
                    cp("act", XwTm[1][0][64:128, :, :].rearrange("p a b -> p (a b)"), pwb[64:128, 384:768], (b_pw,), (XwTm[1][1],))
                    yield
                    for c in range(2):
                        rsl = slice(64 * c, 64 * c + 64)
                        kum, b_kum = kupdm[c]
                        cp("dve", Scb[:, :, :], S_c[:, :, :], (b_S_c,), (b_Scb,))
                        pg0, b_pg0 = ps()
                        pg1, b_pg1 = ps()
                        for h in range(6):
                            ct, par = h // 2, h % 2
                            mm(pg0[rsl, h * 64:(h + 1) * 64], XwTm[par][0][:, ct, 64 * c:64 * c + 64], Scb[:, ct, :], True, True,
                               (XwTm[par][1], b_Scb), (b_pg0,), tp=(0, 64 * c))
                            mm(pg1[rsl, h * 64:(h + 1) * 64], qnTm[par][0][:, ct, 64 * c:64 * c + 64], Scb[:, ct, :], True, True,
                               (qnTm[par][1], b_Scb), (b_pg1,), tp=(0, 64 * c))
                        tt("dve", dtmp[rsl, :, :], X32[rsl, :, 0:64], pg0[rsl, 0:384].rearrange("p (a b) -> p a b", b=64), ALU.subtract,
                           (*B_X32, b_pg0), (b_dtmp,))
                        tt("dve", vnew[rsl, :, :], dtmp[rsl, :, :], bc(beta[rsl, :], [64, 6, 64], 2), ALU.mult, (b_dtmp, b_beta), (b_vnew,))
                        tt("dve", oC[rsl, :, :], pg1[rsl, 0:384].rearrange("p (a b) -> p a b", b=64), bc(eb[rsl, :], [64, 6, 64], 2), ALU.mult,
                           (b_pg1, b_eb), (b_oC,))
                        yield
                        pg2, b_pg2 = ps()
                        pg3, b_pg3 = ps()
                        for h in range(6):
                            ct, par = h // 2, h % 2
                            mm(pg2[rsl, h * 64:(h + 1) * 64], attnT[:, h, 64 * c:64 * c + 64], vnew[:, h, :], True, True, (b_attnT, b_vnew), (b_pg2,),
                               tp=(0, 64 * c))
                            mm(pg3[par * 64:par * 64 + 64, ct * 64:(ct + 1) * 64], kum[:, h, :], vnew[:, h, :], True, True,
                               (b_kum, b_vnew), (b_pg3,), tp=(0, par * 64))
                        tt("dve", oC[rsl, :, :], oC[rsl, :, :], pg2[rsl, 0:384].rearrange("p (a b) -> p a b", b=64), ALU.add,
                           (b_oC, b_pg2), (b_oC,))
                        for par in range(2):
                            psl = slice(par * 64, par * 64 + 64)
                            ebv = eblr[psl, :, c].rearrange("p (a b) -> p a b", b=2)[:, :, par]
                            tt("dve", S_c[psl, :, :], S_c[psl, :, :], bc(ebv, [64, 3, 64], 2), ALU.mult, (b_S_c, b_eblr), (b_S_c,))
                        tt("dve", S_c[:, :, :], S_c[:, :, :], pg3[:, 0:192].rearrange("p (a b) -> p a b", b=64), ALU.add, (b_S_c, b_pg3), (b_S_c,))
                        yield
                chains = [chain_c(), chain_ab()]
                tail_prev = pending_tail[0]
                pending_tail[0] = None
                active = list(chains) + ([tail_prev] if tail_prev is not None else [])
                nxt = head_gen(*head_args(ti + 1)) if ti + 1 < len(tiles) else None
                passed = 0
                head_added = False
                noovl = os.environ.get("K_NOOVL") == "1"
                CW = int(os.environ.get("K_CW", "3"))
                while active:
                    for g_ in list(active):
                        for _rep in range(CW if g_ is chains[0] else 1):
                            try:
                                tok = next(g_)
                            except StopIteration:
                                active.remove(g_)
                                break
                            if tok == "P":
                                passed += 1
                    tail_done = tail_prev is None or tail_prev not in active
                    chains_done = not any(c_ in active for c_ in chains)
                    if nxt is not None and not head_added and tail_done and ((passed >= 2 and not noovl) or chains_done):
                        active.append(nxt)
                        head_added = True
                if nxt is not None and not head_added:
                    run_gens([nxt])
                D_("gC", gC[:, :], b_gC); D_("beta", beta[:, :], b_beta)
                D_("oC", y1[:, 640:1024], b_oC); D_("S_c", S_c[:, :, :].rearrange("p a b -> p (a b)"), b_S_c)
                act(sq[:, 0:640], oAB[:, :], AF.Square, (b_oAB,), (b_sq,))
                act(sq[:, 640:1024], oC[:, :, :].rearrange("p a b -> p (a b)"), AF.Square, (b_oC,), (b_sq,), part=True)
                S.op("dve", lambda e: e.tensor_reduce(out=ss[:, 0:4], in_=sq[:, 0:256].rearrange("p (a b) -> p a b", b=64), axis=AX.X, op=ALU.add),
                     (b_sq,), (b_ss,))
                S.op("dve", lambda e: e.tensor_reduce(out=ss[:, 4:8], in_=sq[:, 256:640].rearrange("p (a b) -> p a b", b=96), axis=AX.X, op=ALU.add),
                     (b_sq,), (b_ss,), part=True)
                S.op("dve", lambda e: e.tensor_reduce(out=ss[:, 8:14], in_=sq[:, 640:1024].rearrange("p (a b) -> p a b", b=64), axis=AX.X, op=ALU.add),
                     (b_sq,), (b_ss,), part=True)
                tt("dve", ss[:, 0:14], ss[:, 0:14], consts[:, C_INVDV:C_INVDV + 14], ALU.mult, (b_ss, b_consts), (b_ss,))
                act(rs[:, 0:14], ss[:, 0:14], AF.Ln, (b_ss,), (b_rs,), bias=eps_ap(NORM_EPS, ss[:, 0:1]))
                act(rs[:, 0:14], rs[:, 0:14], AF.Exp, (b_rs,), (b_rs,), scale=-0.5)
                tt("dve", y1[:, 0:256].rearrange("p (a b) -> p a b", b=64), oAB[:, 0:256].rearrange("p (a b) -> p a b", b=64),
                   bc(rs[:, 0:4], [128, 4, 64], 2), ALU.mult, (b_oAB, b_rs), (b_y1,))
                tt("dve", y1[:, 256:640].rearrange("p (a b) -> p a b", b=96), oAB[:, 256:640].rearrange("p (a b) -> p a b", b=96),
                   bc(rs[:, 4:8], [128, 4, 96], 2), ALU.mult, (b_oAB, b_rs), (b_y1,), part=True)
                tt("dve", y1[:, 640:1024].rearrange("p (a b) -> p a b", b=64), oC[:, :, :],
                   bc(rs[:, 8:14], [128, 6, 64], 2), ALU.mult, (b_oC, b_rs), (b_y1,), part=True)
                tt("dve", yb[:, :], y2[:, :], zs[:, :], ALU.mult, (b_y2, b_zs), (b_yb,))
                def tail_gen(xt=xt, b_xt=b_xt, xh2=xh2, b_xh2=b_xh2, ot=ot, b_ot=b_ot, r0=r0, g_t=g_t, b_g=b_g, slot=slot):
                    py, b_py = ps()
                    pyb = py[:, :].bitcast(BF16)
                    for kc in range(8):
                        tr(pyb[:, kc * 128:(kc + 1) * 128], yb[:, kc * 128:(kc + 1) * 128], identb, (b_yb, b_constb), (b_py,))
                    cp("act", yT[:, :, :].rearrange("p a b -> p (a b)"), pyb[:, :], (b_py,), (b_yT,))
                    yield
                    pos = [ps(), ps()]
                    for nb in range(2):
                        pt, b_pt = pos[nb]
                        for kc in range(8):
                            mm(pt[:, :], yT[:, kc, :], wout[:, kc, nb * 512:(nb + 1) * 512], kc == 0, kc == 7, (b_yT, b_wout), (b_pt,))
                    for nb in range(2):
                        pt, b_pt = pos[nb]
                        tt("dve", t1[:, nb * 512:(nb + 1) * 512], pt[:, :], g_t[:, nb * 512:(nb + 1) * 512], ALU.mult, (b_pt, b_g), (b_t1,),
                           part=(nb > 0))
                    stt(res[:, :], xt[:, :], ALU_ALPHA, t1[:, :], ALU.mult, ALU.add, (b_xt, b_t1), (b_res,))
                    yield
                    S.op("dve", lambda e: e.bn_stats(out=st6b[:, 0:6], in_=res[:, 0:512]), (b_res,), (b_stb,))
                    S.op("dve", lambda e: e.bn_stats(out=st6b[:, 6:12], in_=res[:, 512:1024]), (b_res,), (b_stb,), part=True)
                    S.op("dve", lambda e: e.bn_aggr(out=mvb[:, 0:2], in_=st6b[:, 0:12]), (b_stb,), (b_mvb,))
                    rstd_from(mvb[:, 1:2], LN_EPS, mvb[:, 2:3], mvb[:, 3:4], (b_mvb,), (b_mvb,))
                    ts("dve", mvb[:, 4:5], mvb[:, 0:1], -1.0, mvb[:, 3:4], ALU.mult, ALU.mult, (b_mvb,), (b_mvb,))
                    yield
                    act(xh2[:, :], res[:, :], AF.Identity, (b_res, b_mvb), (b_xh2,), bias=mvb[:, 4:5], scale=mvb[:, 3:4])
                    tt("pool", xh2[:, :], xh2[:, :], lng_b, ALU.mult, (b_xh2, b_rows), (b_xh2,))
                    tt("pool", ot[:, :], xh2[:, :], lnb_b, ALU.add, (b_xh2, b_rows), (b_ot,))
                    D_("y1", y1[:, :], b_y1); D_("t1", t1[:, :], b_t1); D_("rs", rs[:, 0:14], b_rs)
                    S.dma("sp", f"st{li}_{slot}", (lambda ot, r0: lambda e: e.dma_start(out=dst_d[r0:r0 + 128, :], in_=ot[:, :]))(ot, r0),
                          (b_ot,), ())

                pending_tail[0] = tail_gen()

        run_gens([pending_tail[0]] if pending_tail[0] is not None else [])
        pending_tail[0] = None

    ALU_ALPHA = float(ALPHA)
    nl = len(layers)
    for li, l in enumerate(layers):
        src = x_d if li == 0 else scratch
        dst = out_d if li == nl - 1 else scratch
        if li > 0:
            S.final_wait("sp", [k for k in S.count if k.startswith(f"st{li - 1}_")])
        layer(l, src, dst, li)
    S.final_wait("sp", [k for k in S.count if k.startswith(f"st{nl - 1}_") or k.startswith("dbg_")])
    with nc.Block() as block:
        S.emit(block)
    return nc


def prep_layer_inputs(l, w_in, w_out, ada_w, ada_b, ln_g, ln_b, hgrn_lb_logits, gla_w_gk, gla_b_gk,
                      gdn_conv_w, gdn_a_log, gdn_dt_bias, gain_a, gain_b, gain_c):
    w = np.asarray(w_in[l], np.float32)
    offs = np.cumsum([0, 256, 256, 256, 256, 192, 192, 384, 16, 384, 1152, 6, 6, 384])
    qa, fa, ia, za, qb, kb, vb, lr, zb, qkvc, ac, bcc, zc = [w[:, offs[i]:offs[i + 1]] for i in range(13)]
    wp = np.zeros((D, NCOLS), np.float32)
    wp[:, OFF_QA:OFF_QA + 256] = qa
    wp[:, OFF_FA:OFF_FA + 256] = fa
    for h in range(4):
        wp[:, OFF_QB + 64 * h:OFF_QB + 64 * h + 48] = qb[:, 48 * h:48 * (h + 1)]
        wp[:, OFF_KB + 64 * h:OFF_KB + 64 * h + 48] = kb[:, 48 * h:48 * (h + 1)]
    wp[:, OFF_IA:OFF_IA + 256] = ia
    wp[:, OFF_AB:OFF_AB + 6] = ac
    wp[:, OFF_AB + 6:OFF_AB + 12] = bcc
    wp[:, OFF_VB:OFF_VB + 384] = vb
    wp[:, OFF_Z:OFF_Z + 256] = za
    wp[:, OFF_Z + 256:OFF_Z + 640] = zb
    wp[:, OFF_Z + 640:OFF_Z + 1024] = zc
    wp[:, OFF_LR:OFF_LR + 16] = lr
    wp[:, OFF_C:OFF_C + 1152] = qkvc
    rows = np.zeros((1, NROW), np.float32)
    rows[0, R_GAIN:R_GAIN + 1024] = np.concatenate([np.tile(gain_a[l], 4), np.tile(gain_b[l], 4), np.tile(gain_c[l], 6)])
    rows[0, R_LNG:R_LNG + 1024] = ln_g[l]
    rows[0, R_LNB:R_LNB + 1024] = ln_b[l]
    rows[0, R_DT:R_DT + 6] = gdn_dt_bias[l]
    rows[0, R_ALOG:R_ALOG + 6] = gdn_a_log[l]
    wgk = np.zeros((17, 256), np.float32)
    for h in range(4):
        wgk[0:16, 64 * h:64 * h + 48] = gla_w_gk[l][:, 48 * h:48 * (h + 1)]
        wgk[16, 64 * h:64 * h + 48] = gla_b_gk[l][48 * h:48 * (h + 1)]
    cw = np.asarray(gdn_conv_w[l], np.float32)
    convw = np.ascontiguousarray(cw.T.reshape(9, 128, 4).transpose(1, 0, 2)).reshape(128, 36)
    return {f"win{l}": wp, f"wout{l}": np.ascontiguousarray(w_out[l], np.float32),
            f"adaw{l}": np.ascontiguousarray(ada_w[l], np.float32),
            f"adab{l}": np.ascontiguousarray(ada_b[l], np.float32).reshape(1, -1),
            f"rows{l}": rows, f"logits{l}": np.ascontiguousarray(hgrn_lb_logits, np.float32).reshape(1, -1),
            f"wgk{l}": wgk, f"convw{l}": convw}


def core_inputs(xc, cc, layer_maps):
    nseq = xc.shape[0]
    m = {"x": np.ascontiguousarray(xc.reshape(-1, D), np.float32),
         "cT": np.ascontiguousarray(np.asarray(cc, np.float32).reshape(nseq, 8, 128).transpose(2, 1, 0)).reshape(128, 8 * nseq),
         "consts": make_consts()}
    for lm in layer_maps:
        m.update(lm)
    return m


_NC_CACHE = {}


def kernel(x, c, w_in, w_out, ada_w, ada_b, ln_g, ln_b, hgrn_lb_logits, gla_w_gk, gla_b_gk,
           gdn_conv_w, gdn_a_log, gdn_dt_bias, gain_a, gain_b, gain_c):
    x = np.asarray(x, np.float32)
    c = np.asarray(c, np.float32)
    params = [np.asarray(a, np.float32) for a in (w_in, w_out, ada_w, ada_b, ln_g, ln_b, hgrn_lb_logits, gla_w_gk, gla_b_gk,
                                                  gdn_conv_w, gdn_a_log, gdn_dt_bias, gain_a, gain_b, gain_c)]
    nseq = BATCH // NCORES
    ntile = SEQ // 128
    layer_maps = [prep_layer_inputs(l, *params) for l in range(DEPTH)]
    key = ("full",)
    if key not in _NC_CACHE:
        _NC_CACHE[key] = build(nseq, ntile, list(range(DEPTH)))
    nc = _NC_CACHE[key]
    in_maps = [core_inputs(x[i * nseq:(i + 1) * nseq], c[i * nseq:(i + 1) * nseq], layer_maps) for i in range(NCORES)]
    res = run_bass_kernel_spmd(nc, in_maps, core_ids=list(range(NCORES)))
    out = np.concatenate([r["out"].reshape(nseq, SEQ, D) for r in res.results], axis=0)
    return out.astype(np.float32)
```
